# Optimizing a Trainium2 kernel written in Bass

```python
import jax, jax.numpy as jnp
from jax import lax
import numpy as np

D_MODEL = 1024
BATCH = 8
SEQ = 8192
DEPTH = 1

CHUNK = 64
D_MIX = D_MODEL
D_CONV = D_MIX // 2
N_HEADS = 8
HEAD_DIM = (D_MIX - D_CONV) // N_HEADS
D_ATT = N_HEADS * HEAD_DIM
CONV_WIDTH = 31
Q_BLOCK = 128
N_GROUPS = 4
EXPERTS_PER_GROUP = 8
N_EXPERTS = N_GROUPS * EXPERTS_PER_GROUP
TOP_K = 2
D_EXPERT = D_MODEL // 4
ROW_BLOCK = 256
EPS = 1e-6
D_IN = 2 * D_CONV + 3 * D_ATT + N_HEADS

kernel_name = 'hymba_conformer_fox_hiermoe'


def rms_norm(x, g):
    xf = x.astype(jnp.float32)
    y = xf * lax.rsqrt(jnp.mean(xf * xf, axis=-1, keepdims=True) + EPS)
    return (y * g.astype(jnp.float32)).astype(x.dtype)


def layer_norm(x, g, b):
    xf = x.astype(jnp.float32)
    mu = jnp.mean(xf, axis=-1, keepdims=True)
    xc = xf - mu
    y = xc * lax.rsqrt(jnp.mean(xc * xc, axis=-1, keepdims=True) + EPS)
    return (y * g.astype(jnp.float32) + b.astype(jnp.float32)).astype(x.dtype)


def conformer_conv(u_val, u_gate, w_dw, b_dw, ln_g, ln_b):
    a = u_val * jax.nn.sigmoid(u_gate)
    c = a.shape[-1]
    a = lax.conv_general_dilated(
        a, w_dw[:, None, :].astype(a.dtype), window_strides=(1,),
        padding=[(CONV_WIDTH - 1, 0)],
        dimension_numbers=('NWC', 'WIO', 'NWC'),
        feature_group_count=c) + b_dw.astype(a.dtype)
    a = layer_norm(a, ln_g, ln_b)
    return jax.nn.silu(a)


def forgetting_attention(q, k, v, log_f):
    s_len = q.shape[1]
    cum = jnp.transpose(jnp.cumsum(log_f, axis=1), (0, 2, 1))
    scale = HEAD_DIM ** -0.5
    outs = []
    for i in range(s_len // Q_BLOCK):
        q0 = i * Q_BLOCK
        k_end = q0 + Q_BLOCK
        s = jnp.einsum('bqhd,bkhd->bhqk', q[:, q0:k_end], k[:, :k_end],
                       preferred_element_type=jnp.float32) * scale
        s = s + cum[:, :, q0:k_end, None] - cum[:, :, None, :k_end]
        mask = (q0 + jnp.arange(Q_BLOCK))[:, None] >= jnp.arange(k_end)[None, :]
        s = jnp.where(mask, s, -jnp.inf)
        p = jax.nn.softmax(s, axis=-1)
        outs.append(jnp.einsum('bhqk,bkhd->bqhd', p.astype(v.dtype), v[:, :k_end]))
    return jnp.concatenate(outs, axis=1)


def hier_route(h, w_r1, b_r1, w_r2, b_r2):
    lg1 = (h @ w_r1).astype(jnp.float32) + b_r1.astype(jnp.float32)
    p1 = jax.nn.softmax(lg1, axis=-1)
    grp = jnp.argmax(lg1, axis=-1)
    p1_sel = jnp.take_along_axis(p1, grp[:, None], axis=1)[:, 0]
    lg2_all = jnp.einsum('td,gde->tge', h, w_r2).astype(jnp.float32) + b_r2.astype(jnp.float32)
    lg2 = jnp.take_along_axis(lg2_all, grp[:, None, None], axis=1)[:, 0]
    top_v, top_j = lax.top_k(lg2, TOP_K)
    p2 = jax.nn.softmax(top_v, axis=-1)
    weights = p1_sel[:, None] * p2
    experts = grp[:, None] * EXPERTS_PER_GROUP + top_j
    return experts.astype(jnp.int32), weights


def hier_moe(h, w_r1, b_r1, w_r2, b_r2, w_gate, w_up, w_down):
    t_len, d = h.shape
    experts, weights = hier_route(h, w_r1, b_r1, w_r2, b_r2)
    n_assign = t_len * TOP_K
    flat_e = experts.reshape(-1)
    flat_tok = jnp.repeat(jnp.arange(t_len, dtype=jnp.int32), TOP_K)
    flat_w = weights.reshape(-1)
    order = jnp.argsort(flat_e)
    se, stok, sw = flat_e[order], flat_tok[order], flat_w[order]
    counts = jnp.bincount(flat_e, length=N_EXPERTS)
    starts = jnp.cumsum(counts) - counts
    padded_counts = ((counts + ROW_BLOCK - 1) // ROW_BLOCK) * ROW_BLOCK
    padded_ends = jnp.cumsum(padded_counts)
    padded_starts = padded_ends - padded_counts
    dest = padded_starts[se] + (jnp.arange(n_assign) - starts[se])
    n_blocks = -(-n_assign // ROW_BLOCK) + N_EXPERTS
    n_rows = n_blocks * ROW_BLOCK
    row_tok = jnp.full((n_rows,), t_len, jnp.int32).at[dest].set(stok)
    row_w = jnp.zeros((n_rows,), jnp.float32).at[dest].set(sw)
    block_start = jnp.arange(n_blocks) * ROW_BLOCK
    block_e = jnp.minimum(jnp.searchsorted(padded_ends, block_start, side='right'), N_EXPERTS - 1)
    h_pad = jnp.concatenate([h, jnp.zeros((1, d), h.dtype)], axis=0)
    xin = h_pad[row_tok].reshape(n_blocks, ROW_BLOCK, d)

    def expert_block(args):
        xb, e = args
        g = xb @ w_gate[e]
        u = xb @ w_up[e]
        return (jax.nn.silu(g) * u) @ w_down[e]

    yb = lax.map(expert_block, (xin, block_e))
    y = yb.reshape(n_rows, d) * row_w[:, None].astype(yb.dtype)
    return jax.ops.segment_sum(y, row_tok, num_segments=t_len + 1)[:t_len]


def setup_inputs(seed: int = 0) -> dict:
    key = jax.random.key(seed)
    ks = jax.random.split(key, 24)
    f32 = jnp.float32
    L = DEPTH
    nrm = lambda k, shape, s: jax.random.normal(k, shape, f32) * s
    gain = lambda k, shape: 1.0 + 0.05 * jax.random.normal(k, shape, f32)
    b_f = jnp.linspace(1.0, 6.0, N_HEADS, dtype=f32)[None, :] + 0.1 * jax.random.normal(ks[3], (L, N_HEADS), f32)
    return {
        'x': jax.random.normal(ks[0], (BATCH, SEQ, D_MODEL), f32),
        'norm1_g': gain(ks[1], (L, D_MODEL)),
        'w_in': nrm(ks[2], (L, D_MODEL, D_IN), D_MODEL ** -0.5),
        'b_f': b_f,
        'w_dw': nrm(ks[4], (L, CONV_WIDTH, D_CONV), CONV_WIDTH ** -0.5),
        'b_dw': nrm(ks[5], (L, D_CONV), 0.02),
        'conv_ln_g': gain(ks[6], (L, D_CONV)),
        'conv_ln_b': nrm(ks[7], (L, D_CONV), 0.02),
        'out_g_conv': gain(ks[8], (L, D_CONV)),
        'out_g_att': gain(ks[9], (L, D_ATT)),
        'w_out': nrm(ks[10], (L, D_MIX, D_MODEL), D_MIX ** -0.5),
        'norm2_g': gain(ks[11], (L, D_MODEL)),
        'w_r1': nrm(ks[12], (L, D_MODEL, N_GROUPS), D_MODEL ** -0.5),
        'b_r1': nrm(ks[13], (L, N_GROUPS), 0.01),
        'w_r2': nrm(ks[14], (L, N_GROUPS, D_MODEL, EXPERTS_PER_GROUP), D_MODEL ** -0.5),
        'b_r2': nrm(ks[15], (L, N_GROUPS, EXPERTS_PER_GROUP), 0.01),
        'w_gate': nrm(ks[16], (L, N_EXPERTS, D_MODEL, D_EXPERT), D_MODEL ** -0.5),
        'w_up': nrm(ks[17], (L, N_EXPERTS, D_MODEL, D_EXPERT), D_MODEL ** -0.5),
        'w_down': nrm(ks[18], (L, N_EXPERTS, D_EXPERT, D_MODEL), D_EXPERT ** -0.5),
        'final_g': gain(ks[19], (D_MODEL,)),
    }


def reference(x, norm1_g, w_in, b_f, w_dw, b_dw, conv_ln_g, conv_ln_b, out_g_conv, out_g_att,
              w_out, norm2_g, w_r1, b_r1, w_r2, b_r2, w_gate, w_up, w_down, final_g):
    b, s, d = x.shape
    o1 = D_CONV
    o2 = o1 + D_CONV
    o3 = o2 + D_ATT
    o4 = o3 + D_ATT
    o5 = o4 + D_ATT
    for l in range(DEPTH):
        h = rms_norm(x, norm1_g[l])
        z = h @ w_in[l]
        y_conv = conformer_conv(z[..., :o1], z[..., o1:o2], w_dw[l], b_dw[l], conv_ln_g[l], conv_ln_b[l])
        q = z[..., o2:o3].reshape(b, s, N_HEADS, HEAD_DIM)
        k = z[..., o3:o4].reshape(b, s, N_HEADS, HEAD_DIM)
        v = z[..., o4:o5].reshape(b, s, N_HEADS, HEAD_DIM)
        log_f = jax.nn.log_sigmoid(z[..., o5:].astype(jnp.float32) + b_f[l].astype(jnp.float32))
        y_att = forgetting_attention(q, k, v, log_f).reshape(b, s, D_ATT)
        mix = jnp.concatenate([rms_norm(y_conv, out_g_conv[l]), rms_norm(y_att, out_g_att[l])], axis=-1)
        x = x + mix @ w_out[l]
        h2 = rms_norm(x, norm2_g[l]).reshape(b * s, d)
        x = x + hier_moe(h2, w_r1[l], b_r1[l], w_r2[l], b_r2[l], w_gate[l], w_up[l], w_down[l]).reshape(b, s, d)
    return rms_norm(x, final_g)
```

```python
import numpy as np
import ml_dtypes
import concourse.bass as bass
import concourse.mybir as mybir
from concourse.bass_utils import run_bass_kernel_spmd

F32 = mybir.dt.float32
BF16 = mybir.dt.bfloat16
I32 = mybir.dt.int32
AF = mybir.ActivationFunctionType
ALU = mybir.AluOpType
AX = mybir.AxisListType

D = 1024
DIN = 2568
NH = 8
HD = 64
CW = 31
NE = 32
DE = 256
CAP = 768
TS = 256
EPS = 1e-6
MT = 512


class TR:
    __slots__ = ("name", "w", "r", "dsem", "dcnt", "multi")

    all = []

    def __init__(self, name):
        TR.all.append(self)
        self.name = name
        self.w = {}
        self.r = {}
        self.dsem = {}
        self.dcnt = {}
        self.multi = False


class Sched:
    def __init__(self, nc):
        self.nc = nc
        self.eng = {"pe": nc.tensor, "act": nc.scalar, "dve": nc.vector, "pool": nc.gpsimd, "sp": nc.sync}
        self.sem = {k: nc.alloc_semaphore("sem_" + k) for k in self.eng}
        self.cnt = {k: 0 for k in self.eng}
        self.waited = {k: {} for k in self.eng}
        self.nsem = 0
        self.pool = {"sw": [], "hw": []}

    def _deps(self, e, reads, writes):
        deps = {}
        for t in list(reads) + list(writes):
            for k, (s, v) in t.w.items():
                if deps.get(k, (None, 0))[1] < v:
                    deps[k] = (s, v)
        for t in writes:
            for k, (s, v) in t.r.items():
                if deps.get(k, (None, 0))[1] < v:
                    deps[k] = (s, v)
        wd = self.waited[e]
        own = id(self.sem[e])
        for k, (s, v) in deps.items():
            if e == "pe" and k == own:
                continue
            if wd.get(k, 0) < v:
                self.eng[e].wait_ge(s, v)
                wd[k] = v

    def op(self, e, fn, reads=(), writes=()):
        self._deps(e, reads, writes)
        ins = fn()
        s = self.sem[e]
        ins.then_inc(s, 1)
        self.cnt[e] += 1
        v = self.cnt[e]
        k = id(s)
        for t in writes:
            t.w = {k: (s, v)}
            t.r = {}
        for t in reads:
            t.r[k] = (s, v)

    def dma(self, q, out, in_, reads=(), writes=(), owner=None, n=1, fn=None):
        self._deps(q, reads, writes)
        kind = "sw" if q == "pool" else "hw"
        if kind not in owner.dsem:
            if self.pool[kind]:
                owner.dsem[kind], owner.dcnt[kind] = self.pool[kind].pop()
            else:
                owner.dsem[kind] = self.nc.alloc_semaphore("d%s_%d" % (kind, self.nsem))
                owner.dcnt[kind] = 0
                self.nsem += 1
        if fn is None:
            ins = self.eng[q].dma_start(out=out, in_=in_)
        else:
            ins = fn()
        sem = owner.dsem[kind]
        ins.then_inc(sem, 16)
        owner.dcnt[kind] += 16
        k = id(sem)
        ent = (sem, owner.dcnt[kind])
        for t in writes:
            if not t.multi:
                t.w = {}
            t.w[k] = ent
            t.r = {}
        for t in reads:
            t.r[k] = ent

    def wait_all(self, e, tracked):
        self._deps(e, tracked, tracked)


def build(S, debug=None):
    NM = S // MT
    NT = S // 128
    NSLOT = NE * CAP
    nc = bass.Bass("TRN2", target_bir_lowering=False)
    TR.all = []
    sc = Sched(nc)

    def din(name, shape, dt=F32):
        return nc.dram_tensor(name, list(shape), dt, kind="ExternalInput").ap()

    def dscr(name, shape, dt):
        return nc.dram_tensor(name, list(shape), dt, kind=("ExternalOutput" if debug else "Internal")).ap()

    x = din("x", [S, D])
    w_in = din("w_in", [D, DIN])
    w_out = din("w_out", [D, D])
    wr = din("wr", [D, 36])
    w_gate = din("w_gate", [NE * 128, 8 * DE])
    w_up = din("w_up", [NE * 128, 8 * DE])
    w_down = din("w_down", [NE * 128, 2 * D])
    g1T = din("g1T", [128, 8])
    g2T = din("g2T", [128, 8])
    gconvT = din("gconvT", [128, 4])
    gattT = din("gattT", [128, 4])
    bdwT = din("bdwT", [128, 4])
    lngT = din("lngT", [128, 4])
    lnbT = din("lnbT", [128, 4])
    wdwT = din("wdwT", [128, 4 * CW])
    negbf = din("negbf", [128, 1])
    fing = din("fing", [128, D])
    g2bc = din("g2bc", [128, D])
    rbias = din("rbias", [128, 36])
    consts = din("consts", [128, 5 * 128])
    ebase = din("ebase", [128, NE])
    thr256 = din("thr256", [128, 33])
    tst256 = din("tst256", [128, NSLOT // TS])
    pcol = din("pcol", [128, 1])
    iota_tok = din("iota_tok", [128, NT], I32)
    out = nc.dram_tensor("out", [S, D], F32, kind="ExternalOutput").ap()

    Qd = dscr("Qd", [4, NM, 128, MT], BF16)
    Kd = dscr("Kd", [4, NM, 128, MT], BF16)
    Vd = dscr("Vd", [4, NM, 128, 4, 130], BF16)
    Ad = dscr("Ad", [NM, 128, MT], BF16)
    Mcd = dscr("Mcd", [NM, 128, 4, MT], BF16)
    Yad = dscr("Yad", [NM, 64, 8, MT], BF16)
    Xmd = dscr("Xmd", [S, D], F32)
    H2d = dscr("H2d", [S + 128, D], BF16)
    RTd = dscr("RTd", [NE * S + 128, 1], I32)
    Ysd = dscr("Ysd", [NSLOT, D], BF16)


    from contextlib import ExitStack
    stk = [ExitStack()]

    def sb(name, shape, dt=F32):
        return stk[0].enter_context(nc.sbuf_tensor(name, list(shape), dt))

    dma_owners = []

    def barrier():
        ents = [(sc.sem[k], sc.cnt[k]) for k in sc.eng if sc.cnt[k] > 0]
        for t in TR.all:
            for kd in t.dsem:
                if t.dcnt[kd] > 0:
                    ents.append((t.dsem[kd], t.dcnt[kd]))
        for e in sc.eng:
            wd = sc.waited[e]
            for (sm, v) in ents:
                if sm is sc.sem[e]:
                    continue
                if wd.get(id(sm), 0) < v:
                    sc.eng[e].wait_ge(sm, v)
                    wd[id(sm)] = v

    def new_phase():
        barrier()
        for t in TR.all:
            for kd in list(t.dsem):
                sc.pool[kd].append((t.dsem[kd], t.dcnt[kd]))
            t.dsem = {}
            t.dcnt = {}
            t.w = {}
            t.r = {}
        stk[0].close()
        stk[0] = ExitStack()

    psn = [0]

    def ps(name, shape, dt=F32):
        psn[0] += 1
        return stk[0].enter_context(nc.psum_tensor("%s_%d" % (name, psn[0]), list(shape), dt))

    def mkpsum(ntp, npm):
        tp_ = [ps("tp%d" % i, [128, 1024], BF16) for i in range(ntp)]
        ttp_ = [TR("tp%d" % i) for i in range(ntp)]
        pm_ = [ps("pm%d" % i, [128, 512]) for i in range(npm)]
        tpm_ = [TR("pm%d" % i) for i in range(npm)]
        return tp_, ttp_, pm_, tpm_

    V, A, P, PE, SP = "dve", "act", "pool", "pe", "sp"
    E = sc.eng
    rb_rt = E[P].to_reg(NE * S + 127)
    rb_h2 = E[P].to_reg(S + 127)
    rb_ys = E[P].to_reg(NSLOT - 1)

    def E_pool_to_reg(v):
        return E[P].to_reg(v)

    cst = sb("cst", [128, 5 * 128]); t_cst = TR("cst")
    sc.dma(SP, cst[:], consts, writes=[t_cst], owner=t_cst)
    ident_f = cst[:, 0:128]
    ones_f = cst[:, 128:256]
    identb = sb("identb", [128, 128], BF16); t_identb = TR("identb")
    onesb = sb("onesb", [128, 128], BF16)
    lstrb = sb("lstrb", [128, 128], BF16)
    negmb = sb("negmb", [128, 128], BF16)
    sc.op(V, lambda: E[V].tensor_copy(out=identb[:], in_=cst[:, 0:128]), [t_cst], [t_identb])
    sc.op(V, lambda: E[V].tensor_copy(out=onesb[:], in_=cst[:, 128:256]), [t_cst], [t_identb])
    sc.op(V, lambda: E[V].tensor_copy(out=lstrb[:], in_=cst[:, 256:384]), [t_cst], [t_identb])
    sc.op(V, lambda: E[V].tensor_copy(out=negmb[:], in_=cst[:, 384:512]), [t_cst], [t_identb])
    ones512 = sb("ones512", [128, MT]); t_ones512 = TR("ones512")
    sc.op(P, lambda: E[P].memset(ones512[:], 1.0), [], [t_ones512])
    epsb = sb("epsb", [128, 1]); t_eps = TR("epsb")
    sc.op(P, lambda: E[P].memset(epsb[:], EPS), [], [t_eps])
    t_c = TR("consts_all")

    def load_small(name, ap, shape, dt=F32):
        t = sb(name, shape, dt)
        sc.dma(SP, t[:], ap, writes=[t_c], owner=t_c)
        return t

    t_c.multi = True
    g1s = load_small("g1s", g1T, [128, 8])
    g2s = load_small("g2s", g2T, [128, 8])
    gconvs = load_small("gconvs", gconvT, [128, 4])
    gatts = load_small("gatts", gattT, [128, 4])
    bdws = load_small("bdws", bdwT, [128, 4])
    lngs = load_small("lngs", lngT, [128, 4])
    lnbs = load_small("lnbs", lnbT, [128, 4])
    wdws = load_small("wdws", wdwT, [128, 4 * CW])
    negbs = load_small("negbs", negbf, [128, 1])
    rbs = load_small("rbs", rbias, [128, 36])
    ebs = load_small("ebs", ebase, [128, NE])
    thrs = load_small("thrs", thr256, [128, 33])
    tsts = load_small("tsts", tst256, [128, NSLOT // TS])
    pcols = load_small("pcols", pcol, [128, 1])
    iotas = load_small("iotas", iota_tok, [128, NT], I32)

    biasT = sb("biasT", [128, NT, 8]); t_biasT = TR("biasT")
    dest1 = sb("dest1", [128, NT], I32); dest2 = sb("dest2", [128, NT], I32)
    wgt1 = sb("wgt1", [128, NT]); wgt2 = sb("wgt2", [128, NT])
    t_route = TR("route")
    NTL = NSLOT // TS
    widx = sb("widx", [128, NTL], I32); t_widx = TR("widx")
    ridx = sb("ridx", [128, NTL, TS // 128], I32)
    rb_w = E_pool_to_reg(NE * 128 - 1)

    selB = sb("selB", [128, 8, 128], BF16); t_sel = TR("selB")
    selc = sb("selc", [128, 8]); t_selc = TR("selc")
    sc.op(V, lambda: E[V].tensor_tensor(out=selc[:], in0=cst[:, 0:8], in1=cst[:, 32:40], op=ALU.add), [t_cst], [t_selc])
    for h in range(8):
        sc.op(V, lambda: E[V].tensor_scalar(out=selB[:, h, :], in0=cst[:, 128:256], scalar1=selc[:, h:h + 1], scalar2=None,
                                            op0=ALU.mult), [t_selc, t_cst], [t_sel])

    e64 = sb("e64", [65, 64]); t_e64 = TR("e64")
    sc.op(V, lambda: E[V].memset(e64[:], 0.0), [], [t_e64])
    sc.op(V, lambda: E[V].memset(e64[64:65, :], 1.0), [t_e64], [t_e64])
    persist = stk[0]
    stk[0] = ExitStack()
    WinB = sb("WinB", [128, 8, DIN], BF16); t_win = TR("WinB")
    WfB = sb("WfB", [128, 8, 128], BF16); t_wf = TR("WfB")
    stg = [sb("stg%d" % i, [128, DIN]) for i in range(1)] * 2
    t_stg = [TR("stg%d" % i) for i in range(1)] * 2
    sc.op(P, lambda: E[P].memset(WfB[:], 0.0), [], [t_wf])
    w_in_v = w_in.rearrange("(kc p) n -> p kc n", p=128)
    for kc in range(8):
        b = kc % 2
        sc.dma(SP, stg[b][:], w_in_v[:, kc, :], writes=[t_stg[b]], owner=t_stg[b])
        eng = A if kc % 2 == 0 else V
        if eng == A:
            sc.op(A, lambda: E[A].activation(out=WinB[:, kc, :], in_=stg[b][:], func=AF.Copy, scale=g1s[:, kc:kc + 1]),
                  [t_stg[b], t_c], [t_win])
        else:
            sc.op(V, lambda: E[V].tensor_scalar(out=WinB[:, kc, :], in0=stg[b][:], scalar1=g1s[:, kc:kc + 1], scalar2=None,
                                                op0=ALU.mult), [t_stg[b], t_c], [t_win])
    for q in range(4):
        sc.op(P, lambda: E[P].tensor_copy(out=WfB[:, :, q * 32:q * 32 + 8], in_=WinB[:, :, 2560:2568]), [t_win], [t_wf])

    diagW = sb("diagW", [128, CW, 4, 128], BF16); t_diag = TR("diagW")
    for k in range(CW):
        for c in range(4):
            eng = V if (k + c) % 2 == 0 else P
            sc.op(eng, lambda: E[eng].tensor_scalar(out=diagW[:, k, c, :], in0=cst[:, 0:128],
                                                    scalar1=wdws[:, c * CW + k:c * CW + k + 1], scalar2=None, op0=ALU.mult),
                  [t_cst, t_c], [t_diag])

    tp, t_tp, pm, t_pm = mkpsum(2, 6)

    xs = [sb("xs%d" % i, [128, 4, D]) for i in range(2)]; t_xs = [TR("xs%d" % i) for i in range(2)]
    junk = sb("junk", [128, D], BF16); t_junk = TR("junk")
    ssq = sb("ssq", [128, 4]); t_ssq = TR("ssq")
    rstd = sb("rstd", [128, 4]); t_rstd = TR("rstd")
    hb = sb("hb", [128, 4, D], BF16); t_hb = TR("hb")
    hT = sb("hT", [128, 8, MT], BF16); t_hT = TR("hT")
    aT = [sb("aT%d" % i, [128, 4, 30 + MT], BF16) for i in range(2)]; t_aT = [TR("aT%d" % i) for i in range(2)]
    sg = sb("sg", [128, MT]); t_sg = TR("sg")
    QT = sb("QT", [128, 4, MT], BF16); t_QT = TR("QT")
    KT = sb("KT", [128, 4, MT], BF16); t_KT = TR("KT")
    Vt = sb("Vt", [128, 4, 8, 65], BF16); t_Vt = TR("Vt")
    ef = sb("ef", [128, MT]); t_ef = TR("ef")
    spf = sb("spf", [128, MT]); t_spf = TR("spf")
    cum = sb("cum", [128, MT]); t_cum = TR("cum")
    carry = sb("carry", [128, 1]); t_carry = TR("carry")
    hiall = sb("hiall", [128, MT], BF16); t_hiall = TR("hiall")
    arow = sb("arow", [128, MT], BF16); t_arow = TR("arow")
    uu = sb("uu", [128, 4, MT]); t_uu = TR("uu")
    usq = sb("usq", [128, 4, MT], BF16); t_usq = TR("usq")
    m1 = sb("m1", [128, MT]); t_m1 = TR("m1")
    rs1 = sb("rs1", [128, MT]); t_rs1 = TR("rs1")
    mixc = sb("mixc", [128, 4, MT], BF16); t_mixc = TR("mixc")
    t_Qd = TR("Qd"); t_Kd = TR("Kd"); t_Vd = TR("Vd"); t_Ad = TR("Ad"); t_Mcd = TR("Mcd")
    for t in (t_Qd, t_Kd, t_Vd, t_Ad, t_Mcd):
        t.multi = True

    sc.op(P, lambda: E[P].memset(Vt[:], 1.0), [], [t_Vt])
    sc.op(P, lambda: E[P].memset(carry[:], 0.0), [], [t_carry])
    sc.op(P, lambda: E[P].memset(aT[0][:], 0.0), [], [t_aT[0]])
    sc.op(P, lambda: E[P].memset(aT[1][:], 0.0), [], [t_aT[1]])

    x_v = x.rearrange("(m s p) d -> m p s d", p=128, s=4)

    def inproj(bank, tbank, col0, M=128, lhs_src=None):
        def f():
            ins = None
            for kc in range(8):
                lhsT = WinB[:, kc, col0:col0 + M] if lhs_src is None else lhs_src[:, kc, :]
                ins = E[PE].matmul(pm[bank][0:M, :], lhsT=lhsT, rhs=hT[:, kc, :], start=(kc == 0), stop=(kc == 7))
            return ins
        sc.op(PE, f, [t_win, t_wf, t_hT], [tbank])

    def ph_A0(m):
        xb = xs[m % 2]; txb = t_xs[m % 2]
        sc.dma(SP, xb[:], x_v[m], writes=[txb], owner=txb)
        for s in range(4):
            sc.op(A, lambda: E[A].activation(out=junk[:], in_=xb[:, s, :], func=AF.Square, accum_out=ssq[:, s:s + 1]),
                  [txb], [t_junk, t_ssq])
        sc.op(A, lambda: E[A].activation(out=rstd[:], in_=ssq[:], func=AF.Sqrt, scale=1.0 / D, bias=epsb[:, 0:1]),
              [t_ssq, t_eps], [t_rstd])
        sc.op(V, lambda: E[V].reciprocal(out=rstd[:], in_=rstd[:]), [t_rstd], [t_rstd])
        for s in range(4):
            sc.op(A, lambda: E[A].activation(out=hb[:, s, :], in_=xb[:, s, :], func=AF.Copy, scale=rstd[:, s:s + 1]),
                  [txb, t_rstd], [t_hb])

    def ph_A1(m):
        xb = xs[m % 2]; txb = t_xs[m % 2]
        ab = aT[m % 2]; tab = t_aT[m % 2]
        abp = aT[(m + 1) % 2]; tabp = t_aT[(m + 1) % 2]
        for s in range(4):
            tb = tp[s % 2]; ttb = t_tp[s % 2]

            def f():
                ins = None
                for kc in range(8):
                    ins = E[PE].transpose(out=tb[:, kc * 128:(kc + 1) * 128], in_=hb[:, s, kc * 128:(kc + 1) * 128],
                                          identity=identb[:])
                return ins
            sc.op(PE, f, [t_hb, t_identb], [ttb])
            eng = A if s % 2 == 0 else V
            if eng == A:
                sc.op(A, lambda: E[A].copy(out=hT[:, :, s * 128:(s + 1) * 128], in_=tb[:].rearrange("p (k t) -> p k t", k=8)),
                      [ttb], [t_hT])
            else:
                sc.op(V, lambda: E[V].tensor_copy(out=hT[:, :, s * 128:(s + 1) * 128],
                                                  in_=tb[:].rearrange("p (k t) -> p k t", k=8)), [ttb], [t_hT])
        if m > 0:
            sc.op(P, lambda: E[P].tensor_copy(out=ab[:, :, 0:30], in_=abp[:, :, MT:MT + 30]), [tabp], [tab])
        for c in range(4):
            inproj(0, t_pm[0], 512 + c * 128)
            sc.op(A, lambda: E[A].activation(out=sg[:], in_=pm[0][:], func=AF.Sigmoid), [t_pm[0]], [t_sg])
            inproj(1, t_pm[1], c * 128)
            sc.op(V, lambda: E[V].tensor_tensor(out=ab[:, c, 30:30 + MT], in0=pm[1][:], in1=sg[:], op=ALU.mult),
                  [t_pm[1], t_sg], [tab])
        for p in range(4):
            inproj(0, t_pm[0], 1024 + p * 128)
            sc.op(A, lambda: E[A].copy(out=QT[:, p, :], in_=pm[0][:]), [t_pm[0]], [t_QT])
            inproj(1, t_pm[1], 1536 + p * 128)
            sc.op(V, lambda: E[V].tensor_copy(out=KT[:, p, :], in_=pm[1][:]), [t_pm[1]], [t_KT])
        sc.dma(P, Qd[:, m].rearrange("a p t -> p a t"), QT[:], reads=[t_QT], writes=[t_Qd], owner=t_QT)
        sc.dma(P, Kd[:, m].rearrange("a p t -> p a t"), KT[:], reads=[t_KT], writes=[t_Kd], owner=t_KT)
        for s in range(4):
            bank = (s % 2)

            def f():
                ins = None
                for kc in range(8):
                    ins = E[PE].matmul(pm[bank][:], lhsT=hT[:, kc, s * 128:(s + 1) * 128], rhs=WinB[:, kc, 2048:2560],
                                       start=(kc == 0), stop=(kc == 7))
                return ins
            sc.op(PE, f, [t_win, t_hT], [t_pm[bank]])
            eng = A if s % 2 == 0 else V
            src = pm[bank][:].rearrange("p (h d) -> p h d", h=8)
            if eng == A:
                sc.op(A, lambda: E[A].copy(out=Vt[:, s, :, 0:64], in_=src), [t_pm[bank]], [t_Vt])
            else:
                sc.op(V, lambda: E[V].tensor_copy(out=Vt[:, s, :, 0:64], in_=src), [t_pm[bank]], [t_Vt])
        for a in range(4):
            sc.dma(P, Vd[a, m], Vt[:, :, 2 * a:2 * a + 2, :].rearrange("p s h c -> p s (h c)"),
                   reads=[t_Vt], writes=[t_Vd], owner=t_Vt)
        inproj(0, t_pm[0], 0, lhs_src=WfB)
        sc.op(A, lambda: E[A].activation(out=ef[:], in_=pm[0][:], func=AF.Exp, scale=-1.0, bias=negbs[:, 0:1]),
              [t_pm[0], t_c], [t_ef])
        sc.op(A, lambda: E[A].activation(out=spf[:], in_=ef[:], func=AF.Ln, bias=1.0), [t_ef], [t_spf])
        sc.op(V, lambda: E[V].tensor_tensor_scan(out=cum[:], data0=ones512[:], data1=spf[:], initial=carry[:, 0:1],
                                                 op0=ALU.mult, op1=ALU.subtract), [t_spf, t_carry, t_ones512], [t_cum])
        sc.op(V, lambda: E[V].tensor_copy(out=carry[:], in_=cum[:, MT - 1:MT]), [t_cum], [t_carry])
        sc.op(V, lambda: E[V].tensor_scalar(out=hiall[:], in0=cum[:], scalar1=8.0, scalar2=None, op0=ALU.mult), [t_cum], [t_hiall])
        sc.op(P, lambda: E[P].tensor_copy(out=arow[:], in_=hiall[:]), [t_hiall], [t_arow])
        for q in (1, 3):
            sc.op(V, lambda: E[V].scalar_tensor_tensor(out=arow[q * 32:q * 32 + 32, :], in0=cum[q * 32:q * 32 + 32, :], scalar=8.0,
                                                       in1=hiall[q * 32:q * 32 + 32, :], op0=ALU.mult, op1=ALU.subtract),
                  [t_cum, t_hiall, t_arow], [t_arow])
        sc.dma(P, Ad[m], arow[:], reads=[t_arow], writes=[t_Ad], owner=t_arow)
        for s in range(4):
            sc.op(PE, lambda: E[PE].transpose(out=pm[1][:, 0:8], in_=cum[0:8, s * 128:(s + 1) * 128], identity=cst[0:8, 0:8]),
                  [t_cum, t_cst], [t_pm[1]])
            sc.op(V, lambda: E[V].tensor_scalar(out=biasT[:, m * 4 + s, :], in0=pm[1][:, 0:8], scalar1=-1.0, scalar2=None,
                                                op0=ALU.mult), [t_pm[1]], [t_biasT])

    def stat(bank, src, tsrc):
        ones_ = onesb[:] if src is usq else cst[:, 128:256]

        def f():
            ins = None
            for c in range(4):
                ins = E[PE].matmul(pm[bank][:], lhsT=ones_, rhs=src[:, c, :], start=(c == 0), stop=(c == 3))
            return ins
        sc.op(PE, f, [t_cst, t_identb, tsrc], [t_pm[bank]])

    def ph_B1(m):
        ab = aT[m % 2]; tab = t_aT[m % 2]
        for c in range(4):
            bank = 2 + (c % 2)

            def f():
                ins = None
                for k in range(CW):
                    ins = E[PE].matmul(pm[bank][:], lhsT=diagW[:, k, c, :], rhs=ab[:, c, k:k + MT], start=(k == 0),
                                       stop=(k == CW - 1))
                return ins
            sc.op(PE, f, [t_diag, tab], [t_pm[bank]])
            sc.op(A, lambda: E[A].activation(out=uu[:, c, :], in_=pm[bank][:], func=AF.Identity, bias=bdws[:, c:c + 1]),
                  [t_pm[bank], t_c], [t_uu])
            sc.op(A, lambda: E[A].activation(out=usq[:, c, :], in_=pm[bank][:], func=AF.Square, bias=bdws[:, c:c + 1]),
                  [t_pm[bank], t_c], [t_usq])

        stat(4, uu, t_uu)
        stat(5, usq, t_usq)

    def ph_B2(m):
        sc.op(V, lambda: E[V].tensor_scalar(out=m1[:], in0=pm[4][:], scalar1=1.0 / 512, scalar2=None, op0=ALU.mult), [t_pm[4]], [t_m1])
        sc.op(V, lambda: E[V].tensor_tensor(out=rs1[:], in0=m1[:], in1=m1[:], op=ALU.mult), [t_m1], [t_rs1])
        sc.op(V, lambda: E[V].scalar_tensor_tensor(out=rs1[:], in0=pm[5][:], scalar=1.0 / 512, in1=rs1[:], op0=ALU.mult,
                                                   op1=ALU.subtract), [t_pm[5], t_rs1], [t_rs1])
        sc.op(A, lambda: E[A].activation(out=rs1[:], in_=rs1[:], func=AF.Sqrt, bias=epsb[:, 0:1]), [t_rs1, t_eps], [t_rs1])
        sc.op(V, lambda: E[V].reciprocal(out=rs1[:], in_=rs1[:]), [t_rs1], [t_rs1])
        for c in range(4):
            eng = V
            sc.op(eng, lambda: E[eng].tensor_tensor(out=uu[:, c, :], in0=uu[:, c, :], in1=m1[:], op=ALU.subtract), [t_uu, t_m1], [t_uu])
            sc.op(eng, lambda: E[eng].tensor_tensor(out=uu[:, c, :], in0=uu[:, c, :], in1=rs1[:], op=ALU.mult), [t_uu, t_rs1], [t_uu])
            sc.op(A, lambda: E[A].activation(out=uu[:, c, :], in_=uu[:, c, :], func=AF.Silu, scale=lngs[:, c:c + 1],
                                             bias=lnbs[:, c:c + 1]), [t_uu, t_c], [t_uu])
            sc.op(A, lambda: E[A].activation(out=usq[:, c, :], in_=uu[:, c, :], func=AF.Square), [t_uu], [t_usq])
        stat(4, usq, t_usq)
        sc.op(A, lambda: E[A].activation(out=rs1[:], in_=pm[4][:], func=AF.Sqrt, scale=1.0 / 512, bias=epsb[:, 0:1]),
              [t_pm[4], t_eps], [t_rs1])
        sc.op(V, lambda: E[V].reciprocal(out=rs1[:], in_=rs1[:]), [t_rs1], [t_rs1])
        for c in range(4):
            eng = V
            sc.op(eng, lambda: E[eng].tensor_tensor(out=mixc[:, c, :], in0=uu[:, c, :], in1=rs1[:], op=ALU.mult),
                  [t_uu, t_rs1], [t_mixc])
        sc.dma(P, Mcd[m], mixc[:], reads=[t_mixc], writes=[t_Mcd], owner=t_mixc)


    ph_A0(0)
    ph_A1(0)
    for m in range(NM):
        if m + 1 < NM:
            ph_A0(m + 1)
        ph_B1(m)
        if m + 1 < NM:
            ph_A1(m + 1)
        ph_B2(m)

    if debug == "p1a":
        sc.wait_all(P, [t_Qd, t_Kd, t_Vd, t_Ad, t_Mcd, t_biasT])
        return nc

    new_phase()
    tp, t_tp, pm, t_pm = mkpsum(0, 8)
    NSB = 5
    Qs = [[sb("Qs%d_%d" % (i, j), [128, MT], BF16) for j in range(2)] for i in range(2)]
    t_Qs = [[TR("Qs%d_%d" % (i, j)) for j in range(2)] for i in range(2)]
    NKB = 3
    Kc = [[sb("Kc%d_%d" % (i, j), [128, MT], BF16) for j in range(2)] for i in range(NKB)]
    t_Kc = [[TR("Kc%d_%d" % (i, j)) for j in range(2)] for i in range(NKB)]
    Vc = [sb("Vc%d" % i, [128, 4, 2, 128], BF16) for i in range(NKB)]; t_Vc = [TR("Vc%d" % i) for i in range(NKB)]
    NPB = 4
    Pt = [sb("Pt%d" % i, [128, MT], BF16) for i in range(NPB)]; t_Pt = [TR("Pt%d" % i) for i in range(NPB)]
    Osb = [sb("Osb%d" % i, [65, MT]) for i in range(2)]; t_Osb = [TR("Osb%d" % i) for i in range(2)]
    Ya = [sb("Ya%d" % i, [64, 2, MT], BF16) for i in range(2)]; t_Ya = [TR("Ya%d" % i) for i in range(2)]
    for i_ in range(2):
        for j_ in range(2):
            sc.op(P, lambda: E[P].memset(Qs[i_][j_][:], 0.0), [], [t_Qs[i_][j_]])
            t_Qs[i_][j_].multi = True
    for i_ in range(NKB):
        for j_ in range(2):
            sc.op(P, lambda: E[P].memset(Kc[i_][j_][:], 0.0), [], [t_Kc[i_][j_]])
            sc.op(P, lambda: E[P].memset(Kc[i_][j_][64:66, :], 1.0), [t_Kc[i_][j_]], [t_Kc[i_][j_]])
        sc.op(P, lambda: E[P].memset(Vc[i_][:], 0.0), [], [t_Vc[i_]])
        t_Vc[i_].multi = True
    t_Yad = TR("Yad"); t_Yad.multi = True

    steps = []
    grp = 0
    for a in range(4):
        for i in range(NM):
            for c in range(i + 1):
                for hh in range(2):
                    for jj in range(4):
                        steps.append(dict(a=a, i=i, c=c, hh=hh, jj=jj, grp=grp,
                                          first=(c == 0 and jj == 0), last=(c == i and jj == 3)))
            grp += 1
    nld = 0
    cur_q = {}
    cur_c = {}

    def emit_S(n):
        nonlocal nld
        st = steps[n]
        a, i, c, hh, jj = st["a"], st["i"], st["c"], st["hh"], st["jj"]
        g = st["grp"]
        if g not in cur_q:
            qb = g % 2
            for h2 in range(2):
                h_ = 2 * a + h2
                tq = t_Qs[qb][h2]
                sc.dma(SP, Qs[qb][h2][0:64, :], Qd[a, i, h2 * 64:(h2 + 1) * 64, :], reads=[t_Qd], writes=[tq], owner=tq)
                sc.dma(SP, Qs[qb][h2][64:65, :], Ad[i, h_:h_ + 1, :], reads=[t_Ad], writes=[tq], owner=tq)
                sc.dma(SP, Qs[qb][h2][65:66, :], Ad[i, 32 + h_:33 + h_, :], reads=[t_Ad], writes=[tq], owner=tq)
            cur_q[g] = qb
        if (g, c) not in cur_c:
            kb = nld % NKB
            nld += 1
            for h2 in range(2):
                sc.dma(SP, Kc[kb][h2][0:64, :], Kd[a, c, h2 * 64:(h2 + 1) * 64, :], reads=[t_Kd], writes=[t_Kc[kb][h2]],
                       owner=t_Kc[kb][h2])
                sc.dma(SP, Vc[kb][:, :, h2, 0:65], Vd[a, c, :, :, h2 * 65:(h2 + 1) * 65], reads=[t_Vd], writes=[t_Vc[kb]],
                       owner=t_Vc[kb])
            cur_c[(g, c)] = kb
        qb = cur_q[g]; kb = cur_c[(g, c)]
        st["qb"] = qb; st["kb"] = kb
        h = 2 * a + hh
        diag = (c == i)
        q0 = 128 * jj if diag else 0
        st["q0"] = q0; st["h"] = h
        bank = n % NSB
        st["bank"] = bank

        def f():
            ins = E[PE].matmul(pm[bank][:, q0:], lhsT=Kc[kb][hh][:, jj * 128:(jj + 1) * 128], rhs=Qs[qb][hh][:, q0:],
                               start=True, stop=not diag)
            if diag:
                ins = E[PE].matmul(pm[bank][:, q0:q0 + 128], lhsT=identb[:], rhs=negmb[:], start=False, stop=True)
            return ins
        sc.op(PE, f, [t_Kc[kb][hh], t_Qs[qb][hh], t_identb], [t_pm[bank]])

    def emit_exp(n):
        st = steps[n]
        bank = st["bank"]; q0 = st["q0"]; h = st["h"]
        j = 4 * st["c"] + st["jj"]
        pb = n % NPB
        sc.op(A, lambda: E[A].activation(out=Pt[pb][:, q0:], in_=pm[bank][:, q0:], func=AF.Exp, scale=0.125,
                                         bias=biasT[:, j, h:h + 1]), [t_pm[bank], t_biasT], [t_Pt[pb]])

    def emit_PV(n):
        st = steps[n]
        hh = st["hh"]; q0 = st["q0"]; kb = st["kb"]; jj = st["jj"]
        pb = n % NPB
        ob = 5 + hh
        sc.op(PE, lambda: E[PE].matmul(pm[ob][:, q0:], lhsT=Vc[kb][:, jj, hh, :], rhs=Pt[pb][:, q0:],
                                       start=st["first"], stop=st["last"]), [t_Vc[kb], t_Pt[pb]], [t_pm[ob]])
        if st["last"] and hh == 1:
            a, i = st["a"], st["i"]
            yb = st["grp"] % 2
            for h2 in range(2):
                obk = 5 + h2
                sc.op(V, lambda: E[V].tensor_copy(out=Osb[h2][:], in_=pm[obk][0:65, :]), [t_pm[obk]], [t_Osb[h2]])
                sc.op(V, lambda: E[V].reciprocal(out=Osb[h2][64:65, :], in_=Osb[h2][64:65, :]), [t_Osb[h2]], [t_Osb[h2]])
                sc.op(PE, lambda: E[PE].matmul(pm[7][0:64, :], lhsT=e64[:], rhs=Osb[h2][:], start=True, stop=True),
                      [t_e64, t_Osb[h2]], [t_pm[7]])
                sc.op(V, lambda: E[V].tensor_tensor(out=Ya[yb][:, h2, :], in0=Osb[h2][0:64, :], in1=pm[7][0:64, :], op=ALU.mult),
                      [t_Osb[h2], t_pm[7]], [t_Ya[yb]])
            sc.dma(P, Yad[i, :, 2 * a:2 * a + 2, :], Ya[yb][:], reads=[t_Ya[yb]], writes=[t_Yad], owner=t_Ya[yb])

    NS = len(steps)
    LAG = 2
    for n in range(NS + LAG):
        if n < NS:
            emit_S(n)
            emit_exp(n)
        if n >= LAG:
            emit_PV(n - LAG)

    if debug == "p1b":
        sc.wait_all(P, [t_Qd, t_Kd, t_Vd, t_Ad, t_Mcd, t_biasT, t_Yad])
        return nc

    new_phase()
    tp, t_tp, pm, t_pm = mkpsum(2, 6)
    t_Xmd = TR("Xmd"); t_Xmd.multi = True
    t_H2d = TR("H2d"); t_H2d.multi = True
    t_RTd = TR("RTd"); t_RTd.multi = True
    WoC = sb("WoC", [128, 4, D], BF16); t_WoC = TR("WoC")
    WoA = sb("WoA", [128, 4, D], BF16); t_WoA = TR("WoA")
    WrB = sb("WrB", [128, 8, 36], BF16); t_WrB = TR("WrB")
    wst = sb("wst", [128, 8, D]); t_wst = TR("wst")
    wrs = sb("wrs", [128, 8, 36]); t_wrs = TR("wrs")
    g2b = sb("g2b", [128, D]); t_g2b = TR("g2b")
    sc.dma(SP, g2b[:], g2bc, writes=[t_g2b], owner=t_g2b)
    sc.dma(SP, wst[:, 0:4, :], w_out[0:512, :].rearrange("(c p) n -> p c n", p=128), writes=[t_wst], owner=t_wst)
    for c in range(4):
        sc.op(V, lambda: E[V].tensor_scalar(out=WoC[:, c, :], in0=wst[:, c, :], scalar1=gconvs[:, c:c + 1], scalar2=None,
                                            op0=ALU.mult), [t_wst, t_c], [t_WoC])
    sc.dma(SP, wst[:, 4:8, :], w_out[512:1024, :].rearrange("(a p) n -> p a n", p=128), writes=[t_wst], owner=t_wst)
    for a in range(4):
        sc.op(V, lambda: E[V].tensor_scalar(out=WoA[:, a, :], in0=wst[:, 4 + a, :], scalar1=gatts[:, a:a + 1], scalar2=None,
                                            op0=ALU.mult), [t_wst, t_c], [t_WoA])
    sc.dma(SP, wrs[:], wr.rearrange("(kc p) n -> p kc n", p=128), writes=[t_wrs], owner=t_wrs)
    sc.op(V, lambda: E[V].tensor_copy(out=WrB[:], in_=wrs[:]), [t_wrs], [t_WrB])
    NRT = (NE * S + 128) // 128
    rti = sb("rti", [128, NRT], I32); t_rti = TR("rti")
    sc.op(P, lambda: E[P].memset(rti[:], S), [], [t_rti])
    sc.dma(P, RTd.rearrange("(p n) o -> p (n o)", p=128), rti[:], reads=[t_rti], writes=[t_RTd], owner=t_rti)
    dp1 = sb("dp1", [128, NT], I32); dp2 = sb("dp2", [128, NT], I32)
    zrow = sb("zrow", [128, D], BF16); t_zrow = TR("zrow")
    sc.op(P, lambda: E[P].memset(zrow[:], 0.0), [], [t_zrow])
    sc.dma(P, H2d[S:S + 128, :], zrow[:], reads=[t_zrow], writes=[t_H2d], owner=t_zrow)

    mcs = [sb("mcs%d" % i, [128, 4, MT], BF16) for i in range(2)]; t_mcs = [TR("mcs%d" % i) for i in range(2)]
    yas = [sb("yas%d" % i, [128, 4, MT], BF16) for i in range(2)]; t_yas = [TR("yas%d" % i) for i in range(2)]
    for i_ in range(2):
        t_yas[i_].multi = True
    xs2 = [sb("xs2%d" % i, [128, 4, D]) for i in range(2)]; t_xs2 = [TR("xs2%d" % i) for i in range(2)]
    sqa = sb("sqa", [128, 4, MT], BF16); t_sqa = TR("sqa")
    r4 = sb("r4", [128, MT]); t_r4 = TR("r4")
    mixa = sb("mixa", [128, 4, MT], BF16); t_mixa = TR("mixa")
    xm = [sb("xm%d" % i, [128, D]) for i in range(4)]; t_xm = [TR("xm%d" % i) for i in range(4)]
    h2 = [sb("h2%d" % i, [128, D], BF16) for i in range(4)]; t_h2 = [TR("h2%d" % i) for i in range(4)]
    h2T = [sb("h2T%d" % i, [128, 8, 128], BF16) for i in range(4)]; t_h2T = [TR("h2T%d" % i) for i in range(4)]
    junk2 = sb("junk2", [128, D], BF16); t_junk2 = TR("junk2")
    R = []
    for i_ in range(4):
        R.append(dict(
            sm=sb("sm%d" % i_, [128, 32]), lg=sb("lg%d" % i_, [128, 36]), l2=sb("l2_%d" % i_, [128, 8]), l2m=sb("l2m%d" % i_, [128, 8]),
            oh1=sb("oh1_%d" % i_, [128, 8]), oh2=sb("oh2_%d" % i_, [128, 8]), ohg=sb("ohg%d" % i_, [128, 4]), e1=sb("e1_%d" % i_, [128, 4]),
            M1=sb("M1_%d" % i_, [128, 32]), M2=sb("M2_%d" % i_, [128, 32]), Mb=sb("Mb%d" % i_, [128, 32], BF16),
            Rf=sb("Rf%d" % i_, [128, 32]), tmpr=sb("tmpr%d" % i_, [128, 32]), tmpr2=sb("tmpr2_%d" % i_, [128, 32]),
            t=TR("rt%d" % i_), tlg=TR("lgp%d" % i_), tR=TR("Rp%d" % i_), tC=TR("Cp%d" % i_)))
    rank1 = sb("rank1", [128, NT]); rank2 = sb("rank2", [128, NT]); eid1 = sb("eid1", [128, NT]); eid2 = sb("eid2", [128, NT])
    t_rk = TR("rankeid")
    carryR = sb("carryR", [128, 32]); t_carryR = TR("carryR")
    sc.op(V, lambda: E[V].memset(carryR[:], 0.0), [], [t_carryR])
    lg4 = sb("lg4", [128, 4, 36]); mx4 = sb("mx4", [128, 4]); ohg4 = sb("ohg4", [128, 4, 4]); d14 = sb("d14", [128, 4, 4])
    e14 = sb("e14", [128, 4, 4]); se4 = sb("se4", [128, 4]); p1s4 = sb("p1s4", [128, 4]); prod4 = sb("prod4", [128, 4, 4, 8])
    l24 = sb("l24", [128, 4, 8]); m14 = sb("m14", [128, 4]); oh14 = sb("oh14", [128, 4, 8]); l2m4 = sb("l2m4", [128, 4, 8])
    m24 = sb("m24", [128, 4]); oh24 = sb("oh24", [128, 4, 8]); d214 = sb("d214", [128, 4]); ed4 = sb("ed4", [128, 4])
    den4 = sb("den4", [128, 4]); w14 = sb("w14", [128, 4]); w24 = sb("w24", [128, 4])
    M14 = sb("M14", [128, 4, 32]); M24 = sb("M24", [128, 4, 32]); Mb4 = sb("Mb4", [128, 4, 32], BF16)
    Rf4 = sb("Rf4", [128, 4, 32]); tmp4 = sb("tmp4", [128, 4, 32]); dpf4 = sb("dpf4", [128, 4])
    t_rt4 = TR("rt4")

    def vr(fn, reads=(), writes=()):
        sc.op(V, fn, list(reads) + [t_rt4], list(writes) + [t_rt4])

    def bc(ap2d, shape3, axis):
        pst, _ = ap2d.ap[0]
        est, n = ap2d.ap[1]
        if axis == 1:
            return bass.AP(tensor=ap2d.tensor, offset=ap2d.offset, ap=[[pst, 128], [0, shape3[1]], [est, n]])
        return bass.AP(tensor=ap2d.tensor, offset=ap2d.offset, ap=[[pst, 128], [est, n], [0, shape3[2]]])

    def load1c(m):
        b = m % 2
        sc.dma(SP, mcs[b][:], Mcd[m], reads=[t_Mcd], writes=[t_mcs[b]], owner=t_mcs[b])
        for hh in range(2):
            sc.dma(SP, yas[b][hh * 64:(hh + 1) * 64, :, :], Yad[m].rearrange("d (a two) t -> d a two t", two=2)[:, :, hh, :],
                   reads=[t_Yad], writes=[t_yas[b]], owner=t_yas[b])
        sc.dma(SP, xs2[b][:], x_v[m], writes=[t_xs2[b]], owner=t_xs2[b])

    def prolog(m):
        b = m % 2
        sc.op(A, lambda: E[A].activation(out=sqa[:], in_=yas[b][:], func=AF.Square), [t_yas[b]], [t_sqa])

        def f():
            ins = None
            for a in range(4):
                ins = E[PE].matmul(pm[5][:], lhsT=onesb[:], rhs=sqa[:, a, :], start=(a == 0), stop=(a == 3))
            return ins
        sc.op(PE, f, [t_identb, t_sqa], [t_pm[5]])
        sc.op(A, lambda: E[A].activation(out=r4[:], in_=pm[5][:], func=AF.Sqrt, scale=1.0 / 512, bias=epsb[:, 0:1]),
              [t_pm[5], t_eps], [t_r4])
        sc.op(V, lambda: E[V].reciprocal(out=r4[:], in_=r4[:]), [t_r4], [t_r4])
        for a in range(4):
            sc.op(V, lambda: E[V].tensor_tensor(out=mixa[:, a, :], in0=yas[b][:, a, :], in1=r4[:], op=ALU.mult),
                  [t_yas[b], t_r4], [t_mixa])

    load1c(0)
    prolog(0)
    for m in range(NM):
        b = m % 2
        if m + 1 < NM:
            load1c(m + 1)
        for s in range(4):
            tile = m * 4 + s
            rs_ = R[s]; sm = rs_["sm"]
            xb2 = xm[s]; txm = t_xm[s]
            hb2 = h2[s]; th2 = t_h2[s]
            for n in range(2):
                bank = n

                def f():
                    ins = None
                    for c in range(4):
                        ins = E[PE].matmul(pm[bank][:], lhsT=mcs[b][:, c, s * 128:(s + 1) * 128], rhs=WoC[:, c, n * 512:(n + 1) * 512],
                                           start=(c == 0), stop=False)
                    for a in range(4):
                        ins = E[PE].matmul(pm[bank][:], lhsT=mixa[:, a, s * 128:(s + 1) * 128], rhs=WoA[:, a, n * 512:(n + 1) * 512],
                                           start=False, stop=(a == 3))
                    return ins
                sc.op(PE, f, [t_mcs[b], t_mixa, t_WoC, t_WoA], [t_pm[bank]])
                sc.op(V, lambda: E[V].tensor_tensor(out=xb2[:, n * 512:(n + 1) * 512], in0=pm[bank][:],
                                                    in1=xs2[b][:, s, n * 512:(n + 1) * 512], op=ALU.add),
                      [t_pm[bank], t_xs2[b]], [txm])
            sc.dma(SP, Xmd[tile * 128:(tile + 1) * 128, :], xb2[:], reads=[txm], writes=[t_Xmd], owner=txm)
            sc.op(A, lambda: E[A].activation(out=junk2[:], in_=xb2[:], func=AF.Square, accum_out=sm[:, 0:1]),
                  [txm], [t_junk2, rs_["t"]])
            sc.op(A, lambda: E[A].activation(out=sm[:, 1:2], in_=sm[:, 0:1], func=AF.Sqrt, scale=1.0 / D, bias=epsb[:, 0:1]),
                  [rs_["t"], t_eps], [rs_["t"]])
            sc.op(V, lambda: E[V].reciprocal(out=sm[:, 2:3], in_=sm[:, 1:2]), [rs_["t"]], [rs_["t"]])
            sc.op(V, lambda: E[V].scalar_tensor_tensor(out=hb2[:], in0=xb2[:], scalar=sm[:, 2:3], in1=g2b[:], op0=ALU.mult,
                                                       op1=ALU.mult), [txm, rs_["t"], t_g2b], [th2])
            sc.dma(SP, H2d[tile * 128:(tile + 1) * 128, :], hb2[:], reads=[th2], writes=[t_H2d], owner=th2)
        for s in range(4):
            tile = m * 4 + s
            rs_ = R[s]
            hb2 = h2[s]; th2 = t_h2[s]
            tb = tp[tile % 2]; ttb = t_tp[tile % 2]

            def f():
                ins = None
                for kc in range(8):
                    ins = E[PE].transpose(out=tb[:, kc * 128:(kc + 1) * 128], in_=hb2[:, kc * 128:(kc + 1) * 128], identity=identb[:])
                return ins
            sc.op(PE, f, [th2, t_identb], [ttb])
            sc.op(A, lambda: E[A].copy(out=h2T[s][:], in_=tb[:].rearrange("p (k t) -> p k t", k=8)), [ttb], [t_h2T[s]])

            def f():
                ins = None
                for kc in range(8):
                    ins = E[PE].matmul(pm[2][:, s * 64:s * 64 + 36], lhsT=h2T[s][:, kc, :], rhs=WrB[:, kc, :], start=(kc == 0),
                                       stop=(kc == 7))
                return ins
            sc.op(PE, f, [t_h2T[s], t_WrB], [rs_["tlg"]])

        if m + 1 < NM:
            prolog(m + 1)
        c0 = 4 * m
        lgp = pm[2][:, 0:256].rearrange("p (s c) -> p s c", c=64)[:, :, 0:36]
        vr(lambda: E[V].tensor_tensor(out=lg4[:], in0=lgp, in1=bc(rbs[:], [128, 4, 36], 1), op=ALU.add),
           [R[0]["tlg"], R[1]["tlg"], R[2]["tlg"], R[3]["tlg"], t_c])
        vr(lambda: E[V].reduce_max(out=mx4[:], in_=lg4[:, :, 0:4], axis=AX.X))
        vr(lambda: E[V].tensor_tensor(out=ohg4[:], in0=lg4[:, :, 0:4], in1=bc(mx4[:], [128, 4, 4], 2), op=ALU.is_equal))
        vr(lambda: E[V].tensor_tensor(out=d14[:], in0=lg4[:, :, 0:4], in1=bc(mx4[:], [128, 4, 4], 2), op=ALU.subtract))
        sc.op(A, lambda: E[A].activation(out=e14[:], in_=d14[:], func=AF.Exp), [t_rt4], [t_rt4])
        vr(lambda: E[V].reduce_sum(out=se4[:], in_=e14[:], axis=AX.X))
        vr(lambda: E[V].reciprocal(out=p1s4[:], in_=se4[:]))
        vr(lambda: E[V].tensor_tensor(out=prod4[:], in0=lg4[:, :, 4:36].rearrange("p s (g e) -> p s g e", e=8),
                                      in1=bass.AP(tensor=ohg4[:].tensor, offset=ohg4[:].offset,
                                                  ap=[[16, 128], [4, 4], [1, 4], [0, 8]]), op=ALU.mult))
        vr(lambda: E[V].reduce_sum(out=l24[:], in_=prod4[:].rearrange("p s g e -> p s e g"), axis=AX.X))
        vr(lambda: E[V].reduce_max(out=m14[:], in_=l24[:], axis=AX.X))
        vr(lambda: E[V].tensor_tensor(out=oh14[:], in0=l24[:], in1=bc(m14[:], [128, 4, 8], 2), op=ALU.is_equal))
        vr(lambda: E[V].scalar_tensor_tensor(out=l2m4[:], in0=oh14[:], scalar=-1e30, in1=l24[:], op0=ALU.mult, op1=ALU.add))
        vr(lambda: E[V].reduce_max(out=m24[:], in_=l2m4[:], axis=AX.X))
        vr(lambda: E[V].tensor_tensor(out=oh24[:], in0=l2m4[:], in1=bc(m24[:], [128, 4, 8], 2), op=ALU.is_equal))
        vr(lambda: E[V].tensor_tensor(out=d214[:], in0=m24[:], in1=m14[:], op=ALU.subtract))
        sc.op(A, lambda: E[A].activation(out=ed4[:], in_=d214[:], func=AF.Exp), [t_rt4], [t_rt4])
        vr(lambda: E[V].tensor_scalar(out=den4[:], in0=ed4[:], scalar1=1.0, scalar2=None, op0=ALU.add))
        vr(lambda: E[V].reciprocal(out=w14[:], in_=den4[:]))
        vr(lambda: E[V].tensor_tensor(out=wgt1[:, c0:c0 + 4], in0=w14[:], in1=p1s4[:], op=ALU.mult), [], [t_route])
        vr(lambda: E[V].tensor_tensor(out=w24[:], in0=ed4[:], in1=w14[:], op=ALU.mult))
        vr(lambda: E[V].tensor_tensor(out=wgt2[:, c0:c0 + 4], in0=w24[:], in1=p1s4[:], op=ALU.mult), [], [t_route])
        ohg_b = bass.AP(tensor=ohg4[:].tensor, offset=ohg4[:].offset, ap=[[16, 128], [4, 4], [1, 4], [0, 8]])
        oh1_b = bass.AP(tensor=oh14[:].tensor, offset=oh14[:].offset, ap=[[32, 128], [8, 4], [0, 4], [1, 8]])
        oh2_b = bass.AP(tensor=oh24[:].tensor, offset=oh24[:].offset, ap=[[32, 128], [8, 4], [0, 4], [1, 8]])
        vr(lambda: E[V].tensor_tensor(out=M14[:].rearrange("p s (g e) -> p s g e", e=8), in0=ohg_b, in1=oh1_b, op=ALU.mult))
        vr(lambda: E[V].tensor_tensor(out=M24[:].rearrange("p s (g e) -> p s g e", e=8), in0=ohg_b, in1=oh2_b, op=ALU.mult))
        vr(lambda: E[V].tensor_tensor(out=Mb4[:], in0=M14[:], in1=M24[:], op=ALU.add))

        def fR():
            ins = None
            for s_ in range(4):
                E[PE].matmul(pm[3][:, s_ * 32:(s_ + 1) * 32], lhsT=lstrb[:], rhs=Mb4[:, s_, :], start=True, stop=True)
                ins = E[PE].matmul(pm[4][:, s_ * 32:(s_ + 1) * 32], lhsT=onesb[:], rhs=Mb4[:, s_, :], start=True, stop=True)
            return ins
        sc.op(PE, fR, [t_identb, t_rt4], [t_pm[3], t_pm[4]])
        for s_ in range(4):
            vr(lambda: E[V].tensor_tensor(out=Rf4[:, s_, :], in0=pm[3][:, s_ * 32:(s_ + 1) * 32], in1=carryR[:], op=ALU.add),
               [t_pm[3], t_carryR])
            sc.op(V, lambda: E[V].tensor_tensor(out=carryR[:], in0=carryR[:], in1=pm[4][:, s_ * 32:(s_ + 1) * 32], op=ALU.add),
                  [t_pm[4], t_carryR], [t_carryR])
        for (Mx, rk, ei) in ((M14, rank1, eid1), (M24, rank2, eid2)):
            vr(lambda: E[V].tensor_tensor(out=tmp4[:], in0=Rf4[:], in1=Mx[:], op=ALU.mult))
            vr(lambda: E[V].reduce_sum(out=rk[:, c0:c0 + 4], in_=tmp4[:], axis=AX.X), [], [t_rk])
            vr(lambda: E[V].tensor_tensor(out=tmp4[:], in0=bc(ebs[:], [128, 4, 32], 1), in1=Mx[:], op=ALU.mult), [t_c])
            vr(lambda: E[V].reduce_sum(out=ei[:, c0:c0 + 4], in_=tmp4[:], axis=AX.X), [], [t_rk])
        for (ei, rk, dpp) in ((eid1, rank1, dp1), (eid2, rank2, dp2)):
            vr(lambda: E[V].scalar_tensor_tensor(out=dpf4[:], in0=ei[:, c0:c0 + 4], scalar=float(S), in1=rk[:, c0:c0 + 4],
                                                 op0=ALU.mult, op1=ALU.add), [t_rk])
            vr(lambda: E[V].tensor_copy(out=dpp[:, c0:c0 + 4], in_=dpf4[:]))
            for s_ in range(4):
                tile = c0 + s_
                sc.dma(P, None, None, reads=[t_rt4, t_c], writes=[t_RTd], owner=t_rt4,
                       fn=lambda: E[P].indirect_dma_start(out=RTd[:, :], out_offset=bass.IndirectOffsetOnAxis(ap=dpp[:, tile:tile + 1], axis=0),
                                                          in_=iotas[:, tile:tile + 1], in_offset=None,
                                                          bounds_check=rb_rt, oob_is_err=False))

    def bc(ap2d, shape3, axis):
        pst, _ = ap2d.ap[0]
        est, n = ap2d.ap[1]
        if axis == 1:
            return bass.AP(tensor=ap2d.tensor, offset=ap2d.offset, ap=[[pst, 128], [0, shape3[1]], [est, n]])
        return bass.AP(tensor=ap2d.tensor, offset=ap2d.offset, ap=[[pst, 128], [est, n], [0, shape3[2]]])
    cmpk = sb("cmpk", [128, 32, 33]); ntl = sb("ntl", [128, 32]); padd = sb("padd", [128, 32]); pend = sb("pend", [128, 32])
    pstt = sb("pstt", [128, 32]); bigt = sb("bigt", [128, NT, 32]); psel = sb("psel", [128, NT]); destf = sb("destf", [128, NT])
    cmpt = sb("cmpt", [128, NTL, 32]); tef = sb("tef", [128, NTL]); widf = sb("widf", [128, NTL])
    t_fin = TR("fin")

    def fop(fn, reads=(), writes=()):
        sc.op(V, fn, list(reads) + [t_fin], list(writes) + [t_fin])
    fop(lambda: E[V].tensor_tensor(out=cmpk[:], in0=bc(carryR[:], [128, 32, 33], 2), in1=bc(thrs[:], [128, 32, 33], 1), op=ALU.is_gt),
        [t_carryR, t_c])
    fop(lambda: E[V].reduce_sum(out=ntl[:], in_=cmpk[:], axis=AX.X))
    fop(lambda: E[V].tensor_scalar(out=padd[:], in0=ntl[:], scalar1=float(TS), scalar2=None, op0=ALU.mult))
    fop(lambda: E[V].tensor_tensor_scan(out=pend[:], data0=ones512[:, 0:32], data1=padd[:], initial=0.0, op0=ALU.mult, op1=ALU.add),
        [t_ones512])
    fop(lambda: E[V].tensor_tensor(out=pstt[:], in0=pend[:], in1=padd[:], op=ALU.subtract))
    for (rk, ei, dd) in ((rank1, eid1, dest1), (rank2, eid2, dest2)):
        fop(lambda: E[V].tensor_tensor(out=bigt[:], in0=bc(ei[:], [128, NT, 32], 2), in1=bc(ebs[:], [128, NT, 32], 1), op=ALU.is_equal),
            [t_rk, t_c])
        fop(lambda: E[V].tensor_tensor(out=bigt[:], in0=bigt[:], in1=bc(pstt[:], [128, NT, 32], 1), op=ALU.mult))
        fop(lambda: E[V].reduce_sum(out=psel[:], in_=bigt[:], axis=AX.X))
        fop(lambda: E[V].tensor_tensor(out=destf[:], in0=psel[:], in1=rk[:], op=ALU.add), [t_rk])
        fop(lambda: E[V].tensor_copy(out=dd[:], in_=destf[:]), [], [t_route])
    fop(lambda: E[V].tensor_tensor(out=cmpt[:], in0=bc(pend[:], [128, NTL, 32], 1), in1=bc(tsts[:], [128, NTL, 32], 2), op=ALU.is_le),
        [t_c])
    fop(lambda: E[V].reduce_sum(out=tef[:], in_=cmpt[:], axis=AX.X))
    fop(lambda: E[V].tensor_scalar(out=widf[:], in0=tef[:], scalar1=128.0, scalar2=pcols[:, 0:1], op0=ALU.mult, op1=ALU.add), [t_c])
    eqf = sb("eqf", [128, NTL])
    fop(lambda: E[V].memset(eqf[:], 0.0))
    fop(lambda: E[V].tensor_tensor(out=eqf[:, 2:NTL], in0=tef[:, 2:NTL], in1=tef[:, 0:NTL - 2], op=ALU.is_equal))
    fop(lambda: E[V].scalar_tensor_tensor(out=widf[:], in0=eqf[:], scalar=float(NE * 128), in1=widf[:], op0=ALU.mult, op1=ALU.add))
    fop(lambda: E[V].tensor_copy(out=widx[:], in_=widf[:]), [], [t_widx])
    pstl = sb("pstl", [128, NTL]); basef = sb("basef", [128, NTL]); rif = sb("rif", [128, NTL])
    fop(lambda: E[V].tensor_tensor(out=cmpt[:], in0=bc(tef[:], [128, NTL, 32], 2), in1=bc(ebs[:], [128, NTL, 32], 1), op=ALU.is_equal),
        [t_c])
    fop(lambda: E[V].tensor_tensor(out=cmpt[:], in0=cmpt[:], in1=bc(pstt[:], [128, NTL, 32], 1), op=ALU.mult))
    fop(lambda: E[V].reduce_sum(out=pstl[:], in_=cmpt[:], axis=AX.X))
    fop(lambda: E[V].scalar_tensor_tensor(out=basef[:], in0=tef[:], scalar=float(S), in1=tsts[:], op0=ALU.mult, op1=ALU.add), [t_c])
    fop(lambda: E[V].tensor_tensor(out=basef[:], in0=basef[:], in1=pstl[:], op=ALU.subtract))
    for st in range(TS // 128):
        fop(lambda: E[V].tensor_scalar(out=rif[:], in0=basef[:], scalar1=pcols[:, 0:1], scalar2=float(128 * st), op0=ALU.add, op1=ALU.add),
            [t_c])
        fop(lambda: E[V].tensor_copy(out=ridx[:, :, st], in_=rif[:]), [], [t_widx])

    new_phase()
    tp, t_tp, pm, t_pm = mkpsum(2, 6)
    t_Ysd = TR("Ysd"); t_Ysd.multi = True
    NSUB = TS // 128
    Wg = [sb("Wg%d" % i, [128, 8, DE], BF16) for i in range(2)]; t_Wg = [TR("Wg%d" % i) for i in range(2)]
    Wu = [sb("Wu%d" % i, [128, 8, DE], BF16) for i in range(2)]; t_Wu = [TR("Wu%d" % i) for i in range(2)]
    Wd = [sb("Wd%d" % i, [128, 2, D], BF16) for i in range(2)]; t_Wd = [TR("Wd%d" % i) for i in range(2)]
    Wgs = [sb("Wgs%d" % i, [128, 8 * DE]) for i in range(2)]; t_Wgs = [TR("Wgs%d" % i) for i in range(2)]
    Wus = [sb("Wus%d" % i, [128, 8 * DE]) for i in range(2)]; t_Wus = [TR("Wus%d" % i) for i in range(2)]
    Wds = [sb("Wds%d" % i, [128, 2 * D]) for i in range(2)]; t_Wds = [TR("Wds%d" % i) for i in range(2)]
    for i_ in range(2):
        sc.op(V, lambda: E[V].memset(Wgs[i_][:], 0.0), [], [t_Wgs[i_]])
        sc.op(V, lambda: E[V].memset(Wus[i_][:], 0.0), [], [t_Wus[i_]])
        sc.op(V, lambda: E[V].memset(Wds[i_][:], 0.0), [], [t_Wds[i_]])
    idxs = [[sb("idx%d_%d" % (i, j), [128, 1], I32) for j in range(NSUB)] for i in range(2)]
    t_idx = [[TR("idx%d_%d" % (i, j)) for j in range(NSUB)] for i in range(2)]
    Xg = [[sb("Xg%d_%d" % (i, j), [128, D], BF16) for j in range(NSUB)] for i in range(2)]
    t_Xg = [[TR("Xg%d_%d" % (i, j)) for j in range(NSUB)] for i in range(2)]
    XgT = [sb("XgT%d" % i, [128, 8, TS], BF16) for i in range(2)]; t_XgT = [TR("XgT%d" % i) for i in range(2)]
    sgl = [sb("sgl%d" % i, [128, TS]) for i in range(2)]; t_sgl = [TR("sgl%d" % i) for i in range(2)]
    AT = [sb("AT%d" % i, [128, 2, TS], BF16) for i in range(2)]; t_AT = [TR("AT%d" % i) for i in range(2)]
    NYB = 4
    Yt = [sb("Yt%d" % i, [128, D], BF16) for i in range(NYB)]; t_Yt = [TR("Yt%d" % i) for i in range(NYB)]

    def L2(t):
        b = t % 2
        for st in range(NSUB):
            sc.dma(P, None, None, reads=[t_widx, t_RTd], writes=[t_idx[b][st]], owner=t_idx[b][st],
                   fn=lambda: E[P].indirect_dma_start(out=idxs[b][st][:, :], out_offset=None, in_=RTd[:, :],
                                                      in_offset=bass.IndirectOffsetOnAxis(ap=ridx[:, t, st:st + 1], axis=0),
                                                      bounds_check=rb_rt, oob_is_err=False))
        for (wt, twt, src) in ((Wgs[b], t_Wgs[b], w_gate), (Wus[b], t_Wus[b], w_up), (Wds[b], t_Wds[b], w_down)):
            sc.dma(P, None, None, reads=[t_widx], writes=[twt], owner=twt,
                   fn=lambda: E[P].indirect_dma_start(out=wt[:, :], out_offset=None, in_=src[:, :],
                                                      in_offset=bass.IndirectOffsetOnAxis(ap=widx[:, t:t + 1], axis=0),
                                                      bounds_check=rb_w, oob_is_err=False))
        for st in range(NSUB):
            sc.dma(P, None, None, reads=[t_idx[b][st], t_H2d], writes=[t_Xg[b][st]], owner=t_Xg[b][st],
                   fn=lambda: E[P].indirect_dma_start(out=Xg[b][st][:, :], out_offset=None, in_=H2d[:, :],
                                                      in_offset=bass.IndirectOffsetOnAxis(ap=idxs[b][st][:, 0:1], axis=0),
                                                      bounds_check=rb_h2, oob_is_err=False))

    def C2(t):
        b = t % 2
        sc.op(V, lambda: E[V].tensor_copy(out=Wg[b][:].rearrange("p k n -> p (k n)"), in_=Wgs[b][:]), [t_Wgs[b]], [t_Wg[b]])
        sc.op(A, lambda: E[A].copy(out=Wu[b][:].rearrange("p k n -> p (k n)"), in_=Wus[b][:]), [t_Wus[b]], [t_Wu[b]])
        sc.op(V, lambda: E[V].tensor_copy(out=Wd[b][:].rearrange("p k n -> p (k n)"), in_=Wds[b][:]), [t_Wds[b]], [t_Wd[b]])

    def T2(t):
        b = t % 2
        for st in range(NSUB):
            tb = tp[st % 2]; ttb = t_tp[st % 2]

            def f():
                ins = None
                for kc in range(8):
                    ins = E[PE].transpose(out=tb[:, kc * 128:(kc + 1) * 128], in_=Xg[b][st][:, kc * 128:(kc + 1) * 128],
                                          identity=identb[:])
                return ins
            sc.op(PE, f, [t_Xg[b][st], t_identb], [ttb])
            if st % 2 == 0:
                sc.op(A, lambda: E[A].copy(out=XgT[b][:, :, st * 128:(st + 1) * 128], in_=tb[:].rearrange("p (k t) -> p k t", k=8)),
                      [ttb], [t_XgT[b]])
            else:
                sc.op(V, lambda: E[V].tensor_copy(out=XgT[b][:, :, st * 128:(st + 1) * 128],
                                                  in_=tb[:].rearrange("p (k t) -> p k t", k=8)), [ttb], [t_XgT[b]])

    def M2a(t):
        b = t % 2
        N = TS
        for fc in range(2):
            bg = 2 * fc; bu = 2 * fc + 1

            def fg():
                ins = None
                for kc in range(8):
                    ins = E[PE].matmul(pm[bg][:, 0:N], lhsT=Wg[b][:, kc, fc * 128:(fc + 1) * 128], rhs=XgT[b][:, kc, 0:N],
                                       start=(kc == 0), stop=(kc == 7))
                return ins

            def fu():
                ins = None
                for kc in range(8):
                    ins = E[PE].matmul(pm[bu][:, 0:N], lhsT=Wu[b][:, kc, fc * 128:(fc + 1) * 128], rhs=XgT[b][:, kc, 0:N],
                                       start=(kc == 0), stop=(kc == 7))
                return ins
            sc.op(PE, fg, [t_Wg[b], t_XgT[b]], [t_pm[bg]])
            sc.op(PE, fu, [t_Wu[b], t_XgT[b]], [t_pm[bu]])
            sc.op(A, lambda: E[A].activation(out=sgl[fc][:, 0:N], in_=pm[bg][:, 0:N], func=AF.Silu), [t_pm[bg]], [t_sgl[fc]])
            sc.op(V, lambda: E[V].tensor_tensor(out=AT[b][:, fc, 0:N], in0=pm[bu][:, 0:N], in1=sgl[fc][:, 0:N], op=ALU.mult),
                  [t_pm[bu], t_sgl[fc]], [t_AT[b]])

    ny = [0]

    def M2b(t):
        b = t % 2
        for st in range(NSUB):
            yb = ny[0] % NYB; ny[0] += 1
            for n in range(2):
                bank = 4 + n

                def fy():
                    ins = None
                    for fc in range(2):
                        ins = E[PE].matmul(pm[bank][:], lhsT=AT[b][:, fc, st * 128:(st + 1) * 128],
                                           rhs=Wd[b][:, fc, n * 512:(n + 1) * 512], start=(fc == 0), stop=(fc == 1))
                    return ins
                sc.op(PE, fy, [t_AT[b], t_Wd[b]], [t_pm[bank]])
                if n == 0:
                    sc.op(A, lambda: E[A].copy(out=Yt[yb][:, 0:512], in_=pm[bank][:]), [t_pm[bank]], [t_Yt[yb]])
                else:
                    sc.op(V, lambda: E[V].tensor_copy(out=Yt[yb][:, 512:1024], in_=pm[bank][:]), [t_pm[bank]], [t_Yt[yb]])
            r0 = t * TS + st * 128
            sc.dma(SP, Ysd[r0:r0 + 128, :], Yt[yb][:], reads=[t_Yt[yb]], writes=[t_Ysd], owner=t_Yt[yb])

    L2(0); C2(0); T2(0)
    for t in range(NTL):
        if t + 1 < NTL:
            L2(t + 1)
        M2a(t)
        if t + 1 < NTL:
            C2(t + 1)
        M2b(t)
        if t + 1 < NTL:
            T2(t + 1)

    new_phase()
    t_out = TR("out"); t_out.multi = True
    fgs = sb("fgs", [128, D]); t_fgs = TR("fgs")
    sc.dma(SP, fgs[:], fing, writes=[t_fgs], owner=t_fgs)
    NB3 = 4
    xm3 = [sb("xm3%d" % i, [128, D]) for i in range(NB3)]; t_xm3 = [TR("xm3%d" % i) for i in range(NB3)]
    y1 = [sb("y1%d" % i, [128, D], BF16) for i in range(NB3)]; t_y1 = [TR("y1%d" % i) for i in range(NB3)]
    y2 = [sb("y2%d" % i, [128, D], BF16) for i in range(NB3)]; t_y2 = [TR("y2%d" % i) for i in range(NB3)]
    ot = [sb("ot%d" % i, [128, D]) for i in range(2)]; t_ot = [TR("ot%d" % i) for i in range(2)]
    junk3 = sb("junk3", [128, D], BF16); t_junk3 = TR("junk3")
    sm3 = [sb("sm3_%d" % i, [128, 4]) for i in range(2)]; t_sm3 = [TR("sm3_%d" % i) for i in range(2)]

    def L3(t):
        b = t % NB3
        sc.dma(SP, xm3[b][:], Xmd[t * 128:(t + 1) * 128, :], reads=[t_Xmd], writes=[t_xm3[b]], owner=t_xm3[b])
        for (yy, tyy, dd) in ((y1[b], t_y1[b], dest1), (y2[b], t_y2[b], dest2)):
            sc.dma(P, None, None, reads=[t_route, t_Ysd], writes=[tyy], owner=tyy,
                   fn=lambda: E[P].indirect_dma_start(out=yy[:, :], out_offset=None, in_=Ysd[:, :],
                                                      in_offset=bass.IndirectOffsetOnAxis(ap=dd[:, t:t + 1], axis=0),
                                                      bounds_check=rb_ys, oob_is_err=False))

    def C3(t):
        b = t % NB3
        ob = t % 2
        s3 = sm3[ob]; ts3 = t_sm3[ob]
        sc.op(V, lambda: E[V].scalar_tensor_tensor(out=xm3[b][:], in0=y1[b][:], scalar=wgt1[:, t:t + 1], in1=xm3[b][:],
                                                   op0=ALU.mult, op1=ALU.add), [t_y1[b], t_xm3[b], t_route], [t_xm3[b]])
        sc.op(V, lambda: E[V].scalar_tensor_tensor(out=xm3[b][:], in0=y2[b][:], scalar=wgt2[:, t:t + 1], in1=xm3[b][:],
                                                   op0=ALU.mult, op1=ALU.add), [t_y2[b], t_xm3[b], t_route], [t_xm3[b]])
        sc.op(A, lambda: E[A].activation(out=junk3[:], in_=xm3[b][:], func=AF.Square, accum_out=s3[:, 0:1]),
              [t_xm3[b]], [t_junk3, ts3])
        sc.op(A, lambda: E[A].activation(out=s3[:, 1:2], in_=s3[:, 0:1], func=AF.Sqrt, scale=1.0 / D, bias=epsb[:, 0:1]),
              [ts3, t_eps], [ts3])
        sc.op(V, lambda: E[V].reciprocal(out=s3[:, 2:3], in_=s3[:, 1:2]), [ts3], [ts3])
        sc.op(V, lambda: E[V].scalar_tensor_tensor(out=ot[ob][:], in0=xm3[b][:], scalar=s3[:, 2:3], in1=fgs[:],
                                                   op0=ALU.mult, op1=ALU.mult), [t_xm3[b], ts3, t_fgs], [t_ot[ob]])
        sc.dma(SP, out[t * 128:(t + 1) * 128, :], ot[ob][:], reads=[t_ot[ob]], writes=[t_out], owner=t_ot[ob])

    for t in range(min(2, NT)):
        L3(t)
    for t in range(NT):
        if t + 2 < NT:
            L3(t + 2)
        C3(t)
    sc.wait_all(P, [t_out])
    barrier()
    return nc


def _prep_inputs(inp, b, S):
    f = np.float32

    def T8(v):
        return np.ascontiguousarray(v.reshape(8, 128).T).astype(f)

    def T4(v):
        return np.ascontiguousarray(v.reshape(4, 128).T).astype(f)
    d = {}
    d["x"] = np.ascontiguousarray(inp["x"][b][:S])
    d["w_in"] = inp["w_in"][0]
    d["w_out"] = inp["w_out"][0]
    d["wr"] = np.ascontiguousarray(np.concatenate([inp["w_r1"][0]] + [inp["w_r2"][0][g] for g in range(4)], axis=1))
    d["w_gate"] = np.ascontiguousarray(inp["w_gate"][0].reshape(32, 8, 128, 256).transpose(0, 2, 1, 3).reshape(32 * 128, 2048))
    d["w_up"] = np.ascontiguousarray(inp["w_up"][0].reshape(32, 8, 128, 256).transpose(0, 2, 1, 3).reshape(32 * 128, 2048))
    d["w_down"] = np.ascontiguousarray(inp["w_down"][0].reshape(32, 2, 128, 1024).transpose(0, 2, 1, 3).reshape(32 * 128, 2048))
    d["g1T"] = T8(inp["norm1_g"][0])
    d["g2T"] = T8(inp["norm2_g"][0])
    d["gconvT"] = T4(inp["out_g_conv"][0])
    d["gattT"] = T4(inp["out_g_att"][0])
    d["bdwT"] = T4(inp["b_dw"][0])
    d["lngT"] = T4(inp["conv_ln_g"][0])
    d["lnbT"] = T4(inp["conv_ln_b"][0])
    wd = inp["w_dw"][0]
    d["wdwT"] = np.ascontiguousarray(wd.reshape(31, 4, 128).transpose(2, 1, 0).reshape(128, 124))
    nb = np.zeros((128, 1), f)
    for q in range(4):
        nb[q * 32:q * 32 + 8, 0] = -inp["b_f"][0]
    d["negbf"] = nb
    d["fing"] = np.ascontiguousarray(np.broadcast_to(inp["final_g"][None, :], (128, 1024))).astype(f)
    rb = np.concatenate([inp["b_r1"][0], inp["b_r2"][0].reshape(-1)])
    d["g2bc"] = np.ascontiguousarray(np.broadcast_to(inp["norm2_g"][0][None, :], (128, 1024))).astype(f)
    d["rbias"] = np.ascontiguousarray(np.broadcast_to(rb[None, :], (128, 36))).astype(f)
    c = np.zeros((128, 640), f)
    c[:, 0:128] = np.eye(128)
    c[:, 128:256] = 1.0
    c[:, 256:384] = np.triu(np.ones((128, 128)), 1)
    c[:, 384:512] = np.tril(np.ones((128, 128)), -1) * -30000.0
    c[:, 512:640] = 1.0
    d["consts"] = c
    d["ebase"] = np.ascontiguousarray(np.broadcast_to(np.arange(32)[None, :], (128, 32))).astype(f)
    d["thr256"] = np.ascontiguousarray(np.broadcast_to((np.arange(33) * TS)[None, :], (128, 33))).astype(f)
    NTL = NE * CAP // TS
    d["tst256"] = np.ascontiguousarray(np.broadcast_to((np.arange(NTL) * TS)[None, :], (128, NTL))).astype(f)
    d["pcol"] = np.arange(128).reshape(128, 1).astype(f)
    NT = S // 128
    d["iota_tok"] = (np.arange(NT)[None, :] * 128 + np.arange(128)[:, None]).astype(np.int32)
    return d


def kernel(**inputs):
    inp = {k: np.asarray(v) for k, v in inputs.items()}
    B, S, _ = inp["x"].shape
    nc = build(S)
    in_maps = [_prep_inputs(inp, b, S) for b in range(B)]
    res = run_bass_kernel_spmd(nc, in_maps, core_ids=list(range(B)))
    return np.stack([np.asarray(r["out"]) for r in res.results], axis=0).astype(np.float32)
```

```python
import numpy as np
import ml_dtypes
import concourse.bass as bass
import concourse.mybir as mybir
from concourse.bass_utils import run_bass_kernel_spmd

F32 = mybir.dt.float32
BF16 = mybir.dt.bfloat16
I32 = mybir.dt.int32
AF = mybir.ActivationFunctionType
ALU = mybir.AluOpType
AX = mybir.AxisListType

D = 1024
DIN = 2568
NH = 8
HD = 64
CW = 31
NE = 32
DE = 256
CAP = 768
TS = 256
EPS = 1e-6
MT = 512


class TR:
    __slots__ = ("name", "w", "r", "dsem", "dcnt", "multi")

    all = []

    def __init__(self, name):
        TR.all.append(self)
        self.name = name
        self.w = {}
        self.r = {}
        self.dsem = {}
        self.dcnt = {}
        self.multi = False


class Sched:
    def __init__(self, nc):
        self.nc = nc
        self.eng = {"pe": nc.tensor, "act": nc.scalar, "dve": nc.vector, "pool": nc.gpsimd, "sp": nc.sync}
        self.sem = {k: nc.alloc_semaphore("sem_" + k) for k in self.eng}
        self.cnt = {k: 0 for k in self.eng}
        self.waited = {k: {} for k in self.eng}
        self.nsem = 0
        self.pool = {"sw": [], "hw": []}

    def _deps(self, e, reads, writes):
        deps = {}
        for t in list(reads) + list(writes):
            for k, (s, v) in t.w.items():
                if deps.get(k, (None, 0))[1] < v:
                    deps[k] = (s, v)
        for t in writes:
            for k, (s, v) in t.r.items():
                if deps.get(k, (None, 0))[1] < v:
                    deps[k] = (s, v)
        wd = self.waited[e]
        own = id(self.sem[e])
        for k, (s, v) in deps.items():
            if e == "pe" and k == own:
                continue
            if wd.get(k, 0) < v:
                self.eng[e].wait_ge(s, v)
                wd[k] = v

    def op(self, e, fn, reads=(), writes=()):
        self._deps(e, reads, writes)
        ins = fn()
        s = self.sem[e]
        ins.then_inc(s, 1)
        self.cnt[e] += 1
        v = self.cnt[e]
        k = id(s)
        for t in writes:
            t.w = {k: (s, v)}
            t.r = {}
        for t in reads:
            t.r[k] = (s, v)

    def dma(self, q, out, in_, reads=(), writes=(), owner=None, n=1, fn=None):
        self._deps(q, reads, writes)
        kind = "sw" if q == "pool" else "hw"
        if kind not in owner.dsem:
            if self.pool[kind]:
                owner.dsem[kind], owner.dcnt[kind] = self.pool[kind].pop()
            else:
                owner.dsem[kind] = self.nc.alloc_semaphore("d%s_%d" % (kind, self.nsem))
                owner.dcnt[kind] = 0
                self.nsem += 1
        if fn is None:
            ins = self.eng[q].dma_start(out=out, in_=in_)
        else:
            ins = fn()
        sem = owner.dsem[kind]
        ins.then_inc(sem, 16)
        owner.dcnt[kind] += 16
        k = id(sem)
        ent = (sem, owner.dcnt[kind])
        for t in writes:
            if not t.multi:
                t.w = {}
            t.w[k] = ent
            t.r = {}
        for t in reads:
            t.r[k] = ent

    def wait_all(self, e, tracked):
        self._deps(e, tracked, tracked)


def build(S, debug=None):
    NM = S // MT
    NT = S // 128
    NSLOT = NE * CAP
    nc = bass.Bass("TRN2", target_bir_lowering=False)
    TR.all = []
    sc = Sched(nc)

    def din(name, shape, dt=F32):
        return nc.dram_tensor(name, list(shape), dt, kind="ExternalInput").ap()

    def dscr(name, shape, dt):
        return nc.dram_tensor(name, list(shape), dt, kind=("ExternalOutput" if debug else "Internal")).ap()

    x = din("x", [S, D])
    w_in = din("w_in", [D, DIN])
    w_out = din("w_out", [D, D])
    wr = din("wr", [D, 36])
    w_gate = din("w_gate", [NE * 128, 8 * DE])
    w_up = din("w_up", [NE * 128, 8 * DE])
    w_down = din("w_down", [NE * 128, 2 * D])
    g1T = din("g1T", [128, 8])
    g2T = din("g2T", [128, 8])
    gconvT = din("gconvT", [128, 4])
    gattT = din("gattT", [128, 4])
    bdwT = din("bdwT", [128, 4])
    lngT = din("lngT", [128, 4])
    lnbT = din("lnbT", [128, 4])
    wdwT = din("wdwT", [128, 4 * CW])
    negbf = din("negbf", [128, 1])
    fing = din("fing", [128, D])
    g2bc = din("g2bc", [128, D])
    rbias = din("rbias", [128, 36])
    consts = din("consts", [128, 5 * 128])
    ebase = din("ebase", [128, NE])
    thr256 = din("thr256", [128, 33])
    tst256 = din("tst256", [128, NSLOT // TS])
    pcol = din("pcol", [128, 1])
    iota_tok = din("iota_tok", [128, NT], I32)
    out = nc.dram_tensor("out", [S, D], F32, kind="ExternalOutput").ap()

    Qd = dscr("Qd", [4, NM, 128, MT], BF16)
    Kd = dscr("Kd", [4, NM, 128, MT], BF16)
    Vd = dscr("Vd", [4, NM, 128, 4, 130], BF16)
    Ad = dscr("Ad", [NM, 128, MT], BF16)
    Mcd = dscr("Mcd", [NM, 128, 4, MT], BF16)
    Yad = dscr("Yad", [NM, 64, 8, MT], BF16)
    Xmd = dscr("Xmd", [S, D], F32)
    H2d = dscr("H2d", [S + 128, D], BF16)
    RTd = dscr("RTd", [NE * S + 128, 1], I32)
    Ysd = dscr("Ysd", [NSLOT, D], BF16)


    from contextlib import ExitStack
    stk = [ExitStack()]

    def sb(name, shape, dt=F32):
        return stk[0].enter_context(nc.sbuf_tensor(name, list(shape), dt))

    dma_owners = []

    def barrier():
        ents = [(sc.sem[k], sc.cnt[k]) for k in sc.eng if sc.cnt[k] > 0]
        for t in TR.all:
            for kd in t.dsem:
                if t.dcnt[kd] > 0:
                    ents.append((t.dsem[kd], t.dcnt[kd]))
        for e in sc.eng:
            wd = sc.waited[e]
            for (sm, v) in ents:
                if sm is sc.sem[e]:
                    continue
                if wd.get(id(sm), 0) < v:
                    sc.eng[e].wait_ge(sm, v)
                    wd[id(sm)] = v

    def new_phase():
        barrier()
        for t in TR.all:
            for kd in list(t.dsem):
                sc.pool[kd].append((t.dsem[kd], t.dcnt[kd]))
            t.dsem = {}
            t.dcnt = {}
            t.w = {}
            t.r = {}
        stk[0].close()
        stk[0] = ExitStack()

    psn = [0]

    def ps(name, shape, dt=F32):
        psn[0] += 1
        return stk[0].enter_context(nc.psum_tensor("%s_%d" % (name, psn[0]), list(shape), dt))

    def mkpsum(ntp, npm):
        tp_ = [ps("tp%d" % i, [128, 1024], BF16) for i in range(ntp)]
        ttp_ = [TR("tp%d" % i) for i in range(ntp)]
        pm_ = [ps("pm%d" % i, [128, 512]) for i in range(npm)]
        tpm_ = [TR("pm%d" % i) for i in range(npm)]
        return tp_, ttp_, pm_, tpm_

    V, A, P, PE, SP = "dve", "act", "pool", "pe", "sp"
    E = sc.eng
    rb_rt = E[P].to_reg(NE * S + 127)
    rb_h2 = E[P].to_reg(S + 127)
    rb_ys = E[P].to_reg(NSLOT - 1)

    def E_pool_to_reg(v):
        return E[P].to_reg(v)

    cst = sb("cst", [128, 5 * 128]); t_cst = TR("cst")
    sc.dma(SP, cst[:], consts, writes=[t_cst], owner=t_cst)
    ident_f = cst[:, 0:128]
    ones_f = cst[:, 128:256]
    identb = sb("identb", [128, 128], BF16); t_identb = TR("identb")
    onesb = sb("onesb", [128, 128], BF16)
    lstrb = sb("lstrb", [128, 128], BF16)
    negmb = sb("negmb", [128, 128], BF16)
    sc.op(V, lambda: E[V].tensor_copy(out=identb[:], in_=cst[:, 0:128]), [t_cst], [t_identb])
    sc.op(V, lambda: E[V].tensor_copy(out=onesb[:], in_=cst[:, 128:256]), [t_cst], [t_identb])
    sc.op(V, lambda: E[V].tensor_copy(out=lstrb[:], in_=cst[:, 256:384]), [t_cst], [t_identb])
    sc.op(V, lambda: E[V].tensor_copy(out=negmb[:], in_=cst[:, 384:512]), [t_cst], [t_identb])
    ones512 = sb("ones512", [128, MT]); t_ones512 = TR("ones512")
    sc.op(P, lambda: E[P].memset(ones512[:], 1.0), [], [t_ones512])
    epsb = sb("epsb", [128, 1]); t_eps = TR("epsb")
    sc.op(P, lambda: E[P].memset(epsb[:], EPS), [], [t_eps])
    t_c = TR("consts_all")

    def load_small(name, ap, shape, dt=F32):
        t = sb(name, shape, dt)
        sc.dma(SP, t[:], ap, writes=[t_c], owner=t_c)
        return t

    t_c.multi = True
    g1s = load_small("g1s", g1T, [128, 8])
    g2s = load_small("g2s", g2T, [128, 8])
    gconvs = load_small("gconvs", gconvT, [128, 4])
    gatts = load_small("gatts", gattT, [128, 4])
    bdws = load_small("bdws", bdwT, [128, 4])
    lngs = load_small("lngs", lngT, [128, 4])
    lnbs = load_small("lnbs", lnbT, [128, 4])
    wdws = load_small("wdws", wdwT, [128, 4 * CW])
    negbs = load_small("negbs", negbf, [128, 1])
    rbs = load_small("rbs", rbias, [128, 36])
    ebs = load_small("ebs", ebase, [128, NE])
    thrs = load_small("thrs", thr256, [128, 33])
    tsts = load_small("tsts", tst256, [128, NSLOT // TS])
    pcols = load_small("pcols", pcol, [128, 1])
    iotas = load_small("iotas", iota_tok, [128, NT], I32)

    biasT = sb("biasT", [128, NT, 8]); t_biasT = TR("biasT")
    dest1 = sb("dest1", [128, NT], I32); dest2 = sb("dest2", [128, NT], I32)
    wgt1 = sb("wgt1", [128, NT]); wgt2 = sb("wgt2", [128, NT])
    t_route = TR("route")
    NTL = NSLOT // TS
    widx = sb("widx", [128, NTL], I32); t_widx = TR("widx")
    ridx = sb("ridx", [128, NTL, TS // 128], I32)
    rb_w = E_pool_to_reg(NE * 128 - 1)

    selB = sb("selB", [128, 8, 128], BF16); t_sel = TR("selB")
    selc = sb("selc", [128, 8]); t_selc = TR("selc")
    sc.op(V, lambda: E[V].tensor_tensor(out=selc[:], in0=cst[:, 0:8], in1=cst[:, 32:40], op=ALU.add), [t_cst], [t_selc])
    for h in range(8):
        sc.op(V, lambda: E[V].tensor_scalar(out=selB[:, h, :], in0=cst[:, 128:256], scalar1=selc[:, h:h + 1], scalar2=None,
                                            op0=ALU.mult), [t_selc, t_cst], [t_sel])

    e64 = sb("e64", [65, 64]); t_e64 = TR("e64")
    sc.op(V, lambda: E[V].memset(e64[:], 0.0), [], [t_e64])
    sc.op(V, lambda: E[V].memset(e64[64:65, :], 1.0), [t_e64], [t_e64])
    persist = stk[0]
    stk[0] = ExitStack()
    WinB = sb("WinB", [128, 8, DIN], BF16); t_win = TR("WinB")
    WfB = sb("WfB", [128, 8, 128], BF16); t_wf = TR("WfB")
    stg = [sb("stg%d" % i, [128, DIN]) for i in range(1)] * 2
    t_stg = [TR("stg%d" % i) for i in range(1)] * 2
    sc.op(P, lambda: E[P].memset(WfB[:], 0.0), [], [t_wf])
    w_in_v = w_in.rearrange("(kc p) n -> p kc n", p=128)
    for kc in range(8):
        b = kc % 2
        sc.dma(SP, stg[b][:], w_in_v[:, kc, :], writes=[t_stg[b]], owner=t_stg[b])
        eng = A if kc % 2 == 0 else V
        if eng == A:
            sc.op(A, lambda: E[A].activation(out=WinB[:, kc, :], in_=stg[b][:], func=AF.Copy, scale=g1s[:, kc:kc + 1]),
                  [t_stg[b], t_c], [t_win])
        else:
            sc.op(V, lambda: E[V].tensor_scalar(out=WinB[:, kc, :], in0=stg[b][:], scalar1=g1s[:, kc:kc + 1], scalar2=None,
                                                op0=ALU.mult), [t_stg[b], t_c], [t_win])
    for q in range(4):
        sc.op(P, lambda: E[P].tensor_copy(out=WfB[:, :, q * 32:q * 32 + 8], in_=WinB[:, :, 2560:2568]), [t_win], [t_wf])

    diagW = sb("diagW", [128, CW, 4, 128], BF16); t_diag = TR("diagW")
    for k in range(CW):
        for c in range(4):
            eng = V if (k + c) % 2 == 0 else P
            sc.op(eng, lambda: E[eng].tensor_scalar(out=diagW[:, k, c, :], in0=cst[:, 0:128],
                                                    scalar1=wdws[:, c * CW + k:c * CW + k + 1], scalar2=None, op0=ALU.mult),
                  [t_cst, t_c], [t_diag])

    tp, t_tp, pm, t_pm = mkpsum(2, 6)

    xs = [sb("xs%d" % i, [128, 4, D]) for i in range(2)]; t_xs = [TR("xs%d" % i) for i in range(2)]
    junk = sb("junk", [128, D], BF16); t_junk = TR("junk")
    ssq = sb("ssq", [128, 4]); t_ssq = TR("ssq")
    rstd = sb("rstd", [128, 4]); t_rstd = TR("rstd")
    hb = sb("hb", [128, 4, D], BF16); t_hb = TR("hb")
    hT = sb("hT", [128, 8, MT], BF16); t_hT = TR("hT")
    aT = [sb("aT%d" % i, [128, 4, 30 + MT], BF16) for i in range(2)]; t_aT = [TR("aT%d" % i) for i in range(2)]
    sg = sb("sg", [128, MT]); t_sg = TR("sg")
    QT = sb("QT", [128, 4, MT], BF16); t_QT = TR("QT")
    KT = sb("KT", [128, 4, MT], BF16); t_KT = TR("KT")
    Vt = sb("Vt", [128, 4, 8, 65], BF16); t_Vt = TR("Vt")
    ef = sb("ef", [128, MT]); t_ef = TR("ef")
    spf = sb("spf", [128, MT]); t_spf = TR("spf")
    cum = sb("cum", [128, MT]); t_cum = TR("cum")
    carry = sb("carry", [128, 1]); t_carry = TR("carry")
    hiall = sb("hiall", [128, MT], BF16); t_hiall = TR("hiall")
    arow = sb("arow", [128, MT], BF16); t_arow = TR("arow")
    uu = sb("uu", [128, 4, MT]); t_uu = TR("uu")
    usq = sb("usq", [128, 4, MT], BF16); t_usq = TR("usq")
    m1 = sb("m1", [128, MT]); t_m1 = TR("m1")
    rs1 = sb("rs1", [128, MT]); t_rs1 = TR("rs1")
    mixc = sb("mixc", [128, 4, MT], BF16); t_mixc = TR("mixc")
    t_Qd = TR("Qd"); t_Kd = TR("Kd"); t_Vd = TR("Vd"); t_Ad = TR("Ad"); t_Mcd = TR("Mcd")
    for t in (t_Qd, t_Kd, t_Vd, t_Ad, t_Mcd):
        t.multi = True

    sc.op(P, lambda: E[P].memset(Vt[:], 1.0), [], [t_Vt])
    sc.op(P, lambda: E[P].memset(carry[:], 0.0), [], [t_carry])
    sc.op(P, lambda: E[P].memset(aT[0][:], 0.0), [], [t_aT[0]])
    sc.op(P, lambda: E[P].memset(aT[1][:], 0.0), [], [t_aT[1]])

    x_v = x.rearrange("(m s p) d -> m p s d", p=128, s=4)

    def inproj(bank, tbank, col0, M=128, lhs_src=None):
        def f():
            ins = None
            for kc in range(8):
                lhsT = WinB[:, kc, col0:col0 + M] if lhs_src is None else lhs_src[:, kc, :]
                ins = E[PE].matmul(pm[bank][0:M, :], lhsT=lhsT, rhs=hT[:, kc, :], start=(kc == 0), stop=(kc == 7))
            return ins
        sc.op(PE, f, [t_win, t_wf, t_hT], [tbank])

    def ph_A0(m):
        xb = xs[m % 2]; txb = t_xs[m % 2]
        sc.dma(SP, xb[:], x_v[m], writes=[txb], owner=txb)
        for s in range(4):
            sc.op(A, lambda: E[A].activation(out=junk[:], in_=xb[:, s, :], func=AF.Square, accum_out=ssq[:, s:s + 1]),
                  [txb], [t_junk, t_ssq])
        sc.op(A, lambda: E[A].activation(out=rstd[:], in_=ssq[:], func=AF.Sqrt, scale=1.0 / D, bias=epsb[:, 0:1]),
              [t_ssq, t_eps], [t_rstd])
        sc.op(V, lambda: E[V].reciprocal(out=rstd[:], in_=rstd[:]), [t_rstd], [t_rstd])
        for s in range(4):
            sc.op(A, lambda: E[A].activation(out=hb[:, s, :], in_=xb[:, s, :], func=AF.Copy, scale=rstd[:, s:s + 1]),
                  [txb, t_rstd], [t_hb])

    def ph_A1(m):
        xb = xs[m % 2]; txb = t_xs[m % 2]
        ab = aT[m % 2]; tab = t_aT[m % 2]
        abp = aT[(m + 1) % 2]; tabp = t_aT[(m + 1) % 2]
        for s in range(4):
            tb = tp[s % 2]; ttb = t_tp[s % 2]

            def f():
                ins = None
                for kc in range(8):
                    ins = E[PE].transpose(out=tb[:, kc * 128:(kc + 1) * 128], in_=hb[:, s, kc * 128:(kc + 1) * 128],
                                          identity=identb[:])
                return ins
            sc.op(PE, f, [t_hb, t_identb], [ttb])
            eng = A if s % 2 == 0 else V
            if eng == A:
                sc.op(A, lambda: E[A].copy(out=hT[:, :, s * 128:(s + 1) * 128], in_=tb[:].rearrange("p (k t) -> p k t", k=8)),
                      [ttb], [t_hT])
            else:
                sc.op(V, lambda: E[V].tensor_copy(out=hT[:, :, s * 128:(s + 1) * 128],
                                                  in_=tb[:].rearrange("p (k t) -> p k t", k=8)), [ttb], [t_hT])
        if m > 0:
            sc.op(P, lambda: E[P].tensor_copy(out=ab[:, :, 0:30], in_=abp[:, :, MT:MT + 30]), [tabp], [tab])
        for c in range(4):
            inproj(0, t_pm[0], 512 + c * 128)
            sc.op(A, lambda: E[A].activation(out=sg[:], in_=pm[0][:], func=AF.Sigmoid), [t_pm[0]], [t_sg])
            inproj(1, t_pm[1], c * 128)
            sc.op(V, lambda: E[V].tensor_tensor(out=ab[:, c, 30:30 + MT], in0=pm[1][:], in1=sg[:], op=ALU.mult),
                  [t_pm[1], t_sg], [tab])
        for p in range(4):
            inproj(0, t_pm[0], 1024 + p * 128)
            sc.op(A, lambda: E[A].copy(out=QT[:, p, :], in_=pm[0][:]), [t_pm[0]], [t_QT])
            inproj(1, t_pm[1], 1536 + p * 128)
            sc.op(V, lambda: E[V].tensor_copy(out=KT[:, p, :], in_=pm[1][:]), [t_pm[1]], [t_KT])
        sc.dma(P, Qd[:, m].rearrange("a p t -> p a t"), QT[:], reads=[t_QT], writes=[t_Qd], owner=t_QT)
        sc.dma(P, Kd[:, m].rearrange("a p t -> p a t"), KT[:], reads=[t_KT], writes=[t_Kd], owner=t_KT)
        for s in range(4):
            bank = (s % 2)

            def f():
                ins = None
                for kc in range(8):
                    ins = E[PE].matmul(pm[bank][:], lhsT=hT[:, kc, s * 128:(s + 1) * 128], rhs=WinB[:, kc, 2048:2560],
                                       start=(kc == 0), stop=(kc == 7))
                return ins
            sc.op(PE, f, [t_win, t_hT], [t_pm[bank]])
            eng = A if s % 2 == 0 else V
            src = pm[bank][:].rearrange("p (h d) -> p h d", h=8)
            if eng == A:
                sc.op(A, lambda: E[A].copy(out=Vt[:, s, :, 0:64], in_=src), [t_pm[bank]], [t_Vt])
            else:
                sc.op(V, lambda: E[V].tensor_copy(out=Vt[:, s, :, 0:64], in_=src), [t_pm[bank]], [t_Vt])
        for a in range(4):
            sc.dma(P, Vd[a, m], Vt[:, :, 2 * a:2 * a + 2, :].rearrange("p s h c -> p s (h c)"),
                   reads=[t_Vt], writes=[t_Vd], owner=t_Vt)
        inproj(0, t_pm[0], 0, lhs_src=WfB)
        sc.op(A, lambda: E[A].activation(out=ef[:], in_=pm[0][:], func=AF.Exp, scale=-1.0, bias=negbs[:, 0:1]),
              [t_pm[0], t_c], [t_ef])
        sc.op(A, lambda: E[A].activation(out=spf[:], in_=ef[:], func=AF.Ln, bias=1.0), [t_ef], [t_spf])
        sc.op(V, lambda: E[V].tensor_tensor_scan(out=cum[:], data0=ones512[:], data1=spf[:], initial=carry[:, 0:1],
                                                 op0=ALU.mult, op1=ALU.subtract), [t_spf, t_carry, t_ones512], [t_cum])
        sc.op(V, lambda: E[V].tensor_copy(out=carry[:], in_=cum[:, MT - 1:MT]), [t_cum], [t_carry])
        sc.op(V, lambda: E[V].tensor_scalar(out=hiall[:], in0=cum[:], scalar1=8.0, scalar2=None, op0=ALU.mult), [t_cum], [t_hiall])
        sc.op(P, lambda: E[P].tensor_copy(out=arow[:], in_=hiall[:]), [t_hiall], [t_arow])
        for q in (1, 3):
            sc.op(V, lambda: E[V].scalar_tensor_tensor(out=arow[q * 32:q * 32 + 32, :], in0=cum[q * 32:q * 32 + 32, :], scalar=8.0,
                                                       in1=hiall[q * 32:q * 32 + 32, :], op0=ALU.mult, op1=ALU.subtract),
                  [t_cum, t_hiall, t_arow], [t_arow])
        sc.dma(P, Ad[m], arow[:], reads=[t_arow], writes=[t_Ad], owner=t_arow)
        for s in range(4):
            sc.op(PE, lambda: E[PE].transpose(out=pm[1][:, 0:8], in_=cum[0:8, s * 128:(s + 1) * 128], identity=cst[0:8, 0:8]),
                  [t_cum, t_cst], [t_pm[1]])
            sc.op(V, lambda: E[V].tensor_scalar(out=biasT[:, m * 4 + s, :], in0=pm[1][:, 0:8], scalar1=-1.0, scalar2=None,
                                                op0=ALU.mult), [t_pm[1]], [t_biasT])

    def stat(bank, src, tsrc):
        ones_ = onesb[:] if src is usq else cst[:, 128:256]

        def f():
            ins = None
            for c in range(4):
                ins = E[PE].matmul(pm[bank][:], lhsT=ones_, rhs=src[:, c, :], start=(c == 0), stop=(c == 3))
            return ins
        sc.op(PE, f, [t_cst, t_identb, tsrc], [t_pm[bank]])

    def ph_B1(m):
        ab = aT[m % 2]; tab = t_aT[m % 2]
        for c in range(4):
            bank = 2 + (c % 2)

            def f():
                ins = None
                for k in range(CW):
                    ins = E[PE].matmul(pm[bank][:], lhsT=diagW[:, k, c, :], rhs=ab[:, c, k:k + MT], start=(k == 0),
                                       stop=(k == CW - 1))
                return ins
            sc.op(PE, f, [t_diag, tab], [t_pm[bank]])
            sc.op(A, lambda: E[A].activation(out=uu[:, c, :], in_=pm[bank][:], func=AF.Identity, bias=bdws[:, c:c + 1]),
                  [t_pm[bank], t_c], [t_uu])
            sc.op(A, lambda: E[A].activation(out=usq[:, c, :], in_=pm[bank][:], func=AF.Square, bias=bdws[:, c:c + 1]),
                  [t_pm[bank], t_c], [t_usq])

        stat(4, uu, t_uu)
        stat(5, usq, t_usq)

    def ph_B2(m):
        sc.op(V, lambda: E[V].tensor_scalar(out=m1[:], in0=pm[4][:], scalar1=1.0 / 512, scalar2=None, op0=ALU.mult), [t_pm[4]], [t_m1])
        sc.op(V, lambda: E[V].tensor_tensor(out=rs1[:], in0=m1[:], in1=m1[:], op=ALU.mult), [t_m1], [t_rs1])
        sc.op(V, lambda: E[V].scalar_tensor_tensor(out=rs1[:], in0=pm[5][:], scalar=1.0 / 512, in1=rs1[:], op0=ALU.mult,
                                                   op1=ALU.subtract), [t_pm[5], t_rs1], [t_rs1])
        sc.op(A, lambda: E[A].activation(out=rs1[:], in_=rs1[:], func=AF.Ln, bias=epsb[:, 0:1]), [t_rs1, t_eps], [t_rs1])
        sc.op(A, lambda: E[A].activation(out=rs1[:], in_=rs1[:], func=AF.Exp, scale=-0.5), [t_rs1], [t_rs1])
        for c in range(4):
            eng = V
            sc.op(eng, lambda: E[eng].tensor_tensor(out=uu[:, c, :], in0=uu[:, c, :], in1=m1[:], op=ALU.subtract), [t_uu, t_m1], [t_uu])
            sc.op(eng, lambda: E[eng].tensor_tensor(out=uu[:, c, :], in0=uu[:, c, :], in1=rs1[:], op=ALU.mult), [t_uu, t_rs1], [t_uu])
            sc.op(A, lambda: E[A].activation(out=uu[:, c, :], in_=uu[:, c, :], func=AF.Silu, scale=lngs[:, c:c + 1],
                                             bias=lnbs[:, c:c + 1]), [t_uu, t_c], [t_uu])
            sc.op(A, lambda: E[A].activation(out=usq[:, c, :], in_=uu[:, c, :], func=AF.Square), [t_uu], [t_usq])
        stat(4, usq, t_usq)
        sc.op(A, lambda: E[A].activation(out=rs1[:], in_=pm[4][:], func=AF.Ln, scale=1.0 / 512, bias=epsb[:, 0:1]),
              [t_pm[4], t_eps], [t_rs1])
        sc.op(A, lambda: E[A].activation(out=rs1[:], in_=rs1[:], func=AF.Exp, scale=-0.5), [t_rs1], [t_rs1])
        for c in range(4):
            eng = V
            sc.op(eng, lambda: E[eng].tensor_tensor(out=mixc[:, c, :], in0=uu[:, c, :], in1=rs1[:], op=ALU.mult),
                  [t_uu, t_rs1], [t_mixc])
        sc.dma(P, Mcd[m], mixc[:], reads=[t_mixc], writes=[t_Mcd], owner=t_mixc)


    ph_A0(0)
    ph_A1(0)
    for m in range(NM):
        if m + 1 < NM:
            ph_A0(m + 1)
        ph_B1(m)
        if m + 1 < NM:
            ph_A1(m + 1)
        ph_B2(m)

    if debug == "p1a":
        sc.wait_all(P, [t_Qd, t_Kd, t_Vd, t_Ad, t_Mcd, t_biasT])
        return nc

    new_phase()
    tp, t_tp, pm, t_pm = mkpsum(0, 8)
    NSB = 5
    Qs = [[sb("Qs%d_%d" % (i, j), [128, MT], BF16) for j in range(2)] for i in range(2)]
    t_Qs = [[TR("Qs%d_%d" % (i, j)) for j in range(2)] for i in range(2)]
    NKB = 3
    Kc = [[sb("Kc%d_%d" % (i, j), [128, MT], BF16) for j in range(2)] for i in range(NKB)]
    t_Kc = [[TR("Kc%d_%d" % (i, j)) for j in range(2)] for i in range(NKB)]
    Vc = [sb("Vc%d" % i, [128, 4, 2, 128], BF16) for i in range(NKB)]; t_Vc = [TR("Vc%d" % i) for i in range(NKB)]
    NPB = 4
    Pt = [sb("Pt%d" % i, [128, MT], BF16) for i in range(NPB)]; t_Pt = [TR("Pt%d" % i) for i in range(NPB)]
    Osb = [sb("Osb%d" % i, [65, MT]) for i in range(2)]; t_Osb = [TR("Osb%d" % i) for i in range(2)]
    Ya = [sb("Ya%d" % i, [64, 2, MT], BF16) for i in range(2)]; t_Ya = [TR("Ya%d" % i) for i in range(2)]
    for i_ in range(2):
        for j_ in range(2):
            sc.op(P, lambda: E[P].memset(Qs[i_][j_][:], 0.0), [], [t_Qs[i_][j_]])
            t_Qs[i_][j_].multi = True
    for i_ in range(NKB):
        for j_ in range(2):
            sc.op(P, lambda: E[P].memset(Kc[i_][j_][:], 0.0), [], [t_Kc[i_][j_]])
            sc.op(P, lambda: E[P].memset(Kc[i_][j_][64:66, :], 1.0), [t_Kc[i_][j_]], [t_Kc[i_][j_]])
        sc.op(P, lambda: E[P].memset(Vc[i_][:], 0.0), [], [t_Vc[i_]])
        t_Vc[i_].multi = True
    t_Yad = TR("Yad"); t_Yad.multi = True

    steps = []
    grp = 0
    for a in range(4):
        for i in range(NM):
            for c in range(i + 1):
                for hh in range(2):
                    for jj in range(4):
                        steps.append(dict(a=a, i=i, c=c, hh=hh, jj=jj, grp=grp,
                                          first=(c == 0 and jj == 0), last=(c == i and jj == 3)))
            grp += 1
    nld = 0
    cur_q = {}
    cur_c = {}

    def emit_S(n):
        nonlocal nld
        st = steps[n]
        a, i, c, hh, jj = st["a"], st["i"], st["c"], st["hh"], st["jj"]
        g = st["grp"]
        if g not in cur_q:
            qb = g % 2
            for h2 in range(2):
                h_ = 2 * a + h2
                tq = t_Qs[qb][h2]
                sc.dma(SP, Qs[qb][h2][0:64, :], Qd[a, i, h2 * 64:(h2 + 1) * 64, :], reads=[t_Qd], writes=[tq], owner=tq)
                sc.dma(SP, Qs[qb][h2][64:65, :], Ad[i, h_:h_ + 1, :], reads=[t_Ad], writes=[tq], owner=tq)
                sc.dma(SP, Qs[qb][h2][65:66, :], Ad[i, 32 + h_:33 + h_, :], reads=[t_Ad], writes=[tq], owner=tq)
            cur_q[g] = qb
        if (g, c) not in cur_c:
            kb = nld % NKB
            nld += 1
            for h2 in range(2):
                sc.dma(SP, Kc[kb][h2][0:64, :], Kd[a, c, h2 * 64:(h2 + 1) * 64, :], reads=[t_Kd], writes=[t_Kc[kb][h2]],
                       owner=t_Kc[kb][h2])
                sc.dma(SP, Vc[kb][:, :, h2, 0:65], Vd[a, c, :, :, h2 * 65:(h2 + 1) * 65], reads=[t_Vd], writes=[t_Vc[kb]],
                       owner=t_Vc[kb])
            cur_c[(g, c)] = kb
        qb = cur_q[g]; kb = cur_c[(g, c)]
        st["qb"] = qb; st["kb"] = kb
        h = 2 * a + hh
        diag = (c == i)
        q0 = 128 * jj if diag else 0
        st["q0"] = q0; st["h"] = h
        bank = n % NSB
        st["bank"] = bank

        def f():
            ins = E[PE].matmul(pm[bank][:, q0:], lhsT=Kc[kb][hh][:, jj * 128:(jj + 1) * 128], rhs=Qs[qb][hh][:, q0:],
                               start=True, stop=not diag)
            if diag:
                ins = E[PE].matmul(pm[bank][:, q0:q0 + 128], lhsT=identb[:], rhs=negmb[:], start=False, stop=True)
            return ins
        sc.op(PE, f, [t_Kc[kb][hh], t_Qs[qb][hh], t_identb], [t_pm[bank]])

    def emit_exp(n):
        st = steps[n]
        bank = st["bank"]; q0 = st["q0"]; h = st["h"]
        j = 4 * st["c"] + st["jj"]
        pb = n % NPB
        sc.op(A, lambda: E[A].activation(out=Pt[pb][:, q0:], in_=pm[bank][:, q0:], func=AF.Exp, scale=0.125,
                                         bias=biasT[:, j, h:h + 1]), [t_pm[bank], t_biasT], [t_Pt[pb]])

    def emit_PV(n):
        st = steps[n]
        hh = st["hh"]; q0 = st["q0"]; kb = st["kb"]; jj = st["jj"]
        pb = n % NPB
        ob = 5 + hh
        sc.op(PE, lambda: E[PE].matmul(pm[ob][:, q0:], lhsT=Vc[kb][:, jj, hh, :], rhs=Pt[pb][:, q0:],
                                       start=st["first"], stop=st["last"]), [t_Vc[kb], t_Pt[pb]], [t_pm[ob]])
        if st["last"] and hh == 1:
            a, i = st["a"], st["i"]
            yb = st["grp"] % 2
            for h2 in range(2):
                obk = 5 + h2
                sc.op(V, lambda: E[V].tensor_copy(out=Osb[h2][:], in_=pm[obk][0:65, :]), [t_pm[obk]], [t_Osb[h2]])
                sc.op(V, lambda: E[V].reciprocal(out=Osb[h2][64:65, :], in_=Osb[h2][64:65, :]), [t_Osb[h2]], [t_Osb[h2]])
                sc.op(PE, lambda: E[PE].matmul(pm[7][0:64, :], lhsT=e64[:], rhs=Osb[h2][:], start=True, stop=True),
                      [t_e64, t_Osb[h2]], [t_pm[7]])
                sc.op(V, lambda: E[V].tensor_tensor(out=Ya[yb][:, h2, :], in0=Osb[h2][0:64, :], in1=pm[7][0:64, :], op=ALU.mult),
                      [t_Osb[h2], t_pm[7]], [t_Ya[yb]])
            sc.dma(P, Yad[i, :, 2 * a:2 * a + 2, :], Ya[yb][:], reads=[t_Ya[yb]], writes=[t_Yad], owner=t_Ya[yb])

    NS = len(steps)
    LAG = 2
    for n in range(NS + LAG):
        if n < NS:
            emit_S(n)
            emit_exp(n)
        if n >= LAG:
            emit_PV(n - LAG)

    if debug == "p1b":
        sc.wait_all(P, [t_Qd, t_Kd, t_Vd, t_Ad, t_Mcd, t_biasT, t_Yad])
        return nc

    new_phase()
    tp, t_tp, pm, t_pm = mkpsum(2, 6)
    t_Xmd = TR("Xmd"); t_Xmd.multi = True
    t_H2d = TR("H2d"); t_H2d.multi = True
    t_RTd = TR("RTd"); t_RTd.multi = True
    WoC = sb("WoC", [128, 4, D], BF16); t_WoC = TR("WoC")
    WoA = sb("WoA", [128, 4, D], BF16); t_WoA = TR("WoA")
    WrB = sb("WrB", [128, 8, 36], BF16); t_WrB = TR("WrB")
    wst = sb("wst", [128, 8, D]); t_wst = TR("wst")
    wrs = sb("wrs", [128, 8, 36]); t_wrs = TR("wrs")
    g2b = sb("g2b", [128, D]); t_g2b = TR("g2b")
    sc.dma(SP, g2b[:], g2bc, writes=[t_g2b], owner=t_g2b)
    sc.dma(SP, wst[:, 0:4, :], w_out[0:512, :].rearrange("(c p) n -> p c n", p=128), writes=[t_wst], owner=t_wst)
    for c in range(4):
        sc.op(V, lambda: E[V].tensor_scalar(out=WoC[:, c, :], in0=wst[:, c, :], scalar1=gconvs[:, c:c + 1], scalar2=None,
                                            op0=ALU.mult), [t_wst, t_c], [t_WoC])
    sc.dma(SP, wst[:, 4:8, :], w_out[512:1024, :].rearrange("(a p) n -> p a n", p=128), writes=[t_wst], owner=t_wst)
    for a in range(4):
        sc.op(V, lambda: E[V].tensor_scalar(out=WoA[:, a, :], in0=wst[:, 4 + a, :], scalar1=gatts[:, a:a + 1], scalar2=None,
                                            op0=ALU.mult), [t_wst, t_c], [t_WoA])
    sc.dma(SP, wrs[:], wr.rearrange("(kc p) n -> p kc n", p=128), writes=[t_wrs], owner=t_wrs)
    sc.op(V, lambda: E[V].tensor_copy(out=WrB[:], in_=wrs[:]), [t_wrs], [t_WrB])
    NRT = (NE * S + 128) // 128
    rti = sb("rti", [128, NRT], I32); t_rti = TR("rti")
    sc.op(P, lambda: E[P].memset(rti[:], S), [], [t_rti])
    sc.dma(P, RTd.rearrange("(p n) o -> p (n o)", p=128), rti[:], reads=[t_rti], writes=[t_RTd], owner=t_rti)
    dp1 = sb("dp1", [128, NT], I32); dp2 = sb("dp2", [128, NT], I32)
    zrow = sb("zrow", [128, D], BF16); t_zrow = TR("zrow")
    sc.op(P, lambda: E[P].memset(zrow[:], 0.0), [], [t_zrow])
    sc.dma(P, H2d[S:S + 128, :], zrow[:], reads=[t_zrow], writes=[t_H2d], owner=t_zrow)

    mcs = [sb("mcs%d" % i, [128, 4, MT], BF16) for i in range(2)]; t_mcs = [TR("mcs%d" % i) for i in range(2)]
    yas = [sb("yas%d" % i, [128, 4, MT], BF16) for i in range(2)]; t_yas = [TR("yas%d" % i) for i in range(2)]
    for i_ in range(2):
        t_yas[i_].multi = True
    xs2 = [sb("xs2%d" % i, [128, 4, D]) for i in range(2)]; t_xs2 = [TR("xs2%d" % i) for i in range(2)]
    sqa = sb("sqa", [128, 4, MT], BF16); t_sqa = TR("sqa")
    r4 = sb("r4", [128, MT]); t_r4 = TR("r4")
    mixa = sb("mixa", [128, 4, MT], BF16); t_mixa = TR("mixa")
    xm = [sb("xm%d" % i, [128, D]) for i in range(4)]; t_xm = [TR("xm%d" % i) for i in range(4)]
    h2 = [sb("h2%d" % i, [128, D], BF16) for i in range(4)]; t_h2 = [TR("h2%d" % i) for i in range(4)]
    h2T = [sb("h2T%d" % i, [128, 8, 128], BF16) for i in range(4)]; t_h2T = [TR("h2T%d" % i) for i in range(4)]
    junk2 = sb("junk2", [128, D], BF16); t_junk2 = TR("junk2")
    R = []
    for i_ in range(4):
        R.append(dict(
            sm=sb("sm%d" % i_, [128, 32]), lg=sb("lg%d" % i_, [128, 36]), l2=sb("l2_%d" % i_, [128, 8]), l2m=sb("l2m%d" % i_, [128, 8]),
            oh1=sb("oh1_%d" % i_, [128, 8]), oh2=sb("oh2_%d" % i_, [128, 8]), ohg=sb("ohg%d" % i_, [128, 4]), e1=sb("e1_%d" % i_, [128, 4]),
            M1=sb("M1_%d" % i_, [128, 32]), M2=sb("M2_%d" % i_, [128, 32]), Mb=sb("Mb%d" % i_, [128, 32], BF16),
            Rf=sb("Rf%d" % i_, [128, 32]), tmpr=sb("tmpr%d" % i_, [128, 32]), tmpr2=sb("tmpr2_%d" % i_, [128, 32]),
            t=TR("rt%d" % i_), tlg=TR("lgp%d" % i_), tR=TR("Rp%d" % i_), tC=TR("Cp%d" % i_)))
    rank1 = sb("rank1", [128, NT]); rank2 = sb("rank2", [128, NT]); eid1 = sb("eid1", [128, NT]); eid2 = sb("eid2", [128, NT])
    t_rk = TR("rankeid")
    carryR = sb("carryR", [128, 32]); t_carryR = TR("carryR")
    sc.op(V, lambda: E[V].memset(carryR[:], 0.0), [], [t_carryR])
    lg4 = sb("lg4", [128, 4, 36]); mx4 = sb("mx4", [128, 4]); ohg4 = sb("ohg4", [128, 4, 4]); d14 = sb("d14", [128, 4, 4])
    e14 = sb("e14", [128, 4, 4]); se4 = sb("se4", [128, 4]); p1s4 = sb("p1s4", [128, 4]); prod4 = sb("prod4", [128, 4, 4, 8])
    l24 = sb("l24", [128, 4, 8]); m14 = sb("m14", [128, 4]); oh14 = sb("oh14", [128, 4, 8]); l2m4 = sb("l2m4", [128, 4, 8])
    m24 = sb("m24", [128, 4]); oh24 = sb("oh24", [128, 4, 8]); d214 = sb("d214", [128, 4]); ed4 = sb("ed4", [128, 4])
    den4 = sb("den4", [128, 4]); w14 = sb("w14", [128, 4]); w24 = sb("w24", [128, 4])
    M14 = sb("M14", [128, 4, 32]); M24 = sb("M24", [128, 4, 32]); Mb4 = sb("Mb4", [128, 4, 32], BF16)
    Rf4 = sb("Rf4", [128, 4, 32]); tmp4 = sb("tmp4", [128, 4, 32]); dpf4 = sb("dpf4", [128, 4])
    t_rt4 = TR("rt4")

    def vr(fn, reads=(), writes=()):
        sc.op(V, fn, list(reads) + [t_rt4], list(writes) + [t_rt4])

    def bc(ap2d, shape3, axis):
        pst, _ = ap2d.ap[0]
        est, n = ap2d.ap[1]
        if axis == 1:
            return bass.AP(tensor=ap2d.tensor, offset=ap2d.offset, ap=[[pst, 128], [0, shape3[1]], [est, n]])
        return bass.AP(tensor=ap2d.tensor, offset=ap2d.offset, ap=[[pst, 128], [est, n], [0, shape3[2]]])

    def load1c(m):
        b = m % 2
        sc.dma(SP, mcs[b][:], Mcd[m], reads=[t_Mcd], writes=[t_mcs[b]], owner=t_mcs[b])
        for hh in range(2):
            sc.dma(SP, yas[b][hh * 64:(hh + 1) * 64, :, :], Yad[m].rearrange("d (a two) t -> d a two t", two=2)[:, :, hh, :],
                   reads=[t_Yad], writes=[t_yas[b]], owner=t_yas[b])
        sc.dma(SP, xs2[b][:], x_v[m], writes=[t_xs2[b]], owner=t_xs2[b])

    load1c(0)
    for m in range(NM):
        b = m % 2
        if m + 1 < NM:
            load1c(m + 1)
        sc.op(A, lambda: E[A].activation(out=sqa[:], in_=yas[b][:], func=AF.Square), [t_yas[b]], [t_sqa])

        def f():
            ins = None
            for a in range(4):
                ins = E[PE].matmul(pm[5][:], lhsT=onesb[:], rhs=sqa[:, a, :], start=(a == 0), stop=(a == 3))
            return ins
        sc.op(PE, f, [t_identb, t_sqa], [t_pm[5]])
        sc.op(A, lambda: E[A].activation(out=r4[:], in_=pm[5][:], func=AF.Ln, scale=1.0 / 512, bias=epsb[:, 0:1]),
              [t_pm[5], t_eps], [t_r4])
        sc.op(A, lambda: E[A].activation(out=r4[:], in_=r4[:], func=AF.Exp, scale=-0.5), [t_r4], [t_r4])
        for a in range(4):
            sc.op(V, lambda: E[V].tensor_tensor(out=mixa[:, a, :], in0=yas[b][:, a, :], in1=r4[:], op=ALU.mult),
                  [t_yas[b], t_r4], [t_mixa])
        for s in range(4):
            tile = m * 4 + s
            rs_ = R[s]; sm = rs_["sm"]
            xb2 = xm[s]; txm = t_xm[s]
            hb2 = h2[s]; th2 = t_h2[s]
            for n in range(2):
                bank = n

                def f():
                    ins = None
                    for c in range(4):
                        ins = E[PE].matmul(pm[bank][:], lhsT=mcs[b][:, c, s * 128:(s + 1) * 128], rhs=WoC[:, c, n * 512:(n + 1) * 512],
                                           start=(c == 0), stop=False)
                    for a in range(4):
                        ins = E[PE].matmul(pm[bank][:], lhsT=mixa[:, a, s * 128:(s + 1) * 128], rhs=WoA[:, a, n * 512:(n + 1) * 512],
                                           start=False, stop=(a == 3))
                    return ins
                sc.op(PE, f, [t_mcs[b], t_mixa, t_WoC, t_WoA], [t_pm[bank]])
                sc.op(V, lambda: E[V].tensor_tensor(out=xb2[:, n * 512:(n + 1) * 512], in0=pm[bank][:],
                                                    in1=xs2[b][:, s, n * 512:(n + 1) * 512], op=ALU.add),
                      [t_pm[bank], t_xs2[b]], [txm])
            sc.dma(SP, Xmd[tile * 128:(tile + 1) * 128, :], xb2[:], reads=[txm], writes=[t_Xmd], owner=txm)
            sc.op(A, lambda: E[A].activation(out=junk2[:], in_=xb2[:], func=AF.Square, accum_out=sm[:, 0:1]),
                  [txm], [t_junk2, rs_["t"]])
            sc.op(A, lambda: E[A].activation(out=sm[:, 1:2], in_=sm[:, 0:1], func=AF.Sqrt, scale=1.0 / D, bias=epsb[:, 0:1]),
                  [rs_["t"], t_eps], [rs_["t"]])
            sc.op(V, lambda: E[V].reciprocal(out=sm[:, 2:3], in_=sm[:, 1:2]), [rs_["t"]], [rs_["t"]])
            sc.op(V, lambda: E[V].scalar_tensor_tensor(out=hb2[:], in0=xb2[:], scalar=sm[:, 2:3], in1=g2b[:], op0=ALU.mult,
                                                       op1=ALU.mult), [txm, rs_["t"], t_g2b], [th2])
            sc.dma(SP, H2d[tile * 128:(tile + 1) * 128, :], hb2[:], reads=[th2], writes=[t_H2d], owner=th2)
        for s in range(4):
            tile = m * 4 + s
            rs_ = R[s]
            hb2 = h2[s]; th2 = t_h2[s]
            tb = tp[tile % 2]; ttb = t_tp[tile % 2]

            def f():
                ins = None
                for kc in range(8):
                    ins = E[PE].transpose(out=tb[:, kc * 128:(kc + 1) * 128], in_=hb2[:, kc * 128:(kc + 1) * 128], identity=identb[:])
                return ins
            sc.op(PE, f, [th2, t_identb], [ttb])
            sc.op(A, lambda: E[A].copy(out=h2T[s][:], in_=tb[:].rearrange("p (k t) -> p k t", k=8)), [ttb], [t_h2T[s]])

            def f():
                ins = None
                for kc in range(8):
                    ins = E[PE].matmul(pm[2][:, s * 64:s * 64 + 36], lhsT=h2T[s][:, kc, :], rhs=WrB[:, kc, :], start=(kc == 0),
                                       stop=(kc == 7))
                return ins
            sc.op(PE, f, [t_h2T[s], t_WrB], [rs_["tlg"]])

        c0 = 4 * m
        lgp = pm[2][:, 0:256].rearrange("p (s c) -> p s c", c=64)[:, :, 0:36]
        vr(lambda: E[V].tensor_tensor(out=lg4[:], in0=lgp, in1=bc(rbs[:], [128, 4, 36], 1), op=ALU.add),
           [R[0]["tlg"], R[1]["tlg"], R[2]["tlg"], R[3]["tlg"], t_c])
        vr(lambda: E[V].reduce_max(out=mx4[:], in_=lg4[:, :, 0:4], axis=AX.X))
        vr(lambda: E[V].tensor_tensor(out=ohg4[:], in0=lg4[:, :, 0:4], in1=bc(mx4[:], [128, 4, 4], 2), op=ALU.is_equal))
        vr(lambda: E[V].tensor_tensor(out=d14[:], in0=lg4[:, :, 0:4], in1=bc(mx4[:], [128, 4, 4], 2), op=ALU.subtract))
        sc.op(A, lambda: E[A].activation(out=e14[:], in_=d14[:], func=AF.Exp), [t_rt4], [t_rt4])
        vr(lambda: E[V].reduce_sum(out=se4[:], in_=e14[:], axis=AX.X))
        vr(lambda: E[V].reciprocal(out=p1s4[:], in_=se4[:]))
        vr(lambda: E[V].tensor_tensor(out=prod4[:], in0=lg4[:, :, 4:36].rearrange("p s (g e) -> p s g e", e=8),
                                      in1=bass.AP(tensor=ohg4[:].tensor, offset=ohg4[:].offset,
                                                  ap=[[16, 128], [4, 4], [1, 4], [0, 8]]), op=ALU.mult))
        vr(lambda: E[V].reduce_sum(out=l24[:], in_=prod4[:].rearrange("p s g e -> p s e g"), axis=AX.X))
        vr(lambda: E[V].reduce_max(out=m14[:], in_=l24[:], axis=AX.X))
        vr(lambda: E[V].tensor_tensor(out=oh14[:], in0=l24[:], in1=bc(m14[:], [128, 4, 8], 2), op=ALU.is_equal))
        vr(lambda: E[V].scalar_tensor_tensor(out=l2m4[:], in0=oh14[:], scalar=-1e30, in1=l24[:], op0=ALU.mult, op1=ALU.add))
        vr(lambda: E[V].reduce_max(out=m24[:], in_=l2m4[:], axis=AX.X))
        vr(lambda: E[V].tensor_tensor(out=oh24[:], in0=l2m4[:], in1=bc(m24[:], [128, 4, 8], 2), op=ALU.is_equal))
        vr(lambda: E[V].tensor_tensor(out=d214[:], in0=m24[:], in1=m14[:], op=ALU.subtract))
        sc.op(A, lambda: E[A].activation(out=ed4[:], in_=d214[:], func=AF.Exp), [t_rt4], [t_rt4])
        vr(lambda: E[V].tensor_scalar(out=den4[:], in0=ed4[:], scalar1=1.0, scalar2=None, op0=ALU.add))
        vr(lambda: E[V].reciprocal(out=w14[:], in_=den4[:]))
        vr(lambda: E[V].tensor_tensor(out=wgt1[:, c0:c0 + 4], in0=w14[:], in1=p1s4[:], op=ALU.mult), [], [t_route])
        vr(lambda: E[V].tensor_tensor(out=w24[:], in0=ed4[:], in1=w14[:], op=ALU.mult))
        vr(lambda: E[V].tensor_tensor(out=wgt2[:, c0:c0 + 4], in0=w24[:], in1=p1s4[:], op=ALU.mult), [], [t_route])
        ohg_b = bass.AP(tensor=ohg4[:].tensor, offset=ohg4[:].offset, ap=[[16, 128], [4, 4], [1, 4], [0, 8]])
        oh1_b = bass.AP(tensor=oh14[:].tensor, offset=oh14[:].offset, ap=[[32, 128], [8, 4], [0, 4], [1, 8]])
        oh2_b = bass.AP(tensor=oh24[:].tensor, offset=oh24[:].offset, ap=[[32, 128], [8, 4], [0, 4], [1, 8]])
        vr(lambda: E[V].tensor_tensor(out=M14[:].rearrange("p s (g e) -> p s g e", e=8), in0=ohg_b, in1=oh1_b, op=ALU.mult))
        vr(lambda: E[V].tensor_tensor(out=M24[:].rearrange("p s (g e) -> p s g e", e=8), in0=ohg_b, in1=oh2_b, op=ALU.mult))
        vr(lambda: E[V].tensor_tensor(out=Mb4[:], in0=M14[:], in1=M24[:], op=ALU.add))

        def fR():
            ins = None
            for s_ in range(4):
                E[PE].matmul(pm[3][:, s_ * 32:(s_ + 1) * 32], lhsT=lstrb[:], rhs=Mb4[:, s_, :], start=True, stop=True)
                ins = E[PE].matmul(pm[4][:, s_ * 32:(s_ + 1) * 32], lhsT=onesb[:], rhs=Mb4[:, s_, :], start=True, stop=True)
            return ins
        sc.op(PE, fR, [t_identb, t_rt4], [t_pm[3], t_pm[4]])
        for s_ in range(4):
            vr(lambda: E[V].tensor_tensor(out=Rf4[:, s_, :], in0=pm[3][:, s_ * 32:(s_ + 1) * 32], in1=carryR[:], op=ALU.add),
               [t_pm[3], t_carryR])
            sc.op(V, lambda: E[V].tensor_tensor(out=carryR[:], in0=carryR[:], in1=pm[4][:, s_ * 32:(s_ + 1) * 32], op=ALU.add),
                  [t_pm[4], t_carryR], [t_carryR])
        for (Mx, rk, ei) in ((M14, rank1, eid1), (M24, rank2, eid2)):
            vr(lambda: E[V].tensor_tensor(out=tmp4[:], in0=Rf4[:], in1=Mx[:], op=ALU.mult))
            vr(lambda: E[V].reduce_sum(out=rk[:, c0:c0 + 4], in_=tmp4[:], axis=AX.X), [], [t_rk])
            vr(lambda: E[V].tensor_tensor(out=tmp4[:], in0=bc(ebs[:], [128, 4, 32], 1), in1=Mx[:], op=ALU.mult), [t_c])
            vr(lambda: E[V].reduce_sum(out=ei[:, c0:c0 + 4], in_=tmp4[:], axis=AX.X), [], [t_rk])
        for (ei, rk, dpp) in ((eid1, rank1, dp1), (eid2, rank2, dp2)):
            vr(lambda: E[V].scalar_tensor_tensor(out=dpf4[:], in0=ei[:, c0:c0 + 4], scalar=float(S), in1=rk[:, c0:c0 + 4],
                                                 op0=ALU.mult, op1=ALU.add), [t_rk])
            vr(lambda: E[V].tensor_copy(out=dpp[:, c0:c0 + 4], in_=dpf4[:]))
            for s_ in range(4):
                tile = c0 + s_
                sc.dma(P, None, None, reads=[t_rt4, t_c], writes=[t_RTd], owner=t_rt4,
                       fn=lambda: E[P].indirect_dma_start(out=RTd[:, :], out_offset=bass.IndirectOffsetOnAxis(ap=dpp[:, tile:tile + 1], axis=0),
                                                          in_=iotas[:, tile:tile + 1], in_offset=None,
                                                          bounds_check=rb_rt, oob_is_err=False))

    def bc(ap2d, shape3, axis):
        pst, _ = ap2d.ap[0]
        est, n = ap2d.ap[1]
        if axis == 1:
            return bass.AP(tensor=ap2d.tensor, offset=ap2d.offset, ap=[[pst, 128], [0, shape3[1]], [est, n]])
        return bass.AP(tensor=ap2d.tensor, offset=ap2d.offset, ap=[[pst, 128], [est, n], [0, shape3[2]]])
    cmpk = sb("cmpk", [128, 32, 33]); ntl = sb("ntl", [128, 32]); padd = sb("padd", [128, 32]); pend = sb("pend", [128, 32])
    pstt = sb("pstt", [128, 32]); bigt = sb("bigt", [128, NT, 32]); psel = sb("psel", [128, NT]); destf = sb("destf", [128, NT])
    cmpt = sb("cmpt", [128, NTL, 32]); tef = sb("tef", [128, NTL]); widf = sb("widf", [128, NTL])
    t_fin = TR("fin")

    def fop(fn, reads=(), writes=()):
        sc.op(V, fn, list(reads) + [t_fin], list(writes) + [t_fin])
    fop(lambda: E[V].tensor_tensor(out=cmpk[:], in0=bc(carryR[:], [128, 32, 33], 2), in1=bc(thrs[:], [128, 32, 33], 1), op=ALU.is_gt),
        [t_carryR, t_c])
    fop(lambda: E[V].reduce_sum(out=ntl[:], in_=cmpk[:], axis=AX.X))
    fop(lambda: E[V].tensor_scalar(out=padd[:], in0=ntl[:], scalar1=float(TS), scalar2=None, op0=ALU.mult))
    fop(lambda: E[V].tensor_tensor_scan(out=pend[:], data0=ones512[:, 0:32], data1=padd[:], initial=0.0, op0=ALU.mult, op1=ALU.add),
        [t_ones512])
    fop(lambda: E[V].tensor_tensor(out=pstt[:], in0=pend[:], in1=padd[:], op=ALU.subtract))
    for (rk, ei, dd) in ((rank1, eid1, dest1), (rank2, eid2, dest2)):
        fop(lambda: E[V].tensor_tensor(out=bigt[:], in0=bc(ei[:], [128, NT, 32], 2), in1=bc(ebs[:], [128, NT, 32], 1), op=ALU.is_equal),
            [t_rk, t_c])
        fop(lambda: E[V].tensor_tensor(out=bigt[:], in0=bigt[:], in1=bc(pstt[:], [128, NT, 32], 1), op=ALU.mult))
        fop(lambda: E[V].reduce_sum(out=psel[:], in_=bigt[:], axis=AX.X))
        fop(lambda: E[V].tensor_tensor(out=destf[:], in0=psel[:], in1=rk[:], op=ALU.add), [t_rk])
        fop(lambda: E[V].tensor_copy(out=dd[:], in_=destf[:]), [], [t_route])
    fop(lambda: E[V].tensor_tensor(out=cmpt[:], in0=bc(pend[:], [128, NTL, 32], 1), in1=bc(tsts[:], [128, NTL, 32], 2), op=ALU.is_le),
        [t_c])
    fop(lambda: E[V].reduce_sum(out=tef[:], in_=cmpt[:], axis=AX.X))
    fop(lambda: E[V].tensor_scalar(out=widf[:], in0=tef[:], scalar1=128.0, scalar2=pcols[:, 0:1], op0=ALU.mult, op1=ALU.add), [t_c])
    eqf = sb("eqf", [128, NTL])
    fop(lambda: E[V].memset(eqf[:], 0.0))
    fop(lambda: E[V].tensor_tensor(out=eqf[:, 2:NTL], in0=tef[:, 2:NTL], in1=tef[:, 0:NTL - 2], op=ALU.is_equal))
    fop(lambda: E[V].scalar_tensor_tensor(out=widf[:], in0=eqf[:], scalar=float(NE * 128), in1=widf[:], op0=ALU.mult, op1=ALU.add))
    fop(lambda: E[V].tensor_copy(out=widx[:], in_=widf[:]), [], [t_widx])
    pstl = sb("pstl", [128, NTL]); basef = sb("basef", [128, NTL]); rif = sb("rif", [128, NTL])
    fop(lambda: E[V].tensor_tensor(out=cmpt[:], in0=bc(tef[:], [128, NTL, 32], 2), in1=bc(ebs[:], [128, NTL, 32], 1), op=ALU.is_equal),
        [t_c])
    fop(lambda: E[V].tensor_tensor(out=cmpt[:], in0=cmpt[:], in1=bc(pstt[:], [128, NTL, 32], 1), op=ALU.mult))
    fop(lambda: E[V].reduce_sum(out=pstl[:], in_=cmpt[:], axis=AX.X))
    fop(lambda: E[V].scalar_tensor_tensor(out=basef[:], in0=tef[:], scalar=float(S), in1=tsts[:], op0=ALU.mult, op1=ALU.add), [t_c])
    fop(lambda: E[V].tensor_tensor(out=basef[:], in0=basef[:], in1=pstl[:], op=ALU.subtract))
    for st in range(TS // 128):
        fop(lambda: E[V].tensor_scalar(out=rif[:], in0=basef[:], scalar1=pcols[:, 0:1], scalar2=float(128 * st), op0=ALU.add, op1=ALU.add),
            [t_c])
        fop(lambda: E[V].tensor_copy(out=ridx[:, :, st], in_=rif[:]), [], [t_widx])

    new_phase()
    tp, t_tp, pm, t_pm = mkpsum(2, 6)
    t_Ysd = TR("Ysd"); t_Ysd.multi = True
    NSUB = TS // 128
    Wg = [sb("Wg%d" % i, [128, 8, DE], BF16) for i in range(2)]; t_Wg = [TR("Wg%d" % i) for i in range(2)]
    Wu = [sb("Wu%d" % i, [128, 8, DE], BF16) for i in range(2)]; t_Wu = [TR("Wu%d" % i) for i in range(2)]
    Wd = [sb("Wd%d" % i, [128, 2, D], BF16) for i in range(2)]; t_Wd = [TR("Wd%d" % i) for i in range(2)]
    Wgs = [sb("Wgs%d" % i, [128, 8 * DE]) for i in range(2)]; t_Wgs = [TR("Wgs%d" % i) for i in range(2)]
    Wus = [sb("Wus%d" % i, [128, 8 * DE]) for i in range(2)]; t_Wus = [TR("Wus%d" % i) for i in range(2)]
    Wds = [sb("Wds%d" % i, [128, 2 * D]) for i in range(2)]; t_Wds = [TR("Wds%d" % i) for i in range(2)]
    for i_ in range(2):
        sc.op(V, lambda: E[V].memset(Wgs[i_][:], 0.0), [], [t_Wgs[i_]])
        sc.op(V, lambda: E[V].memset(Wus[i_][:], 0.0), [], [t_Wus[i_]])
        sc.op(V, lambda: E[V].memset(Wds[i_][:], 0.0), [], [t_Wds[i_]])
    idxs = [[sb("idx%d_%d" % (i, j), [128, 1], I32) for j in range(NSUB)] for i in range(2)]
    t_idx = [[TR("idx%d_%d" % (i, j)) for j in range(NSUB)] for i in range(2)]
    Xg = [[sb("Xg%d_%d" % (i, j), [128, D], BF16) for j in range(NSUB)] for i in range(2)]
    t_Xg = [[TR("Xg%d_%d" % (i, j)) for j in range(NSUB)] for i in range(2)]
    XgT = [sb("XgT%d" % i, [128, 8, TS], BF16) for i in range(2)]; t_XgT = [TR("XgT%d" % i) for i in range(2)]
    sgl = [sb("sgl%d" % i, [128, TS]) for i in range(2)]; t_sgl = [TR("sgl%d" % i) for i in range(2)]
    AT = [sb("AT%d" % i, [128, 2, TS], BF16) for i in range(2)]; t_AT = [TR("AT%d" % i) for i in range(2)]
    NYB = 4
    Yt = [sb("Yt%d" % i, [128, D], BF16) for i in range(NYB)]; t_Yt = [TR("Yt%d" % i) for i in range(NYB)]

    def L2(t):
        b = t % 2
        for st in range(NSUB):
            sc.dma(P, None, None, reads=[t_widx, t_RTd], writes=[t_idx[b][st]], owner=t_idx[b][st],
                   fn=lambda: E[P].indirect_dma_start(out=idxs[b][st][:, :], out_offset=None, in_=RTd[:, :],
                                                      in_offset=bass.IndirectOffsetOnAxis(ap=ridx[:, t, st:st + 1], axis=0),
                                                      bounds_check=rb_rt, oob_is_err=False))
        for (wt, twt, src) in ((Wgs[b], t_Wgs[b], w_gate), (Wus[b], t_Wus[b], w_up), (Wds[b], t_Wds[b], w_down)):
            sc.dma(P, None, None, reads=[t_widx], writes=[twt], owner=twt,
                   fn=lambda: E[P].indirect_dma_start(out=wt[:, :], out_offset=None, in_=src[:, :],
                                                      in_offset=bass.IndirectOffsetOnAxis(ap=widx[:, t:t + 1], axis=0),
                                                      bounds_check=rb_w, oob_is_err=False))
        for st in range(NSUB):
            sc.dma(P, None, None, reads=[t_idx[b][st], t_H2d], writes=[t_Xg[b][st]], owner=t_Xg[b][st],
                   fn=lambda: E[P].indirect_dma_start(out=Xg[b][st][:, :], out_offset=None, in_=H2d[:, :],
                                                      in_offset=bass.IndirectOffsetOnAxis(ap=idxs[b][st][:, 0:1], axis=0),
                                                      bounds_check=rb_h2, oob_is_err=False))

    def C2(t):
        b = t % 2
        sc.op(V, lambda: E[V].tensor_copy(out=Wg[b][:].rearrange("p k n -> p (k n)"), in_=Wgs[b][:]), [t_Wgs[b]], [t_Wg[b]])
        sc.op(A, lambda: E[A].copy(out=Wu[b][:].rearrange("p k n -> p (k n)"), in_=Wus[b][:]), [t_Wus[b]], [t_Wu[b]])
        sc.op(V, lambda: E[V].tensor_copy(out=Wd[b][:].rearrange("p k n -> p (k n)"), in_=Wds[b][:]), [t_Wds[b]], [t_Wd[b]])

    def T2(t):
        b = t % 2
        for st in range(NSUB):
            tb = tp[st % 2]; ttb = t_tp[st % 2]

            def f():
                ins = None
                for kc in range(8):
                    ins = E[PE].transpose(out=tb[:, kc * 128:(kc + 1) * 128], in_=Xg[b][st][:, kc * 128:(kc + 1) * 128],
                                          identity=identb[:])
                return ins
            sc.op(PE, f, [t_Xg[b][st], t_identb], [ttb])
            if st % 2 == 0:
                sc.op(A, lambda: E[A].copy(out=XgT[b][:, :, st * 128:(st + 1) * 128], in_=tb[:].rearrange("p (k t) -> p k t", k=8)),
                      [ttb], [t_XgT[b]])
            else:
                sc.op(V, lambda: E[V].tensor_copy(out=XgT[b][:, :, st * 128:(st + 1) * 128],
                                                  in_=tb[:].rearrange("p (k t) -> p k t", k=8)), [ttb], [t_XgT[b]])

    def M2a(t):
        b = t % 2
        N = TS
        for fc in range(2):
            bg = 2 * fc; bu = 2 * fc + 1

            def fg():
                ins = None
                for kc in range(8):
                    ins = E[PE].matmul(pm[bg][:, 0:N], lhsT=Wg[b][:, kc, fc * 128:(fc + 1) * 128], rhs=XgT[b][:, kc, 0:N],
                                       start=(kc == 0), stop=(kc == 7))
                return ins

            def fu():
                ins = None
                for kc in range(8):
                    ins = E[PE].matmul(pm[bu][:, 0:N], lhsT=Wu[b][:, kc, fc * 128:(fc + 1) * 128], rhs=XgT[b][:, kc, 0:N],
                                       start=(kc == 0), stop=(kc == 7))
                return ins
            sc.op(PE, fg, [t_Wg[b], t_XgT[b]], [t_pm[bg]])
            sc.op(PE, fu, [t_Wu[b], t_XgT[b]], [t_pm[bu]])
            sc.op(A, lambda: E[A].activation(out=sgl[fc][:, 0:N], in_=pm[bg][:, 0:N], func=AF.Silu), [t_pm[bg]], [t_sgl[fc]])
            sc.op(V, lambda: E[V].tensor_tensor(out=AT[b][:, fc, 0:N], in0=pm[bu][:, 0:N], in1=sgl[fc][:, 0:N], op=ALU.mult),
                  [t_pm[bu], t_sgl[fc]], [t_AT[b]])

    ny = [0]

    def M2b(t):
        b = t % 2
        for st in range(NSUB):
            yb = ny[0] % NYB; ny[0] += 1
            for n in range(2):
                bank = 4 + n

                def fy():
                    ins = None
                    for fc in range(2):
                        ins = E[PE].matmul(pm[bank][:], lhsT=AT[b][:, fc, st * 128:(st + 1) * 128],
                                           rhs=Wd[b][:, fc, n * 512:(n + 1) * 512], start=(fc == 0), stop=(fc == 1))
                    return ins
                sc.op(PE, fy, [t_AT[b], t_Wd[b]], [t_pm[bank]])
                if n == 0:
                    sc.op(A, lambda: E[A].copy(out=Yt[yb][:, 0:512], in_=pm[bank][:]), [t_pm[bank]], [t_Yt[yb]])
                else:
                    sc.op(V, lambda: E[V].tensor_copy(out=Yt[yb][:, 512:1024], in_=pm[bank][:]), [t_pm[bank]], [t_Yt[yb]])
            r0 = t * TS + st * 128
            sc.dma(SP, Ysd[r0:r0 + 128, :], Yt[yb][:], reads=[t_Yt[yb]], writes=[t_Ysd], owner=t_Yt[yb])

    L2(0); C2(0); T2(0)
    for t in range(NTL):
        if t + 1 < NTL:
            L2(t + 1)
        M2a(t)
        if t + 1 < NTL:
            C2(t + 1)
        M2b(t)
        if t + 1 < NTL:
            T2(t + 1)

    new_phase()
    t_out = TR("out"); t_out.multi = True
    fgs = sb("fgs", [128, D]); t_fgs = TR("fgs")
    sc.dma(SP, fgs[:], fing, writes=[t_fgs], owner=t_fgs)
    NB3 = 4
    xm3 = [sb("xm3%d" % i, [128, D]) for i in range(NB3)]; t_xm3 = [TR("xm3%d" % i) for i in range(NB3)]
    y1 = [sb("y1%d" % i, [128, D], BF16) for i in range(NB3)]; t_y1 = [TR("y1%d" % i) for i in range(NB3)]
    y2 = [sb("y2%d" % i, [128, D], BF16) for i in range(NB3)]; t_y2 = [TR("y2%d" % i) for i in range(NB3)]
    ot = [sb("ot%d" % i, [128, D]) for i in range(2)]; t_ot = [TR("ot%d" % i) for i in range(2)]
    junk3 = sb("junk3", [128, D], BF16); t_junk3 = TR("junk3")
    sm3 = [sb("sm3_%d" % i, [128, 4]) for i in range(2)]; t_sm3 = [TR("sm3_%d" % i) for i in range(2)]

    def L3(t):
        b = t % NB3
        sc.dma(SP, xm3[b][:], Xmd[t * 128:(t + 1) * 128, :], reads=[t_Xmd], writes=[t_xm3[b]], owner=t_xm3[b])
        for (yy, tyy, dd) in ((y1[b], t_y1[b], dest1), (y2[b], t_y2[b], dest2)):
            sc.dma(P, None, None, reads=[t_route, t_Ysd], writes=[tyy], owner=tyy,
                   fn=lambda: E[P].indirect_dma_start(out=yy[:, :], out_offset=None, in_=Ysd[:, :],
                                                      in_offset=bass.IndirectOffsetOnAxis(ap=dd[:, t:t + 1], axis=0),
                                                      bounds_check=rb_ys, oob_is_err=False))

    def C3(t):
        b = t % NB3
        ob = t % 2
        s3 = sm3[ob]; ts3 = t_sm3[ob]
        sc.op(V, lambda: E[V].scalar_tensor_tensor(out=xm3[b][:], in0=y1[b][:], scalar=wgt1[:, t:t + 1], in1=xm3[b][:],
                                                   op0=ALU.mult, op1=ALU.add), [t_y1[b], t_xm3[b], t_route], [t_xm3[b]])
        sc.op(V, lambda: E[V].scalar_tensor_tensor(out=xm3[b][:], in0=y2[b][:], scalar=wgt2[:, t:t + 1], in1=xm3[b][:],
                                                   op0=ALU.mult, op1=ALU.add), [t_y2[b], t_xm3[b], t_route], [t_xm3[b]])
        sc.op(A, lambda: E[A].activation(out=junk3[:], in_=xm3[b][:], func=AF.Square, accum_out=s3[:, 0:1]),
              [t_xm3[b]], [t_junk3, ts3])
        sc.op(A, lambda: E[A].activation(out=s3[:, 1:2], in_=s3[:, 0:1], func=AF.Sqrt, scale=1.0 / D, bias=epsb[:, 0:1]),
              [ts3, t_eps], [ts3])
        sc.op(V, lambda: E[V].reciprocal(out=s3[:, 2:3], in_=s3[:, 1:2]), [ts3], [ts3])
        sc.op(V, lambda: E[V].scalar_tensor_tensor(out=ot[ob][:], in0=xm3[b][:], scalar=s3[:, 2:3], in1=fgs[:],
                                                   op0=ALU.mult, op1=ALU.mult), [t_xm3[b], ts3, t_fgs], [t_ot[ob]])
        sc.dma(SP, out[t * 128:(t + 1) * 128, :], ot[ob][:], reads=[t_ot[ob]], writes=[t_out], owner=t_ot[ob])

    for t in range(min(2, NT)):
        L3(t)
    for t in range(NT):
        if t + 2 < NT:
            L3(t + 2)
        C3(t)
    sc.wait_all(P, [t_out])
    barrier()
    return nc


def _prep_inputs(inp, b, S):
    f = np.float32

    def T8(v):
        return np.ascontiguousarray(v.reshape(8, 128).T).astype(f)

    def T4(v):
        return np.ascontiguousarray(v.reshape(4, 128).T).astype(f)
    d = {}
    d["x"] = np.ascontiguousarray(inp["x"][b][:S])
    d["w_in"] = inp["w_in"][0]
    d["w_out"] = inp["w_out"][0]
    d["wr"] = np.ascontiguousarray(np.concatenate([inp["w_r1"][0]] + [inp["w_r2"][0][g] for g in range(4)], axis=1))
    d["w_gate"] = np.ascontiguousarray(inp["w_gate"][0].reshape(32, 8, 128, 256).transpose(0, 2, 1, 3).reshape(32 * 128, 2048))
    d["w_up"] = np.ascontiguousarray(inp["w_up"][0].reshape(32, 8, 128, 256).transpose(0, 2, 1, 3).reshape(32 * 128, 2048))
    d["w_down"] = np.ascontiguousarray(inp["w_down"][0].reshape(32, 2, 128, 1024).transpose(0, 2, 1, 3).reshape(32 * 128, 2048))
    d["g1T"] = T8(inp["norm1_g"][0])
    d["g2T"] = T8(inp["norm2_g"][0])
    d["gconvT"] = T4(inp["out_g_conv"][0])
    d["gattT"] = T4(inp["out_g_att"][0])
    d["bdwT"] = T4(inp["b_dw"][0])
    d["lngT"] = T4(inp["conv_ln_g"][0])
    d["lnbT"] = T4(inp["conv_ln_b"][0])
    wd = inp["w_dw"][0]
    d["wdwT"] = np.ascontiguousarray(wd.reshape(31, 4, 128).transpose(2, 1, 0).reshape(128, 124))
    nb = np.zeros((128, 1), f)
    for q in range(4):
        nb[q * 32:q * 32 + 8, 0] = -inp["b_f"][0]
    d["negbf"] = nb
    d["fing"] = np.ascontiguousarray(np.broadcast_to(inp["final_g"][None, :], (128, 1024))).astype(f)
    rb = np.concatenate([inp["b_r1"][0], inp["b_r2"][0].reshape(-1)])
    d["g2bc"] = np.ascontiguousarray(np.broadcast_to(inp["norm2_g"][0][None, :], (128, 1024))).astype(f)
    d["rbias"] = np.ascontiguousarray(np.broadcast_to(rb[None, :], (128, 36))).astype(f)
    c = np.zeros((128, 640), f)
    c[:, 0:128] = np.eye(128)
    c[:, 128:256] = 1.0
    c[:, 256:384] = np.triu(np.ones((128, 128)), 1)
    c[:, 384:512] = np.tril(np.ones((128, 128)), -1) * -30000.0
    c[:, 512:640] = 1.0
    d["consts"] = c
    d["ebase"] = np.ascontiguousarray(np.broadcast_to(np.arange(32)[None, :], (128, 32))).astype(f)
    d["thr256"] = np.ascontiguousarray(np.broadcast_to((np.arange(33) * TS)[None, :], (128, 33))).astype(f)
    NTL = NE * CAP // TS
    d["tst256"] = np.ascontiguousarray(np.broadcast_to((np.arange(NTL) * TS)[None, :], (128, NTL))).astype(f)
    d["pcol"] = np.arange(128).reshape(128, 1).astype(f)
    NT = S // 128
    d["iota_tok"] = (np.arange(NT)[None, :] * 128 + np.arange(128)[:, None]).astype(np.int32)
    return d


def kernel(**inputs):
    inp = {k: np.asarray(v) for k, v in inputs.items()}
    B, S, _ = inp["x"].shape
    nc = build(S)
    in_maps = [_prep_inputs(inp, b, S) for b in range(B)]
    res = run_bass_kernel_spmd(nc, in_maps, core_ids=list(range(B)))
    return np.stack([np.asarray(r["out"]) for r in res.results], axis=0).astype(np.float32)
```

```python
import numpy as np
import ml_dtypes
import concourse.bass as bass
import concourse.mybir as mybir
from concourse.bass_utils import run_bass_kernel_spmd

F32 = mybir.dt.float32
BF16 = mybir.dt.bfloat16
I32 = mybir.dt.int32
AF = mybir.ActivationFunctionType
ALU = mybir.AluOpType
AX = mybir.AxisListType

D = 1024
DIN = 2568
NH = 8
HD = 64
CW = 31
NE = 32
DE = 256
CAP = 768
TS = 256
EPS = 1e-6
MT = 512


class TR:
    __slots__ = ("name", "w", "r", "dsem", "dcnt", "multi")

    all = []

    def __init__(self, name):
        TR.all.append(self)
        self.name = name
        self.w = {}
        self.r = {}
        self.dsem = {}
        self.dcnt = {}
        self.multi = False


class Sched:
    def __init__(self, nc):
        self.nc = nc
        self.eng = {"pe": nc.tensor, "act": nc.scalar, "dve": nc.vector, "pool": nc.gpsimd, "sp": nc.sync}
        self.sem = {k: nc.alloc_semaphore("sem_" + k) for k in self.eng}
        self.cnt = {k: 0 for k in self.eng}
        self.waited = {k: {} for k in self.eng}
        self.nsem = 0
        self.pool = {"sw": [], "hw": []}

    def _deps(self, e, reads, writes):
        deps = {}
        for t in list(reads) + list(writes):
            for k, (s, v) in t.w.items():
                if deps.get(k, (None, 0))[1] < v:
                    deps[k] = (s, v)
        for t in writes:
            for k, (s, v) in t.r.items():
                if deps.get(k, (None, 0))[1] < v:
                    deps[k] = (s, v)
        wd = self.waited[e]
        own = id(self.sem[e])
        for k, (s, v) in deps.items():
            if e == "pe" and k == own:
                continue
            if wd.get(k, 0) < v:
                self.eng[e].wait_ge(s, v)
                wd[k] = v

    def op(self, e, fn, reads=(), writes=()):
        self._deps(e, reads, writes)
        ins = fn()
        s = self.sem[e]
        ins.then_inc(s, 1)
        self.cnt[e] += 1
        v = self.cnt[e]
        k = id(s)
        for t in writes:
            t.w = {k: (s, v)}
            t.r = {}
        for t in reads:
            t.r[k] = (s, v)

    def dma(self, q, out, in_, reads=(), writes=(), owner=None, n=1, fn=None):
        self._deps(q, reads, writes)
        kind = "sw" if q == "pool" else "hw"
        if kind not in owner.dsem:
            if self.pool[kind]:
                owner.dsem[kind], owner.dcnt[kind] = self.pool[kind].pop()
            else:
                owner.dsem[kind] = self.nc.alloc_semaphore("d%s_%d" % (kind, self.nsem))
                owner.dcnt[kind] = 0
                self.nsem += 1
        if fn is None:
            ins = self.eng[q].dma_start(out=out, in_=in_)
        else:
            ins = fn()
        sem = owner.dsem[kind]
        ins.then_inc(sem, 16)
        owner.dcnt[kind] += 16
        k = id(sem)
        ent = (sem, owner.dcnt[kind])
        for t in writes:
            if not t.multi:
                t.w = {}
            t.w[k] = ent
            t.r = {}
        for t in reads:
            t.r[k] = ent

    def wait_all(self, e, tracked):
        self._deps(e, tracked, tracked)


def build(S, debug=None):
    NM = S // MT
    NT = S // 128
    NSLOT = NE * CAP
    nc = bass.Bass("TRN2", target_bir_lowering=False)
    TR.all = []
    sc = Sched(nc)

    def din(name, shape, dt=F32):
        return nc.dram_tensor(name, list(shape), dt, kind="ExternalInput").ap()

    def dscr(name, shape, dt):
        return nc.dram_tensor(name, list(shape), dt, kind=("ExternalOutput" if debug else "Internal")).ap()

    x = din("x", [S, D])
    w_in = din("w_in", [D, DIN])
    w_out = din("w_out", [D, D])
    wr = din("wr", [D, 36])
    w_gate = din("w_gate", [NE * 128, 8 * DE])
    w_up = din("w_up", [NE * 128, 8 * DE])
    w_down = din("w_down", [NE * 128, 2 * D])
    g1T = din("g1T", [128, 8])
    g2T = din("g2T", [128, 8])
    gconvT = din("gconvT", [128, 4])
    gattT = din("gattT", [128, 4])
    bdwT = din("bdwT", [128, 4])
    lngT = din("lngT", [128, 4])
    lnbT = din("lnbT", [128, 4])
    wdwT = din("wdwT", [128, 4 * CW])
    negbf = din("negbf", [128, 1])
    fing = din("fing", [128, D])
    g2bc = din("g2bc", [128, D])
    rbias = din("rbias", [128, 36])
    consts = din("consts", [128, 5 * 128])
    ebase = din("ebase", [128, NE])
    thr256 = din("thr256", [128, 33])
    tst256 = din("tst256", [128, NSLOT // TS])
    pcol = din("pcol", [128, 1])
    iota_tok = din("iota_tok", [128, NT], I32)
    out = nc.dram_tensor("out", [S, D], F32, kind="ExternalOutput").ap()

    Qd = dscr("Qd", [4, NM, 128, MT], BF16)
    Kd = dscr("Kd", [4, NM, 128, MT], BF16)
    Vd = dscr("Vd", [4, NM, 128, 4, 130], BF16)
    Ad = dscr("Ad", [NM, 128, MT], BF16)
    Mcd = dscr("Mcd", [NM, 128, 4, MT], BF16)
    Yad = dscr("Yad", [NM, 64, 8, MT], BF16)
    Xmd = dscr("Xmd", [S, D], F32)
    H2d = dscr("H2d", [S + 128, D], BF16)
    RTd = dscr("RTd", [NE * S + 128, 1], I32)
    Ysd = dscr("Ysd", [NSLOT, D], BF16)


    from contextlib import ExitStack
    stk = [ExitStack()]

    def sb(name, shape, dt=F32):
        return stk[0].enter_context(nc.sbuf_tensor(name, list(shape), dt))

    dma_owners = []

    def barrier():
        ents = [(sc.sem[k], sc.cnt[k]) for k in sc.eng if sc.cnt[k] > 0]
        for t in TR.all:
            for kd in t.dsem:
                if t.dcnt[kd] > 0:
                    ents.append((t.dsem[kd], t.dcnt[kd]))
        for e in sc.eng:
            wd = sc.waited[e]
            for (sm, v) in ents:
                if sm is sc.sem[e]:
                    continue
                if wd.get(id(sm), 0) < v:
                    sc.eng[e].wait_ge(sm, v)
                    wd[id(sm)] = v

    def new_phase():
        barrier()
        for t in TR.all:
            for kd in list(t.dsem):
                sc.pool[kd].append((t.dsem[kd], t.dcnt[kd]))
            t.dsem = {}
            t.dcnt = {}
            t.w = {}
            t.r = {}
        stk[0].close()
        stk[0] = ExitStack()

    psn = [0]

    def ps(name, shape, dt=F32):
        psn[0] += 1
        return stk[0].enter_context(nc.psum_tensor("%s_%d" % (name, psn[0]), list(shape), dt))

    def mkpsum(ntp, npm):
        tp_ = [ps("tp%d" % i, [128, 1024], BF16) for i in range(ntp)]
        ttp_ = [TR("tp%d" % i) for i in range(ntp)]
        pm_ = [ps("pm%d" % i, [128, 512]) for i in range(npm)]
        tpm_ = [TR("pm%d" % i) for i in range(npm)]
        return tp_, ttp_, pm_, tpm_

    V, A, P, PE, SP = "dve", "act", "pool", "pe", "sp"
    E = sc.eng
    rb_rt = E[P].to_reg(NE * S + 127)
    rb_h2 = E[P].to_reg(S + 127)
    rb_ys = E[P].to_reg(NSLOT - 1)

    def E_pool_to_reg(v):
        return E[P].to_reg(v)

    cst = sb("cst", [128, 5 * 128]); t_cst = TR("cst")
    sc.dma(SP, cst[:], consts, writes=[t_cst], owner=t_cst)
    ident_f = cst[:, 0:128]
    ones_f = cst[:, 128:256]
    identb = sb("identb", [128, 128], BF16); t_identb = TR("identb")
    onesb = sb("onesb", [128, 128], BF16)
    lstrb = sb("lstrb", [128, 128], BF16)
    negmb = sb("negmb", [128, 128], BF16)
    sc.op(V, lambda: E[V].tensor_copy(out=identb[:], in_=cst[:, 0:128]), [t_cst], [t_identb])
    sc.op(V, lambda: E[V].tensor_copy(out=onesb[:], in_=cst[:, 128:256]), [t_cst], [t_identb])
    sc.op(V, lambda: E[V].tensor_copy(out=lstrb[:], in_=cst[:, 256:384]), [t_cst], [t_identb])
    sc.op(V, lambda: E[V].tensor_copy(out=negmb[:], in_=cst[:, 384:512]), [t_cst], [t_identb])
    ones512 = sb("ones512", [128, MT]); t_ones512 = TR("ones512")
    sc.op(P, lambda: E[P].memset(ones512[:], 1.0), [], [t_ones512])
    epsb = sb("epsb", [128, 1]); t_eps = TR("epsb")
    sc.op(P, lambda: E[P].memset(epsb[:], EPS), [], [t_eps])
    t_c = TR("consts_all")

    def load_small(name, ap, shape, dt=F32):
        t = sb(name, shape, dt)
        sc.dma(SP, t[:], ap, writes=[t_c], owner=t_c)
        return t

    t_c.multi = True
    g1s = load_small("g1s", g1T, [128, 8])
    g2s = load_small("g2s", g2T, [128, 8])
    gconvs = load_small("gconvs", gconvT, [128, 4])
    gatts = load_small("gatts", gattT, [128, 4])
    bdws = load_small("bdws", bdwT, [128, 4])
    lngs = load_small("lngs", lngT, [128, 4])
    lnbs = load_small("lnbs", lnbT, [128, 4])
    wdws = load_small("wdws", wdwT, [128, 4 * CW])
    negbs = load_small("negbs", negbf, [128, 1])
    rbs = load_small("rbs", rbias, [128, 36])
    ebs = load_small("ebs", ebase, [128, NE])
    thrs = load_small("thrs", thr256, [128, 33])
    tsts = load_small("tsts", tst256, [128, NSLOT // TS])
    pcols = load_small("pcols", pcol, [128, 1])
    iotas = load_small("iotas", iota_tok, [128, NT], I32)

    biasT = sb("biasT", [128, NT, 8]); t_biasT = TR("biasT")
    dest1 = sb("dest1", [128, NT], I32); dest2 = sb("dest2", [128, NT], I32)
    wgt1 = sb("wgt1", [128, NT]); wgt2 = sb("wgt2", [128, NT])
    t_route = TR("route")
    NTL = NSLOT // TS
    widx = sb("widx", [128, NTL], I32); t_widx = TR("widx")
    ridx = sb("ridx", [128, NTL, TS // 128], I32)
    rb_w = E_pool_to_reg(NE * 128 - 1)

    selB = sb("selB", [128, 8, 128], BF16); t_sel = TR("selB")
    selc = sb("selc", [128, 8]); t_selc = TR("selc")
    sc.op(V, lambda: E[V].tensor_tensor(out=selc[:], in0=cst[:, 0:8], in1=cst[:, 32:40], op=ALU.add), [t_cst], [t_selc])
    for h in range(8):
        sc.op(V, lambda: E[V].tensor_scalar(out=selB[:, h, :], in0=cst[:, 128:256], scalar1=selc[:, h:h + 1], scalar2=None,
                                            op0=ALU.mult), [t_selc, t_cst], [t_sel])

    e64 = sb("e64", [65, 64]); t_e64 = TR("e64")
    sc.op(V, lambda: E[V].memset(e64[:], 0.0), [], [t_e64])
    sc.op(V, lambda: E[V].memset(e64[64:65, :], 1.0), [t_e64], [t_e64])
    persist = stk[0]
    stk[0] = ExitStack()
    WinB = sb("WinB", [128, 8, DIN], BF16); t_win = TR("WinB")
    WfB = sb("WfB", [128, 8, 128], BF16); t_wf = TR("WfB")
    stg = [sb("stg%d" % i, [128, DIN]) for i in range(1)] * 2
    t_stg = [TR("stg%d" % i) for i in range(1)] * 2
    sc.op(P, lambda: E[P].memset(WfB[:], 0.0), [], [t_wf])
    w_in_v = w_in.rearrange("(kc p) n -> p kc n", p=128)
    for kc in range(8):
        b = kc % 2
        sc.dma(SP, stg[b][:], w_in_v[:, kc, :], writes=[t_stg[b]], owner=t_stg[b])
        eng = A if kc % 2 == 0 else V
        if eng == A:
            sc.op(A, lambda: E[A].activation(out=WinB[:, kc, :], in_=stg[b][:], func=AF.Copy, scale=g1s[:, kc:kc + 1]),
                  [t_stg[b], t_c], [t_win])
        else:
            sc.op(V, lambda: E[V].tensor_scalar(out=WinB[:, kc, :], in0=stg[b][:], scalar1=g1s[:, kc:kc + 1], scalar2=None,
                                                op0=ALU.mult), [t_stg[b], t_c], [t_win])
    for q in range(4):
        sc.op(P, lambda: E[P].tensor_copy(out=WfB[:, :, q * 32:q * 32 + 8], in_=WinB[:, :, 2560:2568]), [t_win], [t_wf])

    diagW = sb("diagW", [128, CW, 4, 128], BF16); t_diag = TR("diagW")
    for k in range(CW):
        for c in range(4):
            eng = V if (k + c) % 2 == 0 else P
            sc.op(eng, lambda: E[eng].tensor_scalar(out=diagW[:, k, c, :], in0=cst[:, 0:128],
                                                    scalar1=wdws[:, c * CW + k:c * CW + k + 1], scalar2=None, op0=ALU.mult),
                  [t_cst, t_c], [t_diag])

    tp, t_tp, pm, t_pm = mkpsum(2, 6)

    xs = [sb("xs%d" % i, [128, 4, D]) for i in range(2)]; t_xs = [TR("xs%d" % i) for i in range(2)]
    junk = sb("junk", [128, D], BF16); t_junk = TR("junk")
    ssq = sb("ssq", [128, 4]); t_ssq = TR("ssq")
    rstd = sb("rstd", [128, 4]); t_rstd = TR("rstd")
    hb = sb("hb", [128, 4, D], BF16); t_hb = TR("hb")
    hT = sb("hT", [128, 8, MT], BF16); t_hT = TR("hT")
    aT = [sb("aT%d" % i, [128, 4, 30 + MT], BF16) for i in range(2)]; t_aT = [TR("aT%d" % i) for i in range(2)]
    sg = sb("sg", [128, MT]); t_sg = TR("sg")
    QT = sb("QT", [128, 4, MT], BF16); t_QT = TR("QT")
    KT = sb("KT", [128, 4, MT], BF16); t_KT = TR("KT")
    Vt = sb("Vt", [128, 4, 8, 65], BF16); t_Vt = TR("Vt")
    ef = sb("ef", [128, MT]); t_ef = TR("ef")
    spf = sb("spf", [128, MT]); t_spf = TR("spf")
    cum = sb("cum", [128, MT]); t_cum = TR("cum")
    carry = sb("carry", [128, 1]); t_carry = TR("carry")
    hiall = sb("hiall", [128, MT], BF16); t_hiall = TR("hiall")
    arow = sb("arow", [128, MT], BF16); t_arow = TR("arow")
    uu = sb("uu", [128, 4, MT]); t_uu = TR("uu")
    usq = sb("usq", [128, 4, MT], BF16); t_usq = TR("usq")
    m1 = sb("m1", [128, MT]); t_m1 = TR("m1")
    rs1 = sb("rs1", [128, MT]); t_rs1 = TR("rs1")
    mixc = sb("mixc", [128, 4, MT], BF16); t_mixc = TR("mixc")
    t_Qd = TR("Qd"); t_Kd = TR("Kd"); t_Vd = TR("Vd"); t_Ad = TR("Ad"); t_Mcd = TR("Mcd")
    for t in (t_Qd, t_Kd, t_Vd, t_Ad, t_Mcd):
        t.multi = True

    sc.op(P, lambda: E[P].memset(Vt[:], 1.0), [], [t_Vt])
    sc.op(P, lambda: E[P].memset(carry[:], 0.0), [], [t_carry])
    sc.op(P, lambda: E[P].memset(aT[0][:], 0.0), [], [t_aT[0]])
    sc.op(P, lambda: E[P].memset(aT[1][:], 0.0), [], [t_aT[1]])

    x_v = x.rearrange("(m s p) d -> m p s d", p=128, s=4)

    def inproj(bank, tbank, col0, M=128, lhs_src=None):
        def f():
            ins = None
            for kc in range(8):
                lhsT = WinB[:, kc, col0:col0 + M] if lhs_src is None else lhs_src[:, kc, :]
                ins = E[PE].matmul(pm[bank][0:M, :], lhsT=lhsT, rhs=hT[:, kc, :], start=(kc == 0), stop=(kc == 7))
            return ins
        sc.op(PE, f, [t_win, t_wf, t_hT], [tbank])

    def ph_A0(m):
        xb = xs[m % 2]; txb = t_xs[m % 2]
        sc.dma(SP, xb[:], x_v[m], writes=[txb], owner=txb)
        for s in range(4):
            sc.op(A, lambda: E[A].activation(out=junk[:], in_=xb[:, s, :], func=AF.Square, accum_out=ssq[:, s:s + 1]),
                  [txb], [t_junk, t_ssq])
        sc.op(A, lambda: E[A].activation(out=rstd[:], in_=ssq[:], func=AF.Sqrt, scale=1.0 / D, bias=epsb[:, 0:1]),
              [t_ssq, t_eps], [t_rstd])
        sc.op(V, lambda: E[V].reciprocal(out=rstd[:], in_=rstd[:]), [t_rstd], [t_rstd])
        for s in range(4):
            sc.op(A, lambda: E[A].activation(out=hb[:, s, :], in_=xb[:, s, :], func=AF.Copy, scale=rstd[:, s:s + 1]),
                  [txb, t_rstd], [t_hb])

    def ph_A1(m):
        xb = xs[m % 2]; txb = t_xs[m % 2]
        ab = aT[m % 2]; tab = t_aT[m % 2]
        abp = aT[(m + 1) % 2]; tabp = t_aT[(m + 1) % 2]
        for s in range(4):
            tb = tp[s % 2]; ttb = t_tp[s % 2]

            def f():
                ins = None
                for kc in range(8):
                    ins = E[PE].transpose(out=tb[:, kc * 128:(kc + 1) * 128], in_=hb[:, s, kc * 128:(kc + 1) * 128],
                                          identity=identb[:])
                return ins
            sc.op(PE, f, [t_hb, t_identb], [ttb])
            eng = A if s % 2 == 0 else V
            if eng == A:
                sc.op(A, lambda: E[A].copy(out=hT[:, :, s * 128:(s + 1) * 128], in_=tb[:].rearrange("p (k t) -> p k t", k=8)),
                      [ttb], [t_hT])
            else:
                sc.op(V, lambda: E[V].tensor_copy(out=hT[:, :, s * 128:(s + 1) * 128],
                                                  in_=tb[:].rearrange("p (k t) -> p k t", k=8)), [ttb], [t_hT])
        if m > 0:
            sc.op(P, lambda: E[P].tensor_copy(out=ab[:, :, 0:30], in_=abp[:, :, MT:MT + 30]), [tabp], [tab])
        for c in range(4):
            inproj(0, t_pm[0], 512 + c * 128)
            sc.op(A, lambda: E[A].activation(out=sg[:], in_=pm[0][:], func=AF.Sigmoid), [t_pm[0]], [t_sg])
            inproj(1, t_pm[1], c * 128)
            sc.op(V, lambda: E[V].tensor_tensor(out=ab[:, c, 30:30 + MT], in0=pm[1][:], in1=sg[:], op=ALU.mult),
                  [t_pm[1], t_sg], [tab])
        for p in range(4):
            inproj(0, t_pm[0], 1024 + p * 128)
            sc.op(A, lambda: E[A].copy(out=QT[:, p, :], in_=pm[0][:]), [t_pm[0]], [t_QT])
            inproj(1, t_pm[1], 1536 + p * 128)
            sc.op(V, lambda: E[V].tensor_copy(out=KT[:, p, :], in_=pm[1][:]), [t_pm[1]], [t_KT])
        sc.dma(P, Qd[:, m].rearrange("a p t -> p a t"), QT[:], reads=[t_QT], writes=[t_Qd], owner=t_QT)
        sc.dma(P, Kd[:, m].rearrange("a p t -> p a t"), KT[:], reads=[t_KT], writes=[t_Kd], owner=t_KT)
        for s in range(4):
            bank = (s % 2)

            def f():
                ins = None
                for kc in range(8):
                    ins = E[PE].matmul(pm[bank][:], lhsT=hT[:, kc, s * 128:(s + 1) * 128], rhs=WinB[:, kc, 2048:2560],
                                       start=(kc == 0), stop=(kc == 7))
                return ins
            sc.op(PE, f, [t_win, t_hT], [t_pm[bank]])
            eng = A if s % 2 == 0 else V
            src = pm[bank][:].rearrange("p (h d) -> p h d", h=8)
            if eng == A:
                sc.op(A, lambda: E[A].copy(out=Vt[:, s, :, 0:64], in_=src), [t_pm[bank]], [t_Vt])
            else:
                sc.op(V, lambda: E[V].tensor_copy(out=Vt[:, s, :, 0:64], in_=src), [t_pm[bank]], [t_Vt])
        for a in range(4):
            sc.dma(P, Vd[a, m], Vt[:, :, 2 * a:2 * a + 2, :].rearrange("p s h c -> p s (h c)"),
                   reads=[t_Vt], writes=[t_Vd], owner=t_Vt)
        inproj(0, t_pm[0], 0, lhs_src=WfB)
        sc.op(A, lambda: E[A].activation(out=ef[:], in_=pm[0][:], func=AF.Exp, scale=-1.0, bias=negbs[:, 0:1]),
              [t_pm[0], t_c], [t_ef])
        sc.op(A, lambda: E[A].activation(out=spf[:], in_=ef[:], func=AF.Ln, bias=1.0), [t_ef], [t_spf])
        sc.op(V, lambda: E[V].tensor_tensor_scan(out=cum[:], data0=ones512[:], data1=spf[:], initial=carry[:, 0:1],
                                                 op0=ALU.mult, op1=ALU.subtract), [t_spf, t_carry, t_ones512], [t_cum])
        sc.op(V, lambda: E[V].tensor_copy(out=carry[:], in_=cum[:, MT - 1:MT]), [t_cum], [t_carry])
        sc.op(V, lambda: E[V].tensor_scalar(out=hiall[:], in0=cum[:], scalar1=8.0, scalar2=None, op0=ALU.mult), [t_cum], [t_hiall])
        sc.op(P, lambda: E[P].tensor_copy(out=arow[:], in_=hiall[:]), [t_hiall], [t_arow])
        for q in (1, 3):
            sc.op(V, lambda: E[V].scalar_tensor_tensor(out=arow[q * 32:q * 32 + 32, :], in0=cum[q * 32:q * 32 + 32, :], scalar=8.0,
                                                       in1=hiall[q * 32:q * 32 + 32, :], op0=ALU.mult, op1=ALU.subtract),
                  [t_cum, t_hiall, t_arow], [t_arow])
        sc.dma(P, Ad[m], arow[:], reads=[t_arow], writes=[t_Ad], owner=t_arow)
        for s in range(4):
            sc.op(PE, lambda: E[PE].transpose(out=pm[1][:, 0:8], in_=cum[0:8, s * 128:(s + 1) * 128], identity=cst[0:8, 0:8]),
                  [t_cum, t_cst], [t_pm[1]])
            sc.op(V, lambda: E[V].tensor_scalar(out=biasT[:, m * 4 + s, :], in0=pm[1][:, 0:8], scalar1=-1.0, scalar2=None,
                                                op0=ALU.mult), [t_pm[1]], [t_biasT])

    def stat(bank, src, tsrc):
        ones_ = onesb[:] if src is usq else cst[:, 128:256]

        def f():
            ins = None
            for c in range(4):
                ins = E[PE].matmul(pm[bank][:], lhsT=ones_, rhs=src[:, c, :], start=(c == 0), stop=(c == 3))
            return ins
        sc.op(PE, f, [t_cst, t_identb, tsrc], [t_pm[bank]])

    def ph_B1(m):
        ab = aT[m % 2]; tab = t_aT[m % 2]
        for c in range(4):
            bank = 2 + (c % 2)

            def f():
                ins = None
                for k in range(CW):
                    ins = E[PE].matmul(pm[bank][:], lhsT=diagW[:, k, c, :], rhs=ab[:, c, k:k + MT], start=(k == 0),
                                       stop=(k == CW - 1))
                return ins
            sc.op(PE, f, [t_diag, tab], [t_pm[bank]])
            sc.op(A, lambda: E[A].activation(out=uu[:, c, :], in_=pm[bank][:], func=AF.Identity, bias=bdws[:, c:c + 1]),
                  [t_pm[bank], t_c], [t_uu])
            sc.op(A, lambda: E[A].activation(out=usq[:, c, :], in_=pm[bank][:], func=AF.Square, bias=bdws[:, c:c + 1]),
                  [t_pm[bank], t_c], [t_usq])

        stat(4, uu, t_uu)
        stat(5, usq, t_usq)

    def ph_B2(m):
        sc.op(V, lambda: E[V].tensor_scalar(out=m1[:], in0=pm[4][:], scalar1=1.0 / 512, scalar2=None, op0=ALU.mult), [t_pm[4]], [t_m1])
        sc.op(V, lambda: E[V].tensor_tensor(out=rs1[:], in0=m1[:], in1=m1[:], op=ALU.mult), [t_m1], [t_rs1])
        sc.op(V, lambda: E[V].scalar_tensor_tensor(out=rs1[:], in0=pm[5][:], scalar=1.0 / 512, in1=rs1[:], op0=ALU.mult,
                                                   op1=ALU.subtract), [t_pm[5], t_rs1], [t_rs1])
        sc.op(A, lambda: E[A].activation(out=rs1[:], in_=rs1[:], func=AF.Ln, bias=epsb[:, 0:1]), [t_rs1, t_eps], [t_rs1])
        sc.op(A, lambda: E[A].activation(out=rs1[:], in_=rs1[:], func=AF.Exp, scale=-0.5), [t_rs1], [t_rs1])
        for c in range(4):
            eng = V
            sc.op(eng, lambda: E[eng].tensor_tensor(out=uu[:, c, :], in0=uu[:, c, :], in1=m1[:], op=ALU.subtract), [t_uu, t_m1], [t_uu])
            sc.op(eng, lambda: E[eng].tensor_tensor(out=uu[:, c, :], in0=uu[:, c, :], in1=rs1[:], op=ALU.mult), [t_uu, t_rs1], [t_uu])
            sc.op(A, lambda: E[A].activation(out=uu[:, c, :], in_=uu[:, c, :], func=AF.Silu, scale=lngs[:, c:c + 1],
                                             bias=lnbs[:, c:c + 1]), [t_uu, t_c], [t_uu])
            sc.op(A, lambda: E[A].activation(out=usq[:, c, :], in_=uu[:, c, :], func=AF.Square), [t_uu], [t_usq])
        stat(4, usq, t_usq)
        sc.op(A, lambda: E[A].activation(out=rs1[:], in_=pm[4][:], func=AF.Ln, scale=1.0 / 512, bias=epsb[:, 0:1]),
              [t_pm[4], t_eps], [t_rs1])
        sc.op(A, lambda: E[A].activation(out=rs1[:], in_=rs1[:], func=AF.Exp, scale=-0.5), [t_rs1], [t_rs1])
        for c in range(4):
            eng = V
            sc.op(eng, lambda: E[eng].tensor_tensor(out=mixc[:, c, :], in0=uu[:, c, :], in1=rs1[:], op=ALU.mult),
                  [t_uu, t_rs1], [t_mixc])
        sc.dma(P, Mcd[m], mixc[:], reads=[t_mixc], writes=[t_Mcd], owner=t_mixc)


    ph_A0(0)
    ph_A1(0)
    for m in range(NM):
        if m + 1 < NM:
            ph_A0(m + 1)
        ph_B1(m)
        if m + 1 < NM:
            ph_A1(m + 1)
        ph_B2(m)

    if debug == "p1a":
        sc.wait_all(P, [t_Qd, t_Kd, t_Vd, t_Ad, t_Mcd, t_biasT])
        return nc

    new_phase()
    tp, t_tp, pm, t_pm = mkpsum(0, 8)
    NSB = 3
    Qs = [[sb("Qs%d_%d" % (i, j), [128, MT], BF16) for j in range(2)] for i in range(2)]
    t_Qs = [[TR("Qs%d_%d" % (i, j)) for j in range(2)] for i in range(2)]
    NKB = 3
    Kc = [[sb("Kc%d_%d" % (i, j), [128, MT], BF16) for j in range(2)] for i in range(NKB)]
    t_Kc = [[TR("Kc%d_%d" % (i, j)) for j in range(2)] for i in range(NKB)]
    Vc = [sb("Vc%d" % i, [128, 4, 2, 128], BF16) for i in range(NKB)]; t_Vc = [TR("Vc%d" % i) for i in range(NKB)]
    NPB = 4
    Pt = [sb("Pt%d" % i, [128, MT], BF16) for i in range(NPB)]; t_Pt = [TR("Pt%d" % i) for i in range(NPB)]
    Osb = [sb("Osb%d" % i, [65, MT]) for i in range(2)]; t_Osb = [TR("Osb%d" % i) for i in range(2)]
    Ya = [sb("Ya%d" % i, [64, 2, MT], BF16) for i in range(2)]; t_Ya = [TR("Ya%d" % i) for i in range(2)]
    for i_ in range(2):
        for j_ in range(2):
            sc.op(P, lambda: E[P].memset(Qs[i_][j_][:], 0.0), [], [t_Qs[i_][j_]])
            t_Qs[i_][j_].multi = True
    for i_ in range(NKB):
        for j_ in range(2):
            sc.op(P, lambda: E[P].memset(Kc[i_][j_][:], 0.0), [], [t_Kc[i_][j_]])
            sc.op(P, lambda: E[P].memset(Kc[i_][j_][64:66, :], 1.0), [t_Kc[i_][j_]], [t_Kc[i_][j_]])
        sc.op(P, lambda: E[P].memset(Vc[i_][:], 0.0), [], [t_Vc[i_]])
        t_Vc[i_].multi = True
    t_Yad = TR("Yad"); t_Yad.multi = True

    steps = []
    grp = 0
    for a in range(4):
        for i in range(NM):
            for c in range(i + 1):
                for hh in range(2):
                    for jj in range(4):
                        steps.append(dict(a=a, i=i, c=c, hh=hh, jj=jj, grp=grp,
                                          first=(c == 0 and jj == 0), last=(c == i and jj == 3)))
            grp += 1
    nld = 0
    cur_q = {}
    cur_c = {}

    def emit_S(n):
        nonlocal nld
        st = steps[n]
        a, i, c, hh, jj = st["a"], st["i"], st["c"], st["hh"], st["jj"]
        g = st["grp"]
        if g not in cur_q:
            qb = g % 2
            for h2 in range(2):
                h_ = 2 * a + h2
                tq = t_Qs[qb][h2]
                sc.dma(SP, Qs[qb][h2][0:64, :], Qd[a, i, h2 * 64:(h2 + 1) * 64, :], reads=[t_Qd], writes=[tq], owner=tq)
                sc.dma(SP, Qs[qb][h2][64:65, :], Ad[i, h_:h_ + 1, :], reads=[t_Ad], writes=[tq], owner=tq)
                sc.dma(SP, Qs[qb][h2][65:66, :], Ad[i, 32 + h_:33 + h_, :], reads=[t_Ad], writes=[tq], owner=tq)
            cur_q[g] = qb
        if (g, c) not in cur_c:
            kb = nld % NKB
            nld += 1
            for h2 in range(2):
                sc.dma(SP, Kc[kb][h2][0:64, :], Kd[a, c, h2 * 64:(h2 + 1) * 64, :], reads=[t_Kd], writes=[t_Kc[kb][h2]],
                       owner=t_Kc[kb][h2])
                sc.dma(SP, Vc[kb][:, :, h2, 0:65], Vd[a, c, :, :, h2 * 65:(h2 + 1) * 65], reads=[t_Vd], writes=[t_Vc[kb]],
                       owner=t_Vc[kb])
            cur_c[(g, c)] = kb
        qb = cur_q[g]; kb = cur_c[(g, c)]
        st["qb"] = qb; st["kb"] = kb
        h = 2 * a + hh
        diag = (c == i)
        q0 = 128 * jj if diag else 0
        st["q0"] = q0; st["h"] = h
        bank = n % NSB
        st["bank"] = bank

        def f():
            ins = E[PE].matmul(pm[bank][:, q0:], lhsT=Kc[kb][hh][:, jj * 128:(jj + 1) * 128], rhs=Qs[qb][hh][:, q0:],
                               start=True, stop=not diag)
            if diag:
                ins = E[PE].matmul(pm[bank][:, q0:q0 + 128], lhsT=identb[:], rhs=negmb[:], start=False, stop=True)
            return ins
        sc.op(PE, f, [t_Kc[kb][hh], t_Qs[qb][hh], t_identb], [t_pm[bank]])

    def emit_exp(n):
        st = steps[n]
        bank = st["bank"]; q0 = st["q0"]; h = st["h"]
        j = 4 * st["c"] + st["jj"]
        pb = n % NPB
        sc.op(A, lambda: E[A].activation(out=Pt[pb][:, q0:], in_=pm[bank][:, q0:], func=AF.Exp, scale=0.125,
                                         bias=biasT[:, j, h:h + 1]), [t_pm[bank], t_biasT], [t_Pt[pb]])

    def emit_PV(n):
        st = steps[n]
        hh = st["hh"]; q0 = st["q0"]; kb = st["kb"]; jj = st["jj"]
        pb = n % NPB
        ob = 3 + 2 * (st["grp"] % 2) + hh
        sc.op(PE, lambda: E[PE].matmul(pm[ob][:, q0:], lhsT=Vc[kb][:, jj, hh, :], rhs=Pt[pb][:, q0:],
                                       start=st["first"], stop=st["last"]), [t_Vc[kb], t_Pt[pb]], [t_pm[ob]])
        if st["last"] and hh == 1:
            a, i = st["a"], st["i"]
            yb = st["grp"] % 2
            for h2 in range(2):
                obk = 3 + 2 * (st["grp"] % 2) + h2
                sc.op(V, lambda: E[V].tensor_copy(out=Osb[h2][:], in_=pm[obk][0:65, :]), [t_pm[obk]], [t_Osb[h2]])
                sc.op(V, lambda: E[V].reciprocal(out=Osb[h2][64:65, :], in_=Osb[h2][64:65, :]), [t_Osb[h2]], [t_Osb[h2]])
                sc.op(PE, lambda: E[PE].matmul(pm[7][0:64, :], lhsT=e64[:], rhs=Osb[h2][:], start=True, stop=True),
                      [t_e64, t_Osb[h2]], [t_pm[7]])
                sc.op(V, lambda: E[V].tensor_tensor(out=Ya[yb][:, h2, :], in0=Osb[h2][0:64, :], in1=pm[7][0:64, :], op=ALU.mult),
                      [t_Osb[h2], t_pm[7]], [t_Ya[yb]])
            sc.dma(P, Yad[i, :, 2 * a:2 * a + 2, :], Ya[yb][:], reads=[t_Ya[yb]], writes=[t_Yad], owner=t_Ya[yb])

    NS = len(steps)
    LAG = 2
    for n in range(NS + LAG):
        if n < NS:
            emit_S(n)
            emit_exp(n)
        if n >= LAG:
            emit_PV(n - LAG)

    if debug == "p1b":
        sc.wait_all(P, [t_Qd, t_Kd, t_Vd, t_Ad, t_Mcd, t_biasT, t_Yad])
        return nc

    new_phase()
    tp, t_tp, pm, t_pm = mkpsum(2, 6)
    t_Xmd = TR("Xmd"); t_Xmd.multi = True
    t_H2d = TR("H2d"); t_H2d.multi = True
    t_RTd = TR("RTd"); t_RTd.multi = True
    WoC = sb("WoC", [128, 4, D], BF16); t_WoC = TR("WoC")
    WoA = sb("WoA", [128, 4, D], BF16); t_WoA = TR("WoA")
    WrB = sb("WrB", [128, 8, 36], BF16); t_WrB = TR("WrB")
    wst = sb("wst", [128, 8, D]); t_wst = TR("wst")
    wrs = sb("wrs", [128, 8, 36]); t_wrs = TR("wrs")
    g2b = sb("g2b", [128, D]); t_g2b = TR("g2b")
    sc.dma(SP, g2b[:], g2bc, writes=[t_g2b], owner=t_g2b)
    sc.dma(SP, wst[:, 0:4, :], w_out[0:512, :].rearrange("(c p) n -> p c n", p=128), writes=[t_wst], owner=t_wst)
    for c in range(4):
        sc.op(V, lambda: E[V].tensor_scalar(out=WoC[:, c, :], in0=wst[:, c, :], scalar1=gconvs[:, c:c + 1], scalar2=None,
                                            op0=ALU.mult), [t_wst, t_c], [t_WoC])
    sc.dma(SP, wst[:, 4:8, :], w_out[512:1024, :].rearrange("(a p) n -> p a n", p=128), writes=[t_wst], owner=t_wst)
    for a in range(4):
        sc.op(V, lambda: E[V].tensor_scalar(out=WoA[:, a, :], in0=wst[:, 4 + a, :], scalar1=gatts[:, a:a + 1], scalar2=None,
                                            op0=ALU.mult), [t_wst, t_c], [t_WoA])
    sc.dma(SP, wrs[:], wr.rearrange("(kc p) n -> p kc n", p=128), writes=[t_wrs], owner=t_wrs)
    sc.op(V, lambda: E[V].tensor_copy(out=WrB[:], in_=wrs[:]), [t_wrs], [t_WrB])
    NRT = (NE * S + 128) // 128
    rti = sb("rti", [128, NRT], I32); t_rti = TR("rti")
    sc.op(P, lambda: E[P].memset(rti[:], S), [], [t_rti])
    sc.dma(P, RTd.rearrange("(p n) o -> p (n o)", p=128), rti[:], reads=[t_rti], writes=[t_RTd], owner=t_rti)
    dp1 = sb("dp1", [128, NT], I32); dp2 = sb("dp2", [128, NT], I32)
    zrow = sb("zrow", [128, D], BF16); t_zrow = TR("zrow")
    sc.op(P, lambda: E[P].memset(zrow[:], 0.0), [], [t_zrow])
    sc.dma(P, H2d[S:S + 128, :], zrow[:], reads=[t_zrow], writes=[t_H2d], owner=t_zrow)

    mcs = [sb("mcs%d" % i, [128, 4, MT], BF16) for i in range(2)]; t_mcs = [TR("mcs%d" % i) for i in range(2)]
    yas = [sb("yas%d" % i, [128, 4, MT], BF16) for i in range(2)]; t_yas = [TR("yas%d" % i) for i in range(2)]
    for i_ in range(2):
        t_yas[i_].multi = True
    xs2 = [sb("xs2%d" % i, [128, 4, D]) for i in range(2)]; t_xs2 = [TR("xs2%d" % i) for i in range(2)]
    sqa = sb("sqa", [128, 4, MT], BF16); t_sqa = TR("sqa")
    r4 = sb("r4", [128, MT]); t_r4 = TR("r4")
    mixa = sb("mixa", [128, 4, MT], BF16); t_mixa = TR("mixa")
    xm = [sb("xm%d" % i, [128, D]) for i in range(4)]; t_xm = [TR("xm%d" % i) for i in range(4)]
    h2 = [sb("h2%d" % i, [128, D], BF16) for i in range(4)]; t_h2 = [TR("h2%d" % i) for i in range(4)]
    h2T = [sb("h2T%d" % i, [128, 8, 128], BF16) for i in range(4)]; t_h2T = [TR("h2T%d" % i) for i in range(4)]
    junk2 = sb("junk2", [128, D], BF16); t_junk2 = TR("junk2")
    R = []
    for i_ in range(4):
        R.append(dict(
            sm=sb("sm%d" % i_, [128, 32]), lg=sb("lg%d" % i_, [128, 36]), l2=sb("l2_%d" % i_, [128, 8]), l2m=sb("l2m%d" % i_, [128, 8]),
            oh1=sb("oh1_%d" % i_, [128, 8]), oh2=sb("oh2_%d" % i_, [128, 8]), ohg=sb("ohg%d" % i_, [128, 4]), e1=sb("e1_%d" % i_, [128, 4]),
            M1=sb("M1_%d" % i_, [128, 32]), M2=sb("M2_%d" % i_, [128, 32]), Mb=sb("Mb%d" % i_, [128, 32], BF16),
            Rf=sb("Rf%d" % i_, [128, 32]), tmpr=sb("tmpr%d" % i_, [128, 32]), tmpr2=sb("tmpr2_%d" % i_, [128, 32]),
            t=TR("rt%d" % i_), tlg=TR("lgp%d" % i_), tR=TR("Rp%d" % i_), tC=TR("Cp%d" % i_)))
    rank1 = sb("rank1", [128, NT]); rank2 = sb("rank2", [128, NT]); eid1 = sb("eid1", [128, NT]); eid2 = sb("eid2", [128, NT])
    t_rk = TR("rankeid")
    carryR = sb("carryR", [128, 32]); t_carryR = TR("carryR")
    sc.op(V, lambda: E[V].memset(carryR[:], 0.0), [], [t_carryR])
    lg4 = sb("lg4", [128, 4, 36]); mx4 = sb("mx4", [128, 4]); ohg4 = sb("ohg4", [128, 4, 4]); d14 = sb("d14", [128, 4, 4])
    e14 = sb("e14", [128, 4, 4]); se4 = sb("se4", [128, 4]); p1s4 = sb("p1s4", [128, 4]); prod4 = sb("prod4", [128, 4, 4, 8])
    l24 = sb("l24", [128, 4, 8]); m14 = sb("m14", [128, 4]); oh14 = sb("oh14", [128, 4, 8]); l2m4 = sb("l2m4", [128, 4, 8])
    m24 = sb("m24", [128, 4]); oh24 = sb("oh24", [128, 4, 8]); d214 = sb("d214", [128, 4]); ed4 = sb("ed4", [128, 4])
    den4 = sb("den4", [128, 4]); w14 = sb("w14", [128, 4]); w24 = sb("w24", [128, 4])
    M14 = sb("M14", [128, 4, 32]); M24 = sb("M24", [128, 4, 32]); Mb4 = sb("Mb4", [128, 4, 32], BF16)
    Rf4 = sb("Rf4", [128, 4, 32]); tmp4 = sb("tmp4", [128, 4, 32]); dpf4 = sb("dpf4", [128, 4])
    t_rt4 = TR("rt4")

    def vr(fn, reads=(), writes=()):
        sc.op(V, fn, list(reads) + [t_rt4], list(writes) + [t_rt4])

    def bc(ap2d, shape3, axis):
        pst, _ = ap2d.ap[0]
        est, n = ap2d.ap[1]
        if axis == 1:
            return bass.AP(tensor=ap2d.tensor, offset=ap2d.offset, ap=[[pst, 128], [0, shape3[1]], [est, n]])
        return bass.AP(tensor=ap2d.tensor, offset=ap2d.offset, ap=[[pst, 128], [est, n], [0, shape3[2]]])

    def load1c(m):
        b = m % 2
        sc.dma(SP, mcs[b][:], Mcd[m], reads=[t_Mcd], writes=[t_mcs[b]], owner=t_mcs[b])
        for hh in range(2):
            sc.dma(SP, yas[b][hh * 64:(hh + 1) * 64, :, :], Yad[m].rearrange("d (a two) t -> d a two t", two=2)[:, :, hh, :],
                   reads=[t_Yad], writes=[t_yas[b]], owner=t_yas[b])
        sc.dma(SP, xs2[b][:], x_v[m], writes=[t_xs2[b]], owner=t_xs2[b])

    load1c(0)
    for m in range(NM):
        b = m % 2
        if m + 1 < NM:
            load1c(m + 1)
        sc.op(A, lambda: E[A].activation(out=sqa[:], in_=yas[b][:], func=AF.Square), [t_yas[b]], [t_sqa])

        def f():
            ins = None
            for a in range(4):
                ins = E[PE].matmul(pm[5][:], lhsT=onesb[:], rhs=sqa[:, a, :], start=(a == 0), stop=(a == 3))
            return ins
        sc.op(PE, f, [t_identb, t_sqa], [t_pm[5]])
        sc.op(A, lambda: E[A].activation(out=r4[:], in_=pm[5][:], func=AF.Ln, scale=1.0 / 512, bias=epsb[:, 0:1]),
              [t_pm[5], t_eps], [t_r4])
        sc.op(A, lambda: E[A].activation(out=r4[:], in_=r4[:], func=AF.Exp, scale=-0.5), [t_r4], [t_r4])
        for a in range(4):
            sc.op(V, lambda: E[V].tensor_tensor(out=mixa[:, a, :], in0=yas[b][:, a, :], in1=r4[:], op=ALU.mult),
                  [t_yas[b], t_r4], [t_mixa])
        for s in range(4):
            tile = m * 4 + s
            rs_ = R[s]; sm = rs_["sm"]
            xb2 = xm[s]; txm = t_xm[s]
            hb2 = h2[s]; th2 = t_h2[s]
            for n in range(2):
                bank = n

                def f():
                    ins = None
                    for c in range(4):
                        ins = E[PE].matmul(pm[bank][:], lhsT=mcs[b][:, c, s * 128:(s + 1) * 128], rhs=WoC[:, c, n * 512:(n + 1) * 512],
                                           start=(c == 0), stop=False)
                    for a in range(4):
                        ins = E[PE].matmul(pm[bank][:], lhsT=mixa[:, a, s * 128:(s + 1) * 128], rhs=WoA[:, a, n * 512:(n + 1) * 512],
                                           start=False, stop=(a == 3))
                    return ins
                sc.op(PE, f, [t_mcs[b], t_mixa, t_WoC, t_WoA], [t_pm[bank]])
                sc.op(V, lambda: E[V].tensor_tensor(out=xb2[:, n * 512:(n + 1) * 512], in0=pm[bank][:],
                                                    in1=xs2[b][:, s, n * 512:(n + 1) * 512], op=ALU.add),
                      [t_pm[bank], t_xs2[b]], [txm])
            sc.dma(SP, Xmd[tile * 128:(tile + 1) * 128, :], xb2[:], reads=[txm], writes=[t_Xmd], owner=txm)
            sc.op(A, lambda: E[A].activation(out=junk2[:], in_=xb2[:], func=AF.Square, accum_out=sm[:, 0:1]),
                  [txm], [t_junk2, rs_["t"]])
            sc.op(A, lambda: E[A].activation(out=sm[:, 1:2], in_=sm[:, 0:1], func=AF.Sqrt, scale=1.0 / D, bias=epsb[:, 0:1]),
                  [rs_["t"], t_eps], [rs_["t"]])
            sc.op(V, lambda: E[V].reciprocal(out=sm[:, 2:3], in_=sm[:, 1:2]), [rs_["t"]], [rs_["t"]])
            sc.op(V, lambda: E[V].scalar_tensor_tensor(out=hb2[:], in0=xb2[:], scalar=sm[:, 2:3], in1=g2b[:], op0=ALU.mult,
                                                       op1=ALU.mult), [txm, rs_["t"], t_g2b], [th2])
            sc.dma(SP, H2d[tile * 128:(tile + 1) * 128, :], hb2[:], reads=[th2], writes=[t_H2d], owner=th2)
        for s in range(4):
            tile = m * 4 + s
            rs_ = R[s]
            hb2 = h2[s]; th2 = t_h2[s]
            tb = tp[tile % 2]; ttb = t_tp[tile % 2]

            def f():
                ins = None
                for kc in range(8):
                    ins = E[PE].transpose(out=tb[:, kc * 128:(kc + 1) * 128], in_=hb2[:, kc * 128:(kc + 1) * 128], identity=identb[:])
                return ins
            sc.op(PE, f, [th2, t_identb], [ttb])
            sc.op(A, lambda: E[A].copy(out=h2T[s][:], in_=tb[:].rearrange("p (k t) -> p k t", k=8)), [ttb], [t_h2T[s]])

            def f():
                ins = None
                for kc in range(8):
                    ins = E[PE].matmul(pm[2][:, s * 64:s * 64 + 36], lhsT=h2T[s][:, kc, :], rhs=WrB[:, kc, :], start=(kc == 0),
                                       stop=(kc == 7))
                return ins
            sc.op(PE, f, [t_h2T[s], t_WrB], [rs_["tlg"]])

        c0 = 4 * m
        lgp = pm[2][:, 0:256].rearrange("p (s c) -> p s c", c=64)[:, :, 0:36]
        vr(lambda: E[V].tensor_tensor(out=lg4[:], in0=lgp, in1=bc(rbs[:], [128, 4, 36], 1), op=ALU.add),
           [R[0]["tlg"], R[1]["tlg"], R[2]["tlg"], R[3]["tlg"], t_c])
        vr(lambda: E[V].reduce_max(out=mx4[:], in_=lg4[:, :, 0:4], axis=AX.X))
        vr(lambda: E[V].tensor_tensor(out=ohg4[:], in0=lg4[:, :, 0:4], in1=bc(mx4[:], [128, 4, 4], 2), op=ALU.is_equal))
        vr(lambda: E[V].tensor_tensor(out=d14[:], in0=lg4[:, :, 0:4], in1=bc(mx4[:], [128, 4, 4], 2), op=ALU.subtract))
        sc.op(A, lambda: E[A].activation(out=e14[:], in_=d14[:], func=AF.Exp), [t_rt4], [t_rt4])
        vr(lambda: E[V].reduce_sum(out=se4[:], in_=e14[:], axis=AX.X))
        vr(lambda: E[V].reciprocal(out=p1s4[:], in_=se4[:]))
        vr(lambda: E[V].tensor_tensor(out=prod4[:], in0=lg4[:, :, 4:36].rearrange("p s (g e) -> p s g e", e=8),
                                      in1=bass.AP(tensor=ohg4[:].tensor, offset=ohg4[:].offset,
                                                  ap=[[16, 128], [4, 4], [1, 4], [0, 8]]), op=ALU.mult))
        vr(lambda: E[V].reduce_sum(out=l24[:], in_=prod4[:].rearrange("p s g e -> p s e g"), axis=AX.X))
        vr(lambda: E[V].reduce_max(out=m14[:], in_=l24[:], axis=AX.X))
        vr(lambda: E[V].tensor_tensor(out=oh14[:], in0=l24[:], in1=bc(m14[:], [128, 4, 8], 2), op=ALU.is_equal))
        vr(lambda: E[V].scalar_tensor_tensor(out=l2m4[:], in0=oh14[:], scalar=-1e30, in1=l24[:], op0=ALU.mult, op1=ALU.add))
        vr(lambda: E[V].reduce_max(out=m24[:], in_=l2m4[:], axis=AX.X))
        vr(lambda: E[V].tensor_tensor(out=oh24[:], in0=l2m4[:], in1=bc(m24[:], [128, 4, 8], 2), op=ALU.is_equal))
        vr(lambda: E[V].tensor_tensor(out=d214[:], in0=m24[:], in1=m14[:], op=ALU.subtract))
        sc.op(A, lambda: E[A].activation(out=ed4[:], in_=d214[:], func=AF.Exp), [t_rt4], [t_rt4])
        vr(lambda: E[V].tensor_scalar(out=den4[:], in0=ed4[:], scalar1=1.0, scalar2=None, op0=ALU.add))
        vr(lambda: E[V].reciprocal(out=w14[:], in_=den4[:]))
        vr(lambda: E[V].tensor_tensor(out=wgt1[:, c0:c0 + 4], in0=w14[:], in1=p1s4[:], op=ALU.mult), [], [t_route])
        vr(lambda: E[V].tensor_tensor(out=w24[:], in0=ed4[:], in1=w14[:], op=ALU.mult))
        vr(lambda: E[V].tensor_tensor(out=wgt2[:, c0:c0 + 4], in0=w24[:], in1=p1s4[:], op=ALU.mult), [], [t_route])
        ohg_b = bass.AP(tensor=ohg4[:].tensor, offset=ohg4[:].offset, ap=[[16, 128], [4, 4], [1, 4], [0, 8]])
        oh1_b = bass.AP(tensor=oh14[:].tensor, offset=oh14[:].offset, ap=[[32, 128], [8, 4], [0, 4], [1, 8]])
        oh2_b = bass.AP(tensor=oh24[:].tensor, offset=oh24[:].offset, ap=[[32, 128], [8, 4], [0, 4], [1, 8]])
        vr(lambda: E[V].tensor_tensor(out=M14[:].rearrange("p s (g e) -> p s g e", e=8), in0=ohg_b, in1=oh1_b, op=ALU.mult))
        vr(lambda: E[V].tensor_tensor(out=M24[:].rearrange("p s (g e) -> p s g e", e=8), in0=ohg_b, in1=oh2_b, op=ALU.mult))
        vr(lambda: E[V].tensor_tensor(out=Mb4[:], in0=M14[:], in1=M24[:], op=ALU.add))

        def fR():
            ins = None
            for s_ in range(4):
                E[PE].matmul(pm[3][:, s_ * 32:(s_ + 1) * 32], lhsT=lstrb[:], rhs=Mb4[:, s_, :], start=True, stop=True)
                ins = E[PE].matmul(pm[4][:, s_ * 32:(s_ + 1) * 32], lhsT=onesb[:], rhs=Mb4[:, s_, :], start=True, stop=True)
            return ins
        sc.op(PE, fR, [t_identb, t_rt4], [t_pm[3], t_pm[4]])
        for s_ in range(4):
            vr(lambda: E[V].tensor_tensor(out=Rf4[:, s_, :], in0=pm[3][:, s_ * 32:(s_ + 1) * 32], in1=carryR[:], op=ALU.add),
               [t_pm[3], t_carryR])
            sc.op(V, lambda: E[V].tensor_tensor(out=carryR[:], in0=carryR[:], in1=pm[4][:, s_ * 32:(s_ + 1) * 32], op=ALU.add),
                  [t_pm[4], t_carryR], [t_carryR])
        for (Mx, rk, ei) in ((M14, rank1, eid1), (M24, rank2, eid2)):
            vr(lambda: E[V].tensor_tensor(out=tmp4[:], in0=Rf4[:], in1=Mx[:], op=ALU.mult))
            vr(lambda: E[V].reduce_sum(out=rk[:, c0:c0 + 4], in_=tmp4[:], axis=AX.X), [], [t_rk])
            vr(lambda: E[V].tensor_tensor(out=tmp4[:], in0=bc(ebs[:], [128, 4, 32], 1), in1=Mx[:], op=ALU.mult), [t_c])
            vr(lambda: E[V].reduce_sum(out=ei[:, c0:c0 + 4], in_=tmp4[:], axis=AX.X), [], [t_rk])
        for (ei, rk, dpp) in ((eid1, rank1, dp1), (eid2, rank2, dp2)):
            vr(lambda: E[V].scalar_tensor_tensor(out=dpf4[:], in0=ei[:, c0:c0 + 4], scalar=float(S), in1=rk[:, c0:c0 + 4],
                                                 op0=ALU.mult, op1=ALU.add), [t_rk])
            vr(lambda: E[V].tensor_copy(out=dpp[:, c0:c0 + 4], in_=dpf4[:]))
            for s_ in range(4):
                tile = c0 + s_
                sc.dma(P, None, None, reads=[t_rt4, t_c], writes=[t_RTd], owner=t_rt4,
                       fn=lambda: E[P].indirect_dma_start(out=RTd[:, :], out_offset=bass.IndirectOffsetOnAxis(ap=dpp[:, tile:tile + 1], axis=0),
                                                          in_=iotas[:, tile:tile + 1], in_offset=None,
                                                          bounds_check=rb_rt, oob_is_err=False))

    def bc(ap2d, shape3, axis):
        pst, _ = ap2d.ap[0]
        est, n = ap2d.ap[1]
        if axis == 1:
            return bass.AP(tensor=ap2d.tensor, offset=ap2d.offset, ap=[[pst, 128], [0, shape3[1]], [est, n]])
        return bass.AP(tensor=ap2d.tensor, offset=ap2d.offset, ap=[[pst, 128], [est, n], [0, shape3[2]]])
    cmpk = sb("cmpk", [128, 32, 33]); ntl = sb("ntl", [128, 32]); padd = sb("padd", [128, 32]); pend = sb("pend", [128, 32])
    pstt = sb("pstt", [128, 32]); bigt = sb("bigt", [128, NT, 32]); psel = sb("psel", [128, NT]); destf = sb("destf", [128, NT])
    cmpt = sb("cmpt", [128, NTL, 32]); tef = sb("tef", [128, NTL]); widf = sb("widf", [128, NTL])
    t_fin = TR("fin")

    def fop(fn, reads=(), writes=()):
        sc.op(V, fn, list(reads) + [t_fin], list(writes) + [t_fin])
    fop(lambda: E[V].tensor_tensor(out=cmpk[:], in0=bc(carryR[:], [128, 32, 33], 2), in1=bc(thrs[:], [128, 32, 33], 1), op=ALU.is_gt),
        [t_carryR, t_c])
    fop(lambda: E[V].reduce_sum(out=ntl[:], in_=cmpk[:], axis=AX.X))
    fop(lambda: E[V].tensor_scalar(out=padd[:], in0=ntl[:], scalar1=float(TS), scalar2=None, op0=ALU.mult))
    fop(lambda: E[V].tensor_tensor_scan(out=pend[:], data0=ones512[:, 0:32], data1=padd[:], initial=0.0, op0=ALU.mult, op1=ALU.add),
        [t_ones512])
    fop(lambda: E[V].tensor_tensor(out=pstt[:], in0=pend[:], in1=padd[:], op=ALU.subtract))
    for (rk, ei, dd) in ((rank1, eid1, dest1), (rank2, eid2, dest2)):
        fop(lambda: E[V].tensor_tensor(out=bigt[:], in0=bc(ei[:], [128, NT, 32], 2), in1=bc(ebs[:], [128, NT, 32], 1), op=ALU.is_equal),
            [t_rk, t_c])
        fop(lambda: E[V].tensor_tensor(out=bigt[:], in0=bigt[:], in1=bc(pstt[:], [128, NT, 32], 1), op=ALU.mult))
        fop(lambda: E[V].reduce_sum(out=psel[:], in_=bigt[:], axis=AX.X))
        fop(lambda: E[V].tensor_tensor(out=destf[:], in0=psel[:], in1=rk[:], op=ALU.add), [t_rk])
        fop(lambda: E[V].tensor_copy(out=dd[:], in_=destf[:]), [], [t_route])
    fop(lambda: E[V].tensor_tensor(out=cmpt[:], in0=bc(pend[:], [128, NTL, 32], 1), in1=bc(tsts[:], [128, NTL, 32], 2), op=ALU.is_le),
        [t_c])
    fop(lambda: E[V].reduce_sum(out=tef[:], in_=cmpt[:], axis=AX.X))
    fop(lambda: E[V].tensor_scalar(out=widf[:], in0=tef[:], scalar1=128.0, scalar2=pcols[:, 0:1], op0=ALU.mult, op1=ALU.add), [t_c])
    eqf = sb("eqf", [128, NTL])
    fop(lambda: E[V].memset(eqf[:], 0.0))
    fop(lambda: E[V].tensor_tensor(out=eqf[:, 2:NTL], in0=tef[:, 2:NTL], in1=tef[:, 0:NTL - 2], op=ALU.is_equal))
    fop(lambda: E[V].scalar_tensor_tensor(out=widf[:], in0=eqf[:], scalar=float(NE * 128), in1=widf[:], op0=ALU.mult, op1=ALU.add))
    fop(lambda: E[V].tensor_copy(out=widx[:], in_=widf[:]), [], [t_widx])
    pstl = sb("pstl", [128, NTL]); basef = sb("basef", [128, NTL]); rif = sb("rif", [128, NTL])
    fop(lambda: E[V].tensor_tensor(out=cmpt[:], in0=bc(tef[:], [128, NTL, 32], 2), in1=bc(ebs[:], [128, NTL, 32], 1), op=ALU.is_equal),
        [t_c])
    fop(lambda: E[V].tensor_tensor(out=cmpt[:], in0=cmpt[:], in1=bc(pstt[:], [128, NTL, 32], 1), op=ALU.mult))
    fop(lambda: E[V].reduce_sum(out=pstl[:], in_=cmpt[:], axis=AX.X))
    fop(lambda: E[V].scalar_tensor_tensor(out=basef[:], in0=tef[:], scalar=float(S), in1=tsts[:], op0=ALU.mult, op1=ALU.add), [t_c])
    fop(lambda: E[V].tensor_tensor(out=basef[:], in0=basef[:], in1=pstl[:], op=ALU.subtract))
    for st in range(TS // 128):
        fop(lambda: E[V].tensor_scalar(out=rif[:], in0=basef[:], scalar1=pcols[:, 0:1], scalar2=float(128 * st), op0=ALU.add, op1=ALU.add),
            [t_c])
        fop(lambda: E[V].tensor_copy(out=ridx[:, :, st], in_=rif[:]), [], [t_widx])

    new_phase()
    tp, t_tp, pm, t_pm = mkpsum(2, 6)
    t_Ysd = TR("Ysd"); t_Ysd.multi = True
    NSUB = TS // 128
    Wg = [sb("Wg%d" % i, [128, 8, DE], BF16) for i in range(2)]; t_Wg = [TR("Wg%d" % i) for i in range(2)]
    Wu = [sb("Wu%d" % i, [128, 8, DE], BF16) for i in range(2)]; t_Wu = [TR("Wu%d" % i) for i in range(2)]
    Wd = [sb("Wd%d" % i, [128, 2, D], BF16) for i in range(2)]; t_Wd = [TR("Wd%d" % i) for i in range(2)]
    Wgs = [sb("Wgs%d" % i, [128, 8 * DE]) for i in range(2)]; t_Wgs = [TR("Wgs%d" % i) for i in range(2)]
    Wus = [sb("Wus%d" % i, [128, 8 * DE]) for i in range(2)]; t_Wus = [TR("Wus%d" % i) for i in range(2)]
    Wds = [sb("Wds%d" % i, [128, 2 * D]) for i in range(2)]; t_Wds = [TR("Wds%d" % i) for i in range(2)]
    for i_ in range(2):
        sc.op(V, lambda: E[V].memset(Wgs[i_][:], 0.0), [], [t_Wgs[i_]])
        sc.op(V, lambda: E[V].memset(Wus[i_][:], 0.0), [], [t_Wus[i_]])
        sc.op(V, lambda: E[V].memset(Wds[i_][:], 0.0), [], [t_Wds[i_]])
    idxs = [[sb("idx%d_%d" % (i, j), [128, 1], I32) for j in range(NSUB)] for i in range(2)]
    t_idx = [[TR("idx%d_%d" % (i, j)) for j in range(NSUB)] for i in range(2)]
    Xg = [[sb("Xg%d_%d" % (i, j), [128, D], BF16) for j in range(NSUB)] for i in range(2)]
    t_Xg = [[TR("Xg%d_%d" % (i, j)) for j in range(NSUB)] for i in range(2)]
    XgT = [sb("XgT%d" % i, [128, 8, TS], BF16) for i in range(2)]; t_XgT = [TR("XgT%d" % i) for i in range(2)]
    sgl = [sb("sgl%d" % i, [128, TS]) for i in range(2)]; t_sgl = [TR("sgl%d" % i) for i in range(2)]
    AT = [sb("AT%d" % i, [128, 2, TS], BF16) for i in range(2)]; t_AT = [TR("AT%d" % i) for i in range(2)]
    NYB = 4
    Yt = [sb("Yt%d" % i, [128, D], BF16) for i in range(NYB)]; t_Yt = [TR("Yt%d" % i) for i in range(NYB)]

    def L2(t):
        b = t % 2
        for st in range(NSUB):
            sc.dma(P, None, None, reads=[t_widx, t_RTd], writes=[t_idx[b][st]], owner=t_idx[b][st],
                   fn=lambda: E[P].indirect_dma_start(out=idxs[b][st][:, :], out_offset=None, in_=RTd[:, :],
                                                      in_offset=bass.IndirectOffsetOnAxis(ap=ridx[:, t, st:st + 1], axis=0),
                                                      bounds_check=rb_rt, oob_is_err=False))
        for (wt, twt, src) in ((Wgs[b], t_Wgs[b], w_gate), (Wus[b], t_Wus[b], w_up), (Wds[b], t_Wds[b], w_down)):
            sc.dma(P, None, None, reads=[t_widx], writes=[twt], owner=twt,
                   fn=lambda: E[P].indirect_dma_start(out=wt[:, :], out_offset=None, in_=src[:, :],
                                                      in_offset=bass.IndirectOffsetOnAxis(ap=widx[:, t:t + 1], axis=0),
                                                      bounds_check=rb_w, oob_is_err=False))
        for st in range(NSUB):
            sc.dma(P, None, None, reads=[t_idx[b][st], t_H2d], writes=[t_Xg[b][st]], owner=t_Xg[b][st],
                   fn=lambda: E[P].indirect_dma_start(out=Xg[b][st][:, :], out_offset=None, in_=H2d[:, :],
                                                      in_offset=bass.IndirectOffsetOnAxis(ap=idxs[b][st][:, 0:1], axis=0),
                                                      bounds_check=rb_h2, oob_is_err=False))

    def C2(t):
        b = t % 2
        sc.op(V, lambda: E[V].tensor_copy(out=Wg[b][:].rearrange("p k n -> p (k n)"), in_=Wgs[b][:]), [t_Wgs[b]], [t_Wg[b]])
        sc.op(A, lambda: E[A].copy(out=Wu[b][:].rearrange("p k n -> p (k n)"), in_=Wus[b][:]), [t_Wus[b]], [t_Wu[b]])
        sc.op(V, lambda: E[V].tensor_copy(out=Wd[b][:].rearrange("p k n -> p (k n)"), in_=Wds[b][:]), [t_Wds[b]], [t_Wd[b]])

    def T2(t):
        b = t % 2
        for st in range(NSUB):
            tb = tp[st % 2]; ttb = t_tp[st % 2]

            def f():
                ins = None
                for kc in range(8):
                    ins = E[PE].transpose(out=tb[:, kc * 128:(kc + 1) * 128], in_=Xg[b][st][:, kc * 128:(kc + 1) * 128],
                                          identity=identb[:])
                return ins
            sc.op(PE, f, [t_Xg[b][st], t_identb], [ttb])
            if st % 2 == 0:
                sc.op(A, lambda: E[A].copy(out=XgT[b][:, :, st * 128:(st + 1) * 128], in_=tb[:].rearrange("p (k t) -> p k t", k=8)),
                      [ttb], [t_XgT[b]])
            else:
                sc.op(V, lambda: E[V].tensor_copy(out=XgT[b][:, :, st * 128:(st + 1) * 128],
                                                  in_=tb[:].rearrange("p (k t) -> p k t", k=8)), [ttb], [t_XgT[b]])

    def M2a(t):
        b = t % 2
        N = TS
        for fc in range(2):
            bg = 2 * fc; bu = 2 * fc + 1

            def fg():
                ins = None
                for kc in range(8):
                    ins = E[PE].matmul(pm[bg][:, 0:N], lhsT=Wg[b][:, kc, fc * 128:(fc + 1) * 128], rhs=XgT[b][:, kc, 0:N],
                                       start=(kc == 0), stop=(kc == 7))
                return ins

            def fu():
                ins = None
                for kc in range(8):
                    ins = E[PE].matmul(pm[bu][:, 0:N], lhsT=Wu[b][:, kc, fc * 128:(fc + 1) * 128], rhs=XgT[b][:, kc, 0:N],
                                       start=(kc == 0), stop=(kc == 7))
                return ins
            sc.op(PE, fg, [t_Wg[b], t_XgT[b]], [t_pm[bg]])
            sc.op(PE, fu, [t_Wu[b], t_XgT[b]], [t_pm[bu]])
            sc.op(A, lambda: E[A].activation(out=sgl[fc][:, 0:N], in_=pm[bg][:, 0:N], func=AF.Silu), [t_pm[bg]], [t_sgl[fc]])
            sc.op(V, lambda: E[V].tensor_tensor(out=AT[b][:, fc, 0:N], in0=pm[bu][:, 0:N], in1=sgl[fc][:, 0:N], op=ALU.mult),
                  [t_pm[bu], t_sgl[fc]], [t_AT[b]])

    ny = [0]

    def M2b(t):
        b = t % 2
        for st in range(NSUB):
            yb = ny[0] % NYB; ny[0] += 1
            for n in range(2):
                bank = 4 + n

                def fy():
                    ins = None
                    for fc in range(2):
                        ins = E[PE].matmul(pm[bank][:], lhsT=AT[b][:, fc, st * 128:(st + 1) * 128],
                                           rhs=Wd[b][:, fc, n * 512:(n + 1) * 512], start=(fc == 0), stop=(fc == 1))
                    return ins
                sc.op(PE, fy, [t_AT[b], t_Wd[b]], [t_pm[bank]])
                if n == 0:
                    sc.op(A, lambda: E[A].copy(out=Yt[yb][:, 0:512], in_=pm[bank][:]), [t_pm[bank]], [t_Yt[yb]])
                else:
                    sc.op(V, lambda: E[V].tensor_copy(out=Yt[yb][:, 512:1024], in_=pm[bank][:]), [t_pm[bank]], [t_Yt[yb]])
            r0 = t * TS + st * 128
            sc.dma(SP, Ysd[r0:r0 + 128, :], Yt[yb][:], reads=[t_Yt[yb]], writes=[t_Ysd], owner=t_Yt[yb])

    L2(0); C2(0); T2(0)
    for t in range(NTL):
        if t + 1 < NTL:
            L2(t + 1)
        M2a(t)
        if t + 1 < NTL:
            C2(t + 1)
        M2b(t)
        if t + 1 < NTL:
            T2(t + 1)

    new_phase()
    t_out = TR("out"); t_out.multi = True
    fgs = sb("fgs", [128, D]); t_fgs = TR("fgs")
    sc.dma(SP, fgs[:], fing, writes=[t_fgs], owner=t_fgs)
    NB3 = 4
    xm3 = [sb("xm3%d" % i, [128, D]) for i in range(NB3)]; t_xm3 = [TR("xm3%d" % i) for i in range(NB3)]
    y1 = [sb("y1%d" % i, [128, D], BF16) for i in range(NB3)]; t_y1 = [TR("y1%d" % i) for i in range(NB3)]
    y2 = [sb("y2%d" % i, [128, D], BF16) for i in range(NB3)]; t_y2 = [TR("y2%d" % i) for i in range(NB3)]
    ot = [sb("ot%d" % i, [128, D]) for i in range(2)]; t_ot = [TR("ot%d" % i) for i in range(2)]
    junk3 = sb("junk3", [128, D], BF16); t_junk3 = TR("junk3")
    sm3 = [sb("sm3_%d" % i, [128, 4]) for i in range(2)]; t_sm3 = [TR("sm3_%d" % i) for i in range(2)]

    def L3(t):
        b = t % NB3
        sc.dma(SP, xm3[b][:], Xmd[t * 128:(t + 1) * 128, :], reads=[t_Xmd], writes=[t_xm3[b]], owner=t_xm3[b])
        for (yy, tyy, dd) in ((y1[b], t_y1[b], dest1), (y2[b], t_y2[b], dest2)):
            sc.dma(P, None, None, reads=[t_route, t_Ysd], writes=[tyy], owner=tyy,
                   fn=lambda: E[P].indirect_dma_start(out=yy[:, :], out_offset=None, in_=Ysd[:, :],
                                                      in_offset=bass.IndirectOffsetOnAxis(ap=dd[:, t:t + 1], axis=0),
                                                      bounds_check=rb_ys, oob_is_err=False))

    def C3(t):
        b = t % NB3
        ob = t % 2
        s3 = sm3[ob]; ts3 = t_sm3[ob]
        sc.op(V, lambda: E[V].scalar_tensor_tensor(out=xm3[b][:], in0=y1[b][:], scalar=wgt1[:, t:t + 1], in1=xm3[b][:],
                                                   op0=ALU.mult, op1=ALU.add), [t_y1[b], t_xm3[b], t_route], [t_xm3[b]])
        sc.op(V, lambda: E[V].scalar_tensor_tensor(out=xm3[b][:], in0=y2[b][:], scalar=wgt2[:, t:t + 1], in1=xm3[b][:],
                                                   op0=ALU.mult, op1=ALU.add), [t_y2[b], t_xm3[b], t_route], [t_xm3[b]])
        sc.op(A, lambda: E[A].activation(out=junk3[:], in_=xm3[b][:], func=AF.Square, accum_out=s3[:, 0:1]),
              [t_xm3[b]], [t_junk3, ts3])
        sc.op(A, lambda: E[A].activation(out=s3[:, 1:2], in_=s3[:, 0:1], func=AF.Sqrt, scale=1.0 / D, bias=epsb[:, 0:1]),
              [ts3, t_eps], [ts3])
        sc.op(V, lambda: E[V].reciprocal(out=s3[:, 2:3], in_=s3[:, 1:2]), [ts3], [ts3])
        sc.op(V, lambda: E[V].scalar_tensor_tensor(out=ot[ob][:], in0=xm3[b][:], scalar=s3[:, 2:3], in1=fgs[:],
                                                   op0=ALU.mult, op1=ALU.mult), [t_xm3[b], ts3, t_fgs], [t_ot[ob]])
        sc.dma(SP, out[t * 128:(t + 1) * 128, :], ot[ob][:], reads=[t_ot[ob]], writes=[t_out], owner=t_ot[ob])

    for t in range(min(2, NT)):
        L3(t)
    for t in range(NT):
        if t + 2 < NT:
            L3(t + 2)
        C3(t)
    sc.wait_all(P, [t_out])
    barrier()
    return nc


def _prep_inputs(inp, b, S):
    f = np.float32

    def T8(v):
        return np.ascontiguousarray(v.reshape(8, 128).T).astype(f)

    def T4(v):
        return np.ascontiguousarray(v.reshape(4, 128).T).astype(f)
    d = {}
    d["x"] = np.ascontiguousarray(inp["x"][b][:S])
    d["w_in"] = inp["w_in"][0]
    d["w_out"] = inp["w_out"][0]
    d["wr"] = np.ascontiguousarray(np.concatenate([inp["w_r1"][0]] + [inp["w_r2"][0][g] for g in range(4)], axis=1))
    d["w_gate"] = np.ascontiguousarray(inp["w_gate"][0].reshape(32, 8, 128, 256).transpose(0, 2, 1, 3).reshape(32 * 128, 2048))
    d["w_up"] = np.ascontiguousarray(inp["w_up"][0].reshape(32, 8, 128, 256).transpose(0, 2, 1, 3).reshape(32 * 128, 2048))
    d["w_down"] = np.ascontiguousarray(inp["w_down"][0].reshape(32, 2, 128, 1024).transpose(0, 2, 1, 3).reshape(32 * 128, 2048))
    d["g1T"] = T8(inp["norm1_g"][0])
    d["g2T"] = T8(inp["norm2_g"][0])
    d["gconvT"] = T4(inp["out_g_conv"][0])
    d["gattT"] = T4(inp["out_g_att"][0])
    d["bdwT"] = T4(inp["b_dw"][0])
    d["lngT"] = T4(inp["conv_ln_g"][0])
    d["lnbT"] = T4(inp["conv_ln_b"][0])
    wd = inp["w_dw"][0]
    d["wdwT"] = np.ascontiguousarray(wd.reshape(31, 4, 128).transpose(2, 1, 0).reshape(128, 124))
    nb = np.zeros((128, 1), f)
    for q in range(4):
        nb[q * 32:q * 32 + 8, 0] = -inp["b_f"][0]
    d["negbf"] = nb
    d["fing"] = np.ascontiguousarray(np.broadcast_to(inp["final_g"][None, :], (128, 1024))).astype(f)
    rb = np.concatenate([inp["b_r1"][0], inp["b_r2"][0].reshape(-1)])
    d["g2bc"] = np.ascontiguousarray(np.broadcast_to(inp["norm2_g"][0][None, :], (128, 1024))).astype(f)
    d["rbias"] = np.ascontiguousarray(np.broadcast_to(rb[None, :], (128, 36))).astype(f)
    c = np.zeros((128, 640), f)
    c[:, 0:128] = np.eye(128)
    c[:, 128:256] = 1.0
    c[:, 256:384] = np.triu(np.ones((128, 128)), 1)
    c[:, 384:512] = np.tril(np.ones((128, 128)), -1) * -30000.0
    c[:, 512:640] = 1.0
    d["consts"] = c
    d["ebase"] = np.ascontiguousarray(np.broadcast_to(np.arange(32)[None, :], (128, 32))).astype(f)
    d["thr256"] = np.ascontiguousarray(np.broadcast_to((np.arange(33) * TS)[None, :], (128, 33))).astype(f)
    NTL = NE * CAP // TS
    d["tst256"] = np.ascontiguousarray(np.broadcast_to((np.arange(NTL) * TS)[None, :], (128, NTL))).astype(f)
    d["pcol"] = np.arange(128).reshape(128, 1).astype(f)
    NT = S // 128
    d["iota_tok"] = (np.arange(NT)[None, :] * 128 + np.arange(128)[:, None]).astype(np.int32)
    return d


def kernel(**inputs):
    inp = {k: np.asarray(v) for k, v in inputs.items()}
    B, S, _ = inp["x"].shape
    nc = build(S)
    in_maps = [_prep_inputs(inp, b, S) for b in range(B)]
    res = run_bass_kernel_spmd(nc, in_maps, core_ids=list(range(B)))
    return np.stack([np.asarray(r["out"]) for r in res.results], axis=0).astype(np.float32)
```

```python
import numpy as np
import ml_dtypes
import concourse.bass as bass
import concourse.mybir as mybir
from concourse.bass_utils import run_bass_kernel_spmd

F32 = mybir.dt.float32
BF16 = mybir.dt.bfloat16
I32 = mybir.dt.int32
AF = mybir.ActivationFunctionType
ALU = mybir.AluOpType
AX = mybir.AxisListType

D = 1024
DIN = 2568
NH = 8
HD = 64
CW = 31
NE = 32
DE = 256
CAP = 768
TS = 256
EPS = 1e-6
MT = 512


class TR:
    __slots__ = ("name", "w", "r", "dsem", "dcnt", "multi")

    all = []

    def __init__(self, name):
        TR.all.append(self)
        self.name = name
        self.w = {}
        self.r = {}
        self.dsem = {}
        self.dcnt = {}
        self.multi = False


class Sched:
    def __init__(self, nc):
        self.nc = nc
        self.eng = {"pe": nc.tensor, "act": nc.scalar, "dve": nc.vector, "pool": nc.gpsimd, "sp": nc.sync}
        self.sem = {k: nc.alloc_semaphore("sem_" + k) for k in self.eng}
        self.cnt = {k: 0 for k in self.eng}
        self.waited = {k: {} for k in self.eng}
        self.nsem = 0
        self.pool = {"sw": [], "hw": []}

    def _deps(self, e, reads, writes):
        deps = {}
        for t in list(reads) + list(writes):
            for k, (s, v) in t.w.items():
                if deps.get(k, (None, 0))[1] < v:
                    deps[k] = (s, v)
        for t in writes:
            for k, (s, v) in t.r.items():
                if deps.get(k, (None, 0))[1] < v:
                    deps[k] = (s, v)
        wd = self.waited[e]
        own = id(self.sem[e])
        for k, (s, v) in deps.items():
            if e == "pe" and k == own:
                continue
            if wd.get(k, 0) < v:
                self.eng[e].wait_ge(s, v)
                wd[k] = v

    def op(self, e, fn, reads=(), writes=()):
        self._deps(e, reads, writes)
        ins = fn()
        s = self.sem[e]
        ins.then_inc(s, 1)
        self.cnt[e] += 1
        v = self.cnt[e]
        k = id(s)
        for t in writes:
            t.w = {k: (s, v)}
            t.r = {}
        for t in reads:
            t.r[k] = (s, v)

    def dma(self, q, out, in_, reads=(), writes=(), owner=None, n=1, fn=None):
        self._deps(q, reads, writes)
        kind = "sw" if q == "pool" else "hw"
        if kind not in owner.dsem:
            if self.pool[kind]:
                owner.dsem[kind], owner.dcnt[kind] = self.pool[kind].pop()
            else:
                owner.dsem[kind] = self.nc.alloc_semaphore("d%s_%d" % (kind, self.nsem))
                owner.dcnt[kind] = 0
                self.nsem += 1
        if fn is None:
            ins = self.eng[q].dma_start(out=out, in_=in_)
        else:
            ins = fn()
        sem = owner.dsem[kind]
        ins.then_inc(sem, 16)
        owner.dcnt[kind] += 16
        k = id(sem)
        ent = (sem, owner.dcnt[kind])
        for t in writes:
            if not t.multi:
                t.w = {}
            t.w[k] = ent
            t.r = {}
        for t in reads:
            t.r[k] = ent

    def wait_all(self, e, tracked):
        self._deps(e, tracked, tracked)


def build(S, debug=None):
    NM = S // MT
    NT = S // 128
    NSLOT = NE * CAP
    nc = bass.Bass("TRN2", target_bir_lowering=False)
    TR.all = []
    sc = Sched(nc)

    def din(name, shape, dt=F32):
        return nc.dram_tensor(name, list(shape), dt, kind="ExternalInput").ap()

    def dscr(name, shape, dt):
        return nc.dram_tensor(name, list(shape), dt, kind=("ExternalOutput" if debug else "Internal")).ap()

    x = din("x", [S, D])
    w_in = din("w_in", [D, DIN])
    w_out = din("w_out", [D, D])
    wr = din("wr", [D, 36])
    w_gate = din("w_gate", [NE * 128, 8 * DE])
    w_up = din("w_up", [NE * 128, 8 * DE])
    w_down = din("w_down", [NE * 128, 2 * D])
    g1T = din("g1T", [128, 8])
    g2T = din("g2T", [128, 8])
    gconvT = din("gconvT", [128, 4])
    gattT = din("gattT", [128, 4])
    bdwT = din("bdwT", [128, 4])
    lngT = din("lngT", [128, 4])
    lnbT = din("lnbT", [128, 4])
    wdwT = din("wdwT", [128, 4 * CW])
    negbf = din("negbf", [128, 1])
    fing = din("fing", [128, D])
    g2bc = din("g2bc", [128, D])
    rbias = din("rbias", [128, 36])
    consts = din("consts", [128, 5 * 128])
    ebase = din("ebase", [128, NE])
    thr256 = din("thr256", [128, 33])
    tst256 = din("tst256", [128, NSLOT // TS])
    pcol = din("pcol", [128, 1])
    iota_tok = din("iota_tok", [128, NT], I32)
    out = nc.dram_tensor("out", [S, D], F32, kind="ExternalOutput").ap()

    Qd = dscr("Qd", [4, NM, 128, MT], BF16)
    Kd = dscr("Kd", [4, NM, 128, MT], BF16)
    Vd = dscr("Vd", [4, NM, 128, 4, 130], BF16)
    Ad = dscr("Ad", [NM, 128, MT], BF16)
    Mcd = dscr("Mcd", [NM, 128, 4, MT], BF16)
    Yad = dscr("Yad", [NM, 64, 8, MT], BF16)
    Xmd = dscr("Xmd", [S, D], F32)
    H2d = dscr("H2d", [S + 128, D], BF16)
    RTd = dscr("RTd", [NE * S + 128, 1], I32)
    Ysd = dscr("Ysd", [NSLOT, D], BF16)


    from contextlib import ExitStack
    stk = [ExitStack()]

    def sb(name, shape, dt=F32):
        return stk[0].enter_context(nc.sbuf_tensor(name, list(shape), dt))

    dma_owners = []

    def barrier():
        ents = [(sc.sem[k], sc.cnt[k]) for k in sc.eng if sc.cnt[k] > 0]
        for t in TR.all:
            for kd in t.dsem:
                if t.dcnt[kd] > 0:
                    ents.append((t.dsem[kd], t.dcnt[kd]))
        for e in sc.eng:
            wd = sc.waited[e]
            for (sm, v) in ents:
                if sm is sc.sem[e]:
                    continue
                if wd.get(id(sm), 0) < v:
                    sc.eng[e].wait_ge(sm, v)
                    wd[id(sm)] = v

    def new_phase():
        barrier()
        for t in TR.all:
            for kd in list(t.dsem):
                sc.pool[kd].append((t.dsem[kd], t.dcnt[kd]))
            t.dsem = {}
            t.dcnt = {}
            t.w = {}
            t.r = {}
        stk[0].close()
        stk[0] = ExitStack()

    psn = [0]

    def ps(name, shape, dt=F32):
        psn[0] += 1
        return stk[0].enter_context(nc.psum_tensor("%s_%d" % (name, psn[0]), list(shape), dt))

    def mkpsum(ntp, npm):
        tp_ = [ps("tp%d" % i, [128, 1024], BF16) for i in range(ntp)]
        ttp_ = [TR("tp%d" % i) for i in range(ntp)]
        pm_ = [ps("pm%d" % i, [128, 512]) for i in range(npm)]
        tpm_ = [TR("pm%d" % i) for i in range(npm)]
        return tp_, ttp_, pm_, tpm_

    V, A, P, PE, SP = "dve", "act", "pool", "pe", "sp"
    E = sc.eng
    rb_rt = E[P].to_reg(NE * S + 127)
    rb_h2 = E[P].to_reg(S + 127)
    rb_ys = E[P].to_reg(NSLOT - 1)

    def E_pool_to_reg(v):
        return E[P].to_reg(v)

    cst = sb("cst", [128, 5 * 128]); t_cst = TR("cst")
    sc.dma(SP, cst[:], consts, writes=[t_cst], owner=t_cst)
    ident_f = cst[:, 0:128]
    ones_f = cst[:, 128:256]
    identb = sb("identb", [128, 128], BF16); t_identb = TR("identb")
    onesb = sb("onesb", [128, 128], BF16)
    lstrb = sb("lstrb", [128, 128], BF16)
    negmb = sb("negmb", [128, 128], BF16)
    sc.op(V, lambda: E[V].tensor_copy(out=identb[:], in_=cst[:, 0:128]), [t_cst], [t_identb])
    sc.op(V, lambda: E[V].tensor_copy(out=onesb[:], in_=cst[:, 128:256]), [t_cst], [t_identb])
    sc.op(V, lambda: E[V].tensor_copy(out=lstrb[:], in_=cst[:, 256:384]), [t_cst], [t_identb])
    sc.op(V, lambda: E[V].tensor_copy(out=negmb[:], in_=cst[:, 384:512]), [t_cst], [t_identb])
    ones512 = sb("ones512", [128, MT]); t_ones512 = TR("ones512")
    sc.op(P, lambda: E[P].memset(ones512[:], 1.0), [], [t_ones512])
    epsb = sb("epsb", [128, 1]); t_eps = TR("epsb")
    sc.op(P, lambda: E[P].memset(epsb[:], EPS), [], [t_eps])
    t_c = TR("consts_all")

    def load_small(name, ap, shape, dt=F32):
        t = sb(name, shape, dt)
        sc.dma(SP, t[:], ap, writes=[t_c], owner=t_c)
        return t

    t_c.multi = True
    g1s = load_small("g1s", g1T, [128, 8])
    g2s = load_small("g2s", g2T, [128, 8])
    gconvs = load_small("gconvs", gconvT, [128, 4])
    gatts = load_small("gatts", gattT, [128, 4])
    bdws = load_small("bdws", bdwT, [128, 4])
    lngs = load_small("lngs", lngT, [128, 4])
    lnbs = load_small("lnbs", lnbT, [128, 4])
    wdws = load_small("wdws", wdwT, [128, 4 * CW])
    negbs = load_small("negbs", negbf, [128, 1])
    rbs = load_small("rbs", rbias, [128, 36])
    ebs = load_small("ebs", ebase, [128, NE])
    thrs = load_small("thrs", thr256, [128, 33])
    tsts = load_small("tsts", tst256, [128, NSLOT // TS])
    pcols = load_small("pcols", pcol, [128, 1])
    iotas = load_small("iotas", iota_tok, [128, NT], I32)

    biasT = sb("biasT", [128, NT, 8]); t_biasT = TR("biasT")
    dest1 = sb("dest1", [128, NT], I32); dest2 = sb("dest2", [128, NT], I32)
    wgt1 = sb("wgt1", [128, NT]); wgt2 = sb("wgt2", [128, NT])
    t_route = TR("route")
    NTL = NSLOT // TS
    widx = sb("widx", [128, NTL], I32); t_widx = TR("widx")
    ridx = sb("ridx", [128, NTL, TS // 128], I32)
    rb_w = E_pool_to_reg(NE * 128 - 1)

    selB = sb("selB", [128, 8, 128], BF16); t_sel = TR("selB")
    selc = sb("selc", [128, 8]); t_selc = TR("selc")
    sc.op(V, lambda: E[V].tensor_tensor(out=selc[:], in0=cst[:, 0:8], in1=cst[:, 32:40], op=ALU.add), [t_cst], [t_selc])
    for h in range(8):
        sc.op(V, lambda: E[V].tensor_scalar(out=selB[:, h, :], in0=cst[:, 128:256], scalar1=selc[:, h:h + 1], scalar2=None,
                                            op0=ALU.mult), [t_selc, t_cst], [t_sel])

    e64 = sb("e64", [65, 64]); t_e64 = TR("e64")
    sc.op(V, lambda: E[V].memset(e64[:], 0.0), [], [t_e64])
    sc.op(V, lambda: E[V].memset(e64[64:65, :], 1.0), [t_e64], [t_e64])
    persist = stk[0]
    stk[0] = ExitStack()
    WinB = sb("WinB", [128, 8, DIN], BF16); t_win = TR("WinB")
    WfB = sb("WfB", [128, 8, 128], BF16); t_wf = TR("WfB")
    stg = [sb("stg%d" % i, [128, DIN]) for i in range(1)] * 2
    t_stg = [TR("stg%d" % i) for i in range(1)] * 2
    sc.op(P, lambda: E[P].memset(WfB[:], 0.0), [], [t_wf])
    w_in_v = w_in.rearrange("(kc p) n -> p kc n", p=128)
    for kc in range(8):
        b = kc % 2
        sc.dma(SP, stg[b][:], w_in_v[:, kc, :], writes=[t_stg[b]], owner=t_stg[b])
        eng = A if kc % 2 == 0 else V
        if eng == A:
            sc.op(A, lambda: E[A].activation(out=WinB[:, kc, :], in_=stg[b][:], func=AF.Copy, scale=g1s[:, kc:kc + 1]),
                  [t_stg[b], t_c], [t_win])
        else:
            sc.op(V, lambda: E[V].tensor_scalar(out=WinB[:, kc, :], in0=stg[b][:], scalar1=g1s[:, kc:kc + 1], scalar2=None,
                                                op0=ALU.mult), [t_stg[b], t_c], [t_win])
    for q in range(4):
        sc.op(P, lambda: E[P].tensor_copy(out=WfB[:, :, q * 32:q * 32 + 8], in_=WinB[:, :, 2560:2568]), [t_win], [t_wf])

    diagW = sb("diagW", [128, CW, 4, 128], BF16); t_diag = TR("diagW")
    for k in range(CW):
        for c in range(4):
            eng = V if (k + c) % 2 == 0 else P
            sc.op(eng, lambda: E[eng].tensor_scalar(out=diagW[:, k, c, :], in0=cst[:, 0:128],
                                                    scalar1=wdws[:, c * CW + k:c * CW + k + 1], scalar2=None, op0=ALU.mult),
                  [t_cst, t_c], [t_diag])

    tp, t_tp, pm, t_pm = mkpsum(2, 6)

    xs = [sb("xs%d" % i, [128, 4, D]) for i in range(2)]; t_xs = [TR("xs%d" % i) for i in range(2)]
    junk = sb("junk", [128, D], BF16); t_junk = TR("junk")
    ssq = sb("ssq", [128, 4]); t_ssq = TR("ssq")
    rstd = sb("rstd", [128, 4]); t_rstd = TR("rstd")
    hb = sb("hb", [128, 4, D], BF16); t_hb = TR("hb")
    hT = sb("hT", [128, 8, MT], BF16); t_hT = TR("hT")
    aT = [sb("aT%d" % i, [128, 4, 30 + MT], BF16) for i in range(2)]; t_aT = [TR("aT%d" % i) for i in range(2)]
    sg = sb("sg", [128, MT]); t_sg = TR("sg")
    QT = sb("QT", [128, 4, MT], BF16); t_QT = TR("QT")
    KT = sb("KT", [128, 4, MT], BF16); t_KT = TR("KT")
    Vt = sb("Vt", [128, 4, 8, 65], BF16); t_Vt = TR("Vt")
    ef = sb("ef", [128, MT]); t_ef = TR("ef")
    spf = sb("spf", [128, MT]); t_spf = TR("spf")
    cum = sb("cum", [128, MT]); t_cum = TR("cum")
    carry = sb("carry", [128, 1]); t_carry = TR("carry")
    hiall = sb("hiall", [128, MT], BF16); t_hiall = TR("hiall")
    arow = sb("arow", [128, MT], BF16); t_arow = TR("arow")
    uu = sb("uu", [128, 4, MT]); t_uu = TR("uu")
    usq = sb("usq", [128, 4, MT], BF16); t_usq = TR("usq")
    m1 = sb("m1", [128, MT]); t_m1 = TR("m1")
    rs1 = sb("rs1", [128, MT]); t_rs1 = TR("rs1")
    mixc = sb("mixc", [128, 4, MT], BF16); t_mixc = TR("mixc")
    t_Qd = TR("Qd"); t_Kd = TR("Kd"); t_Vd = TR("Vd"); t_Ad = TR("Ad"); t_Mcd = TR("Mcd")
    for t in (t_Qd, t_Kd, t_Vd, t_Ad, t_Mcd):
        t.multi = True

    sc.op(P, lambda: E[P].memset(Vt[:], 1.0), [], [t_Vt])
    sc.op(P, lambda: E[P].memset(carry[:], 0.0), [], [t_carry])
    sc.op(P, lambda: E[P].memset(aT[0][:], 0.0), [], [t_aT[0]])
    sc.op(P, lambda: E[P].memset(aT[1][:], 0.0), [], [t_aT[1]])

    x_v = x.rearrange("(m s p) d -> m p s d", p=128, s=4)

    def inproj(bank, tbank, col0, M=128, lhs_src=None):
        def f():
            ins = None
            for kc in range(8):
                lhsT = WinB[:, kc, col0:col0 + M] if lhs_src is None else lhs_src[:, kc, :]
                ins = E[PE].matmul(pm[bank][0:M, :], lhsT=lhsT, rhs=hT[:, kc, :], start=(kc == 0), stop=(kc == 7))
            return ins
        sc.op(PE, f, [t_win, t_wf, t_hT], [tbank])

    def ph_A0(m):
        xb = xs[m % 2]; txb = t_xs[m % 2]
        sc.dma(SP, xb[:], x_v[m], writes=[txb], owner=txb)
        for s in range(4):
            sc.op(A, lambda: E[A].activation(out=junk[:], in_=xb[:, s, :], func=AF.Square, accum_out=ssq[:, s:s + 1]),
                  [txb], [t_junk, t_ssq])
        sc.op(A, lambda: E[A].activation(out=rstd[:], in_=ssq[:], func=AF.Sqrt, scale=1.0 / D, bias=epsb[:, 0:1]),
              [t_ssq, t_eps], [t_rstd])
        sc.op(V, lambda: E[V].reciprocal(out=rstd[:], in_=rstd[:]), [t_rstd], [t_rstd])
        for s in range(4):
            sc.op(A, lambda: E[A].activation(out=hb[:, s, :], in_=xb[:, s, :], func=AF.Copy, scale=rstd[:, s:s + 1]),
                  [txb, t_rstd], [t_hb])

    def ph_A1(m):
        xb = xs[m % 2]; txb = t_xs[m % 2]
        ab = aT[m % 2]; tab = t_aT[m % 2]
        abp = aT[(m + 1) % 2]; tabp = t_aT[(m + 1) % 2]
        for s in range(4):
            tb = tp[s % 2]; ttb = t_tp[s % 2]

            def f():
                ins = None
                for kc in range(8):
                    ins = E[PE].transpose(out=tb[:, kc * 128:(kc + 1) * 128], in_=hb[:, s, kc * 128:(kc + 1) * 128],
                                          identity=identb[:])
                return ins
            sc.op(PE, f, [t_hb, t_identb], [ttb])
            eng = A if s % 2 == 0 else V
            if eng == A:
                sc.op(A, lambda: E[A].copy(out=hT[:, :, s * 128:(s + 1) * 128], in_=tb[:].rearrange("p (k t) -> p k t", k=8)),
                      [ttb], [t_hT])
            else:
                sc.op(V, lambda: E[V].tensor_copy(out=hT[:, :, s * 128:(s + 1) * 128],
                                                  in_=tb[:].rearrange("p (k t) -> p k t", k=8)), [ttb], [t_hT])
        if m > 0:
            sc.op(P, lambda: E[P].tensor_copy(out=ab[:, :, 0:30], in_=abp[:, :, MT:MT + 30]), [tabp], [tab])
        for c in range(4):
            inproj(0, t_pm[0], 512 + c * 128)
            sc.op(A, lambda: E[A].activation(out=sg[:], in_=pm[0][:], func=AF.Sigmoid), [t_pm[0]], [t_sg])
            inproj(1, t_pm[1], c * 128)
            sc.op(V, lambda: E[V].tensor_tensor(out=ab[:, c, 30:30 + MT], in0=pm[1][:], in1=sg[:], op=ALU.mult),
                  [t_pm[1], t_sg], [tab])
        for p in range(4):
            inproj(0, t_pm[0], 1024 + p * 128)
            sc.op(A, lambda: E[A].copy(out=QT[:, p, :], in_=pm[0][:]), [t_pm[0]], [t_QT])
            inproj(1, t_pm[1], 1536 + p * 128)
            sc.op(V, lambda: E[V].tensor_copy(out=KT[:, p, :], in_=pm[1][:]), [t_pm[1]], [t_KT])
        sc.dma(P, Qd[:, m].rearrange("a p t -> p a t"), QT[:], reads=[t_QT], writes=[t_Qd], owner=t_QT)
        sc.dma(P, Kd[:, m].rearrange("a p t -> p a t"), KT[:], reads=[t_KT], writes=[t_Kd], owner=t_KT)
        for s in range(4):
            bank = (s % 2)

            def f():
                ins = None
                for kc in range(8):
                    ins = E[PE].matmul(pm[bank][:], lhsT=hT[:, kc, s * 128:(s + 1) * 128], rhs=WinB[:, kc, 2048:2560],
                                       start=(kc == 0), stop=(kc == 7))
                return ins
            sc.op(PE, f, [t_win, t_hT], [t_pm[bank]])
            eng = A if s % 2 == 0 else V
            src = pm[bank][:].rearrange("p (h d) -> p h d", h=8)
            if eng == A:
                sc.op(A, lambda: E[A].copy(out=Vt[:, s, :, 0:64], in_=src), [t_pm[bank]], [t_Vt])
            else:
                sc.op(V, lambda: E[V].tensor_copy(out=Vt[:, s, :, 0:64], in_=src), [t_pm[bank]], [t_Vt])
        for a in range(4):
            sc.dma(P, Vd[a, m], Vt[:, :, 2 * a:2 * a + 2, :].rearrange("p s h c -> p s (h c)"),
                   reads=[t_Vt], writes=[t_Vd], owner=t_Vt)
        inproj(0, t_pm[0], 0, lhs_src=WfB)
        sc.op(A, lambda: E[A].activation(out=ef[:], in_=pm[0][:], func=AF.Exp, scale=-1.0, bias=negbs[:, 0:1]),
              [t_pm[0], t_c], [t_ef])
        sc.op(A, lambda: E[A].activation(out=spf[:], in_=ef[:], func=AF.Ln, bias=1.0), [t_ef], [t_spf])
        sc.op(V, lambda: E[V].tensor_tensor_scan(out=cum[:], data0=ones512[:], data1=spf[:], initial=carry[:, 0:1],
                                                 op0=ALU.mult, op1=ALU.subtract), [t_spf, t_carry, t_ones512], [t_cum])
        sc.op(V, lambda: E[V].tensor_copy(out=carry[:], in_=cum[:, MT - 1:MT]), [t_cum], [t_carry])
        sc.op(V, lambda: E[V].tensor_scalar(out=hiall[:], in0=cum[:], scalar1=8.0, scalar2=None, op0=ALU.mult), [t_cum], [t_hiall])
        sc.op(P, lambda: E[P].tensor_copy(out=arow[:], in_=hiall[:]), [t_hiall], [t_arow])
        for q in (1, 3):
            sc.op(V, lambda: E[V].scalar_tensor_tensor(out=arow[q * 32:q * 32 + 32, :], in0=cum[q * 32:q * 32 + 32, :], scalar=8.0,
                                                       in1=hiall[q * 32:q * 32 + 32, :], op0=ALU.mult, op1=ALU.subtract),
                  [t_cum, t_hiall, t_arow], [t_arow])
        sc.dma(P, Ad[m], arow[:], reads=[t_arow], writes=[t_Ad], owner=t_arow)
        for s in range(4):
            sc.op(PE, lambda: E[PE].transpose(out=pm[1][:, 0:8], in_=cum[0:8, s * 128:(s + 1) * 128], identity=cst[0:8, 0:8]),
                  [t_cum, t_cst], [t_pm[1]])
            sc.op(V, lambda: E[V].tensor_scalar(out=biasT[:, m * 4 + s, :], in0=pm[1][:, 0:8], scalar1=-1.0, scalar2=None,
                                                op0=ALU.mult), [t_pm[1]], [t_biasT])

    def stat(bank, src, tsrc):
        ones_ = onesb[:] if src is usq else cst[:, 128:256]

        def f():
            ins = None
            for c in range(4):
                ins = E[PE].matmul(pm[bank][:], lhsT=ones_, rhs=src[:, c, :], start=(c == 0), stop=(c == 3))
            return ins
        sc.op(PE, f, [t_cst, t_identb, tsrc], [t_pm[bank]])

    def ph_B1(m):
        ab = aT[m % 2]; tab = t_aT[m % 2]
        for c in range(4):
            bank = 2 + (c % 2)

            def f():
                ins = None
                for k in range(CW):
                    ins = E[PE].matmul(pm[bank][:], lhsT=diagW[:, k, c, :], rhs=ab[:, c, k:k + MT], start=(k == 0),
                                       stop=(k == CW - 1))
                return ins
            sc.op(PE, f, [t_diag, tab], [t_pm[bank]])
            sc.op(A, lambda: E[A].activation(out=uu[:, c, :], in_=pm[bank][:], func=AF.Identity, bias=bdws[:, c:c + 1]),
                  [t_pm[bank], t_c], [t_uu])
            sc.op(A, lambda: E[A].activation(out=usq[:, c, :], in_=pm[bank][:], func=AF.Square, bias=bdws[:, c:c + 1]),
                  [t_pm[bank], t_c], [t_usq])

        stat(4, uu, t_uu)
        stat(5, usq, t_usq)

    def ph_B2(m):
        sc.op(V, lambda: E[V].tensor_scalar(out=m1[:], in0=pm[4][:], scalar1=1.0 / 512, scalar2=None, op0=ALU.mult), [t_pm[4]], [t_m1])
        sc.op(V, lambda: E[V].tensor_tensor(out=rs1[:], in0=m1[:], in1=m1[:], op=ALU.mult), [t_m1], [t_rs1])
        sc.op(V, lambda: E[V].scalar_tensor_tensor(out=rs1[:], in0=pm[5][:], scalar=1.0 / 512, in1=rs1[:], op0=ALU.mult,
                                                   op1=ALU.subtract), [t_pm[5], t_rs1], [t_rs1])
        sc.op(A, lambda: E[A].activation(out=rs1[:], in_=rs1[:], func=AF.Ln, bias=epsb[:, 0:1]), [t_rs1, t_eps], [t_rs1])
        sc.op(A, lambda: E[A].activation(out=rs1[:], in_=rs1[:], func=AF.Exp, scale=-0.5), [t_rs1], [t_rs1])
        for c in range(4):
            eng = V
            sc.op(eng, lambda: E[eng].tensor_tensor(out=uu[:, c, :], in0=uu[:, c, :], in1=m1[:], op=ALU.subtract), [t_uu, t_m1], [t_uu])
            sc.op(eng, lambda: E[eng].tensor_tensor(out=uu[:, c, :], in0=uu[:, c, :], in1=rs1[:], op=ALU.mult), [t_uu, t_rs1], [t_uu])
            sc.op(A, lambda: E[A].activation(out=uu[:, c, :], in_=uu[:, c, :], func=AF.Silu, scale=lngs[:, c:c + 1],
                                             bias=lnbs[:, c:c + 1]), [t_uu, t_c], [t_uu])
            sc.op(A, lambda: E[A].activation(out=usq[:, c, :], in_=uu[:, c, :], func=AF.Square), [t_uu], [t_usq])
        stat(4, usq, t_usq)
        sc.op(A, lambda: E[A].activation(out=rs1[:], in_=pm[4][:], func=AF.Ln, scale=1.0 / 512, bias=epsb[:, 0:1]),
              [t_pm[4], t_eps], [t_rs1])
        sc.op(A, lambda: E[A].activation(out=rs1[:], in_=rs1[:], func=AF.Exp, scale=-0.5), [t_rs1], [t_rs1])
        for c in range(4):
            eng = V
            sc.op(eng, lambda: E[eng].tensor_tensor(out=mixc[:, c, :], in0=uu[:, c, :], in1=rs1[:], op=ALU.mult),
                  [t_uu, t_rs1], [t_mixc])
        sc.dma(P, Mcd[m], mixc[:], reads=[t_mixc], writes=[t_Mcd], owner=t_mixc)


    ph_A0(0)
    ph_A1(0)
    for m in range(NM):
        if m + 1 < NM:
            ph_A0(m + 1)
        ph_B1(m)
        if m + 1 < NM:
            ph_A1(m + 1)
        ph_B2(m)

    if debug == "p1a":
        sc.wait_all(P, [t_Qd, t_Kd, t_Vd, t_Ad, t_Mcd, t_biasT])
        return nc

    new_phase()
    tp, t_tp, pm, t_pm = mkpsum(0, 8)
    NSB = 3
    Qs = [[sb("Qs%d_%d" % (i, j), [128, MT], BF16) for j in range(2)] for i in range(2)]
    t_Qs = [[TR("Qs%d_%d" % (i, j)) for j in range(2)] for i in range(2)]
    NKB = 3
    Kc = [[sb("Kc%d_%d" % (i, j), [128, MT], BF16) for j in range(2)] for i in range(NKB)]
    t_Kc = [[TR("Kc%d_%d" % (i, j)) for j in range(2)] for i in range(NKB)]
    Vc = [sb("Vc%d" % i, [128, 4, 2, 128], BF16) for i in range(NKB)]; t_Vc = [TR("Vc%d" % i) for i in range(NKB)]
    NPB = 4
    Pt = [sb("Pt%d" % i, [128, MT], BF16) for i in range(NPB)]; t_Pt = [TR("Pt%d" % i) for i in range(NPB)]
    Osb = [sb("Osb%d" % i, [65, MT]) for i in range(2)]; t_Osb = [TR("Osb%d" % i) for i in range(2)]
    Ya = [sb("Ya%d" % i, [64, 2, MT], BF16) for i in range(2)]; t_Ya = [TR("Ya%d" % i) for i in range(2)]
    for i_ in range(2):
        for j_ in range(2):
            sc.op(P, lambda: E[P].memset(Qs[i_][j_][:], 0.0), [], [t_Qs[i_][j_]])
            t_Qs[i_][j_].multi = True
    for i_ in range(NKB):
        for j_ in range(2):
            sc.op(P, lambda: E[P].memset(Kc[i_][j_][:], 0.0), [], [t_Kc[i_][j_]])
            sc.op(P, lambda: E[P].memset(Kc[i_][j_][64:66, :], 1.0), [t_Kc[i_][j_]], [t_Kc[i_][j_]])
        sc.op(P, lambda: E[P].memset(Vc[i_][:], 0.0), [], [t_Vc[i_]])
        t_Vc[i_].multi = True
    t_Yad = TR("Yad"); t_Yad.multi = True

    steps = []
    grp = 0
    for a in range(4):
        for i in range(NM):
            for c in range(i + 1):
                for hh in range(2):
                    for jj in range(4):
                        steps.append(dict(a=a, i=i, c=c, hh=hh, jj=jj, grp=grp,
                                          first=(c == 0 and jj == 0), last=(c == i and jj == 3)))
            grp += 1
    nld = 0
    pendB = []
    cur_q = {}
    cur_c = {}

    def emit_S(n):
        nonlocal nld
        st = steps[n]
        a, i, c, hh, jj = st["a"], st["i"], st["c"], st["hh"], st["jj"]
        g = st["grp"]
        if g not in cur_q:
            qb = g % 2
            for h2 in range(2):
                h_ = 2 * a + h2
                tq = t_Qs[qb][h2]
                sc.dma(SP, Qs[qb][h2][0:64, :], Qd[a, i, h2 * 64:(h2 + 1) * 64, :], reads=[t_Qd], writes=[tq], owner=tq)
                sc.dma(SP, Qs[qb][h2][64:65, :], Ad[i, h_:h_ + 1, :], reads=[t_Ad], writes=[tq], owner=tq)
                sc.dma(SP, Qs[qb][h2][65:66, :], Ad[i, 32 + h_:33 + h_, :], reads=[t_Ad], writes=[tq], owner=tq)
            cur_q[g] = qb
        if (g, c) not in cur_c:
            kb = nld % NKB
            nld += 1
            for h2 in range(2):
                sc.dma(SP, Kc[kb][h2][0:64, :], Kd[a, c, h2 * 64:(h2 + 1) * 64, :], reads=[t_Kd], writes=[t_Kc[kb][h2]],
                       owner=t_Kc[kb][h2])
                sc.dma(SP, Vc[kb][:, :, h2, 0:65], Vd[a, c, :, :, h2 * 65:(h2 + 1) * 65], reads=[t_Vd], writes=[t_Vc[kb]],
                       owner=t_Vc[kb])
            cur_c[(g, c)] = kb
        qb = cur_q[g]; kb = cur_c[(g, c)]
        st["qb"] = qb; st["kb"] = kb
        h = 2 * a + hh
        diag = (c == i)
        q0 = 128 * jj if diag else 0
        st["q0"] = q0; st["h"] = h
        bank = n % NSB
        st["bank"] = bank

        def f():
            ins = E[PE].matmul(pm[bank][:, q0:], lhsT=Kc[kb][hh][:, jj * 128:(jj + 1) * 128], rhs=Qs[qb][hh][:, q0:],
                               start=True, stop=not diag)
            if diag:
                ins = E[PE].matmul(pm[bank][:, q0:q0 + 128], lhsT=identb[:], rhs=negmb[:], start=False, stop=True)
            return ins
        sc.op(PE, f, [t_Kc[kb][hh], t_Qs[qb][hh], t_identb], [t_pm[bank]])

    def emit_exp(n):
        st = steps[n]
        bank = st["bank"]; q0 = st["q0"]; h = st["h"]
        j = 4 * st["c"] + st["jj"]
        pb = n % NPB
        sc.op(A, lambda: E[A].activation(out=Pt[pb][:, q0:], in_=pm[bank][:, q0:], func=AF.Exp, scale=0.125,
                                         bias=biasT[:, j, h:h + 1]), [t_pm[bank], t_biasT], [t_Pt[pb]])

    def emit_PV(n):
        st = steps[n]
        hh = st["hh"]; q0 = st["q0"]; kb = st["kb"]; jj = st["jj"]
        pb = n % NPB
        ob = 3 + 2 * (st["grp"] % 2) + hh
        sc.op(PE, lambda: E[PE].matmul(pm[ob][:, q0:], lhsT=Vc[kb][:, jj, hh, :], rhs=Pt[pb][:, q0:],
                                       start=st["first"], stop=st["last"]), [t_Vc[kb], t_Pt[pb]], [t_pm[ob]])
        if st["last"] and hh == 1:
            a, i = st["a"], st["i"]
            yb = st["grp"] % 2
            gsel = st["grp"] % 2
            for h2 in range(2):
                obk = 3 + 2 * gsel + h2
                sc.op(V, lambda: E[V].tensor_copy(out=Osb[h2][:], in_=pm[obk][0:65, :]), [t_pm[obk]], [t_Osb[h2]])
                sc.op(V, lambda: E[V].reciprocal(out=Osb[h2][64:65, :], in_=Osb[h2][64:65, :]), [t_Osb[h2]], [t_Osb[h2]])

            def partB(a=a, i=i, yb=yb):
                for h2 in range(2):
                    sc.op(PE, lambda: E[PE].matmul(pm[7][0:64, :], lhsT=e64[:], rhs=Osb[h2][:], start=True, stop=True),
                          [t_e64, t_Osb[h2]], [t_pm[7]])
                    sc.op(V, lambda: E[V].tensor_tensor(out=Ya[yb][:, h2, :], in0=Osb[h2][0:64, :], in1=pm[7][0:64, :], op=ALU.mult),
                          [t_Osb[h2], t_pm[7]], [t_Ya[yb]])
                sc.dma(P, Yad[i, :, 2 * a:2 * a + 2, :], Ya[yb][:], reads=[t_Ya[yb]], writes=[t_Yad], owner=t_Ya[yb])
            pendB.append([6, partB])

    NS = len(steps)
    LAG = 2
    for n in range(NS + LAG):
        if n < NS:
            emit_S(n)
            emit_exp(n)
        if n >= LAG:
            emit_PV(n - LAG)
        for pb_ in list(pendB):
            pb_[0] -= 1
            if pb_[0] <= 0:
                pb_[1]()
                pendB.remove(pb_)
    for pb_ in list(pendB):
        pb_[1]()

    if debug == "p1b":
        sc.wait_all(P, [t_Qd, t_Kd, t_Vd, t_Ad, t_Mcd, t_biasT, t_Yad])
        return nc

    new_phase()
    tp, t_tp, pm, t_pm = mkpsum(2, 6)
    t_Xmd = TR("Xmd"); t_Xmd.multi = True
    t_H2d = TR("H2d"); t_H2d.multi = True
    t_RTd = TR("RTd"); t_RTd.multi = True
    WoC = sb("WoC", [128, 4, D], BF16); t_WoC = TR("WoC")
    WoA = sb("WoA", [128, 4, D], BF16); t_WoA = TR("WoA")
    WrB = sb("WrB", [128, 8, 36], BF16); t_WrB = TR("WrB")
    wst = sb("wst", [128, 8, D]); t_wst = TR("wst")
    wrs = sb("wrs", [128, 8, 36]); t_wrs = TR("wrs")
    g2b = sb("g2b", [128, D]); t_g2b = TR("g2b")
    sc.dma(SP, g2b[:], g2bc, writes=[t_g2b], owner=t_g2b)
    sc.dma(SP, wst[:, 0:4, :], w_out[0:512, :].rearrange("(c p) n -> p c n", p=128), writes=[t_wst], owner=t_wst)
    for c in range(4):
        sc.op(V, lambda: E[V].tensor_scalar(out=WoC[:, c, :], in0=wst[:, c, :], scalar1=gconvs[:, c:c + 1], scalar2=None,
                                            op0=ALU.mult), [t_wst, t_c], [t_WoC])
    sc.dma(SP, wst[:, 4:8, :], w_out[512:1024, :].rearrange("(a p) n -> p a n", p=128), writes=[t_wst], owner=t_wst)
    for a in range(4):
        sc.op(V, lambda: E[V].tensor_scalar(out=WoA[:, a, :], in0=wst[:, 4 + a, :], scalar1=gatts[:, a:a + 1], scalar2=None,
                                            op0=ALU.mult), [t_wst, t_c], [t_WoA])
    sc.dma(SP, wrs[:], wr.rearrange("(kc p) n -> p kc n", p=128), writes=[t_wrs], owner=t_wrs)
    sc.op(V, lambda: E[V].tensor_copy(out=WrB[:], in_=wrs[:]), [t_wrs], [t_WrB])
    NRT = (NE * S + 128) // 128
    rti = sb("rti", [128, NRT], I32); t_rti = TR("rti")
    sc.op(P, lambda: E[P].memset(rti[:], S), [], [t_rti])
    sc.dma(P, RTd.rearrange("(p n) o -> p (n o)", p=128), rti[:], reads=[t_rti], writes=[t_RTd], owner=t_rti)
    dp1 = sb("dp1", [128, NT], I32); dp2 = sb("dp2", [128, NT], I32)
    zrow = sb("zrow", [128, D], BF16); t_zrow = TR("zrow")
    sc.op(P, lambda: E[P].memset(zrow[:], 0.0), [], [t_zrow])
    sc.dma(P, H2d[S:S + 128, :], zrow[:], reads=[t_zrow], writes=[t_H2d], owner=t_zrow)

    mcs = [sb("mcs%d" % i, [128, 4, MT], BF16) for i in range(2)]; t_mcs = [TR("mcs%d" % i) for i in range(2)]
    yas = [sb("yas%d" % i, [128, 4, MT], BF16) for i in range(2)]; t_yas = [TR("yas%d" % i) for i in range(2)]
    for i_ in range(2):
        t_yas[i_].multi = True
    xs2 = [sb("xs2%d" % i, [128, 4, D]) for i in range(2)]; t_xs2 = [TR("xs2%d" % i) for i in range(2)]
    sqa = sb("sqa", [128, 4, MT], BF16); t_sqa = TR("sqa")
    r4 = sb("r4", [128, MT]); t_r4 = TR("r4")
    mixa = sb("mixa", [128, 4, MT], BF16); t_mixa = TR("mixa")
    xm = [sb("xm%d" % i, [128, D]) for i in range(4)]; t_xm = [TR("xm%d" % i) for i in range(4)]
    h2 = [sb("h2%d" % i, [128, D], BF16) for i in range(4)]; t_h2 = [TR("h2%d" % i) for i in range(4)]
    h2T = [sb("h2T%d" % i, [128, 8, 128], BF16) for i in range(4)]; t_h2T = [TR("h2T%d" % i) for i in range(4)]
    junk2 = sb("junk2", [128, D], BF16); t_junk2 = TR("junk2")
    R = []
    for i_ in range(4):
        R.append(dict(
            sm=sb("sm%d" % i_, [128, 32]), lg=sb("lg%d" % i_, [128, 36]), l2=sb("l2_%d" % i_, [128, 8]), l2m=sb("l2m%d" % i_, [128, 8]),
            oh1=sb("oh1_%d" % i_, [128, 8]), oh2=sb("oh2_%d" % i_, [128, 8]), ohg=sb("ohg%d" % i_, [128, 4]), e1=sb("e1_%d" % i_, [128, 4]),
            M1=sb("M1_%d" % i_, [128, 32]), M2=sb("M2_%d" % i_, [128, 32]), Mb=sb("Mb%d" % i_, [128, 32], BF16),
            Rf=sb("Rf%d" % i_, [128, 32]), tmpr=sb("tmpr%d" % i_, [128, 32]), tmpr2=sb("tmpr2_%d" % i_, [128, 32]),
            t=TR("rt%d" % i_), tlg=TR("lgp%d" % i_), tR=TR("Rp%d" % i_), tC=TR("Cp%d" % i_)))
    rank1 = sb("rank1", [128, NT]); rank2 = sb("rank2", [128, NT]); eid1 = sb("eid1", [128, NT]); eid2 = sb("eid2", [128, NT])
    t_rk = TR("rankeid")
    carryR = sb("carryR", [128, 32]); t_carryR = TR("carryR")
    sc.op(V, lambda: E[V].memset(carryR[:], 0.0), [], [t_carryR])
    lg4 = sb("lg4", [128, 4, 36]); mx4 = sb("mx4", [128, 4]); ohg4 = sb("ohg4", [128, 4, 4]); d14 = sb("d14", [128, 4, 4])
    e14 = sb("e14", [128, 4, 4]); se4 = sb("se4", [128, 4]); p1s4 = sb("p1s4", [128, 4]); prod4 = sb("prod4", [128, 4, 4, 8])
    l24 = sb("l24", [128, 4, 8]); m14 = sb("m14", [128, 4]); oh14 = sb("oh14", [128, 4, 8]); l2m4 = sb("l2m4", [128, 4, 8])
    m24 = sb("m24", [128, 4]); oh24 = sb("oh24", [128, 4, 8]); d214 = sb("d214", [128, 4]); ed4 = sb("ed4", [128, 4])
    den4 = sb("den4", [128, 4]); w14 = sb("w14", [128, 4]); w24 = sb("w24", [128, 4])
    M14 = sb("M14", [128, 4, 32]); M24 = sb("M24", [128, 4, 32]); Mb4 = sb("Mb4", [128, 4, 32], BF16)
    Rf4 = sb("Rf4", [128, 4, 32]); tmp4 = sb("tmp4", [128, 4, 32]); dpf4 = sb("dpf4", [128, 4])
    t_rt4 = TR("rt4")

    def vr(fn, reads=(), writes=()):
        sc.op(V, fn, list(reads) + [t_rt4], list(writes) + [t_rt4])

    def bc(ap2d, shape3, axis):
        pst, _ = ap2d.ap[0]
        est, n = ap2d.ap[1]
        if axis == 1:
            return bass.AP(tensor=ap2d.tensor, offset=ap2d.offset, ap=[[pst, 128], [0, shape3[1]], [est, n]])
        return bass.AP(tensor=ap2d.tensor, offset=ap2d.offset, ap=[[pst, 128], [est, n], [0, shape3[2]]])

    def load1c(m):
        b = m % 2
        sc.dma(SP, mcs[b][:], Mcd[m], reads=[t_Mcd], writes=[t_mcs[b]], owner=t_mcs[b])
        for hh in range(2):
            sc.dma(SP, yas[b][hh * 64:(hh + 1) * 64, :, :], Yad[m].rearrange("d (a two) t -> d a two t", two=2)[:, :, hh, :],
                   reads=[t_Yad], writes=[t_yas[b]], owner=t_yas[b])
        sc.dma(SP, xs2[b][:], x_v[m], writes=[t_xs2[b]], owner=t_xs2[b])

    load1c(0)
    for m in range(NM):
        b = m % 2
        if m + 1 < NM:
            load1c(m + 1)
        sc.op(A, lambda: E[A].activation(out=sqa[:], in_=yas[b][:], func=AF.Square), [t_yas[b]], [t_sqa])

        def f():
            ins = None
            for a in range(4):
                ins = E[PE].matmul(pm[5][:], lhsT=onesb[:], rhs=sqa[:, a, :], start=(a == 0), stop=(a == 3))
            return ins
        sc.op(PE, f, [t_identb, t_sqa], [t_pm[5]])
        sc.op(A, lambda: E[A].activation(out=r4[:], in_=pm[5][:], func=AF.Ln, scale=1.0 / 512, bias=epsb[:, 0:1]),
              [t_pm[5], t_eps], [t_r4])
        sc.op(A, lambda: E[A].activation(out=r4[:], in_=r4[:], func=AF.Exp, scale=-0.5), [t_r4], [t_r4])
        for a in range(4):
            sc.op(V, lambda: E[V].tensor_tensor(out=mixa[:, a, :], in0=yas[b][:, a, :], in1=r4[:], op=ALU.mult),
                  [t_yas[b], t_r4], [t_mixa])
        for s in range(4):
            tile = m * 4 + s
            rs_ = R[s]; sm = rs_["sm"]
            xb2 = xm[s]; txm = t_xm[s]
            hb2 = h2[s]; th2 = t_h2[s]
            for n in range(2):
                bank = n

                def f():
                    ins = None
                    for c in range(4):
                        ins = E[PE].matmul(pm[bank][:], lhsT=mcs[b][:, c, s * 128:(s + 1) * 128], rhs=WoC[:, c, n * 512:(n + 1) * 512],
                                           start=(c == 0), stop=False)
                    for a in range(4):
                        ins = E[PE].matmul(pm[bank][:], lhsT=mixa[:, a, s * 128:(s + 1) * 128], rhs=WoA[:, a, n * 512:(n + 1) * 512],
                                           start=False, stop=(a == 3))
                    return ins
                sc.op(PE, f, [t_mcs[b], t_mixa, t_WoC, t_WoA], [t_pm[bank]])
                sc.op(V, lambda: E[V].tensor_tensor(out=xb2[:, n * 512:(n + 1) * 512], in0=pm[bank][:],
                                                    in1=xs2[b][:, s, n * 512:(n + 1) * 512], op=ALU.add),
                      [t_pm[bank], t_xs2[b]], [txm])
            sc.dma(SP, Xmd[tile * 128:(tile + 1) * 128, :], xb2[:], reads=[txm], writes=[t_Xmd], owner=txm)
            sc.op(A, lambda: E[A].activation(out=junk2[:], in_=xb2[:], func=AF.Square, accum_out=sm[:, 0:1]),
                  [txm], [t_junk2, rs_["t"]])
            sc.op(A, lambda: E[A].activation(out=sm[:, 1:2], in_=sm[:, 0:1], func=AF.Sqrt, scale=1.0 / D, bias=epsb[:, 0:1]),
                  [rs_["t"], t_eps], [rs_["t"]])
            sc.op(V, lambda: E[V].reciprocal(out=sm[:, 2:3], in_=sm[:, 1:2]), [rs_["t"]], [rs_["t"]])
            sc.op(V, lambda: E[V].scalar_tensor_tensor(out=hb2[:], in0=xb2[:], scalar=sm[:, 2:3], in1=g2b[:], op0=ALU.mult,
                                                       op1=ALU.mult), [txm, rs_["t"], t_g2b], [th2])
            sc.dma(SP, H2d[tile * 128:(tile + 1) * 128, :], hb2[:], reads=[th2], writes=[t_H2d], owner=th2)
        for s in range(4):
            tile = m * 4 + s
            rs_ = R[s]
            hb2 = h2[s]; th2 = t_h2[s]
            tb = tp[tile % 2]; ttb = t_tp[tile % 2]

            def f():
                ins = None
                for kc in range(8):
                    ins = E[PE].transpose(out=tb[:, kc * 128:(kc + 1) * 128], in_=hb2[:, kc * 128:(kc + 1) * 128], identity=identb[:])
                return ins
            sc.op(PE, f, [th2, t_identb], [ttb])
            sc.op(A, lambda: E[A].copy(out=h2T[s][:], in_=tb[:].rearrange("p (k t) -> p k t", k=8)), [ttb], [t_h2T[s]])

            def f():
                ins = None
                for kc in range(8):
                    ins = E[PE].matmul(pm[2][:, s * 64:s * 64 + 36], lhsT=h2T[s][:, kc, :], rhs=WrB[:, kc, :], start=(kc == 0),
                                       stop=(kc == 7))
                return ins
            sc.op(PE, f, [t_h2T[s], t_WrB], [rs_["tlg"]])

        c0 = 4 * m
        lgp = pm[2][:, 0:256].rearrange("p (s c) -> p s c", c=64)[:, :, 0:36]
        vr(lambda: E[V].tensor_tensor(out=lg4[:], in0=lgp, in1=bc(rbs[:], [128, 4, 36], 1), op=ALU.add),
           [R[0]["tlg"], R[1]["tlg"], R[2]["tlg"], R[3]["tlg"], t_c])
        vr(lambda: E[V].reduce_max(out=mx4[:], in_=lg4[:, :, 0:4], axis=AX.X))
        vr(lambda: E[V].tensor_tensor(out=ohg4[:], in0=lg4[:, :, 0:4], in1=bc(mx4[:], [128, 4, 4], 2), op=ALU.is_equal))
        vr(lambda: E[V].tensor_tensor(out=d14[:], in0=lg4[:, :, 0:4], in1=bc(mx4[:], [128, 4, 4], 2), op=ALU.subtract))
        sc.op(A, lambda: E[A].activation(out=e14[:], in_=d14[:], func=AF.Exp), [t_rt4], [t_rt4])
        vr(lambda: E[V].reduce_sum(out=se4[:], in_=e14[:], axis=AX.X))
        vr(lambda: E[V].reciprocal(out=p1s4[:], in_=se4[:]))
        vr(lambda: E[V].tensor_tensor(out=prod4[:], in0=lg4[:, :, 4:36].rearrange("p s (g e) -> p s g e", e=8),
                                      in1=bass.AP(tensor=ohg4[:].tensor, offset=ohg4[:].offset,
                                                  ap=[[16, 128], [4, 4], [1, 4], [0, 8]]), op=ALU.mult))
        vr(lambda: E[V].reduce_sum(out=l24[:], in_=prod4[:].rearrange("p s g e -> p s e g"), axis=AX.X))
        vr(lambda: E[V].reduce_max(out=m14[:], in_=l24[:], axis=AX.X))
        vr(lambda: E[V].tensor_tensor(out=oh14[:], in0=l24[:], in1=bc(m14[:], [128, 4, 8], 2), op=ALU.is_equal))
        vr(lambda: E[V].scalar_tensor_tensor(out=l2m4[:], in0=oh14[:], scalar=-1e30, in1=l24[:], op0=ALU.mult, op1=ALU.add))
        vr(lambda: E[V].reduce_max(out=m24[:], in_=l2m4[:], axis=AX.X))
        vr(lambda: E[V].tensor_tensor(out=oh24[:], in0=l2m4[:], in1=bc(m24[:], [128, 4, 8], 2), op=ALU.is_equal))
        vr(lambda: E[V].tensor_tensor(out=d214[:], in0=m24[:], in1=m14[:], op=ALU.subtract))
        sc.op(A, lambda: E[A].activation(out=ed4[:], in_=d214[:], func=AF.Exp), [t_rt4], [t_rt4])
        vr(lambda: E[V].tensor_scalar(out=den4[:], in0=ed4[:], scalar1=1.0, scalar2=None, op0=ALU.add))
        vr(lambda: E[V].reciprocal(out=w14[:], in_=den4[:]))
        vr(lambda: E[V].tensor_tensor(out=wgt1[:, c0:c0 + 4], in0=w14[:], in1=p1s4[:], op=ALU.mult), [], [t_route])
        vr(lambda: E[V].tensor_tensor(out=w24[:], in0=ed4[:], in1=w14[:], op=ALU.mult))
        vr(lambda: E[V].tensor_tensor(out=wgt2[:, c0:c0 + 4], in0=w24[:], in1=p1s4[:], op=ALU.mult), [], [t_route])
        ohg_b = bass.AP(tensor=ohg4[:].tensor, offset=ohg4[:].offset, ap=[[16, 128], [4, 4], [1, 4], [0, 8]])
        oh1_b = bass.AP(tensor=oh14[:].tensor, offset=oh14[:].offset, ap=[[32, 128], [8, 4], [0, 4], [1, 8]])
        oh2_b = bass.AP(tensor=oh24[:].tensor, offset=oh24[:].offset, ap=[[32, 128], [8, 4], [0, 4], [1, 8]])
        vr(lambda: E[V].tensor_tensor(out=M14[:].rearrange("p s (g e) -> p s g e", e=8), in0=ohg_b, in1=oh1_b, op=ALU.mult))
        vr(lambda: E[V].tensor_tensor(out=M24[:].rearrange("p s (g e) -> p s g e", e=8), in0=ohg_b, in1=oh2_b, op=ALU.mult))
        vr(lambda: E[V].tensor_tensor(out=Mb4[:], in0=M14[:], in1=M24[:], op=ALU.add))

        def fR():
            ins = None
            for s_ in range(4):
                E[PE].matmul(pm[3][:, s_ * 32:(s_ + 1) * 32], lhsT=lstrb[:], rhs=Mb4[:, s_, :], start=True, stop=True)
                ins = E[PE].matmul(pm[4][:, s_ * 32:(s_ + 1) * 32], lhsT=onesb[:], rhs=Mb4[:, s_, :], start=True, stop=True)
            return ins
        sc.op(PE, fR, [t_identb, t_rt4], [t_pm[3], t_pm[4]])
        for s_ in range(4):
            vr(lambda: E[V].tensor_tensor(out=Rf4[:, s_, :], in0=pm[3][:, s_ * 32:(s_ + 1) * 32], in1=carryR[:], op=ALU.add),
               [t_pm[3], t_carryR])
            sc.op(V, lambda: E[V].tensor_tensor(out=carryR[:], in0=carryR[:], in1=pm[4][:, s_ * 32:(s_ + 1) * 32], op=ALU.add),
                  [t_pm[4], t_carryR], [t_carryR])
        for (Mx, rk, ei) in ((M14, rank1, eid1), (M24, rank2, eid2)):
            vr(lambda: E[V].tensor_tensor(out=tmp4[:], in0=Rf4[:], in1=Mx[:], op=ALU.mult))
            vr(lambda: E[V].reduce_sum(out=rk[:, c0:c0 + 4], in_=tmp4[:], axis=AX.X), [], [t_rk])
            vr(lambda: E[V].tensor_tensor(out=tmp4[:], in0=bc(ebs[:], [128, 4, 32], 1), in1=Mx[:], op=ALU.mult), [t_c])
            vr(lambda: E[V].reduce_sum(out=ei[:, c0:c0 + 4], in_=tmp4[:], axis=AX.X), [], [t_rk])
        for (ei, rk, dpp) in ((eid1, rank1, dp1), (eid2, rank2, dp2)):
            vr(lambda: E[V].scalar_tensor_tensor(out=dpf4[:], in0=ei[:, c0:c0 + 4], scalar=float(S), in1=rk[:, c0:c0 + 4],
                                                 op0=ALU.mult, op1=ALU.add), [t_rk])
            vr(lambda: E[V].tensor_copy(out=dpp[:, c0:c0 + 4], in_=dpf4[:]))
            for s_ in range(4):
                tile = c0 + s_
                sc.dma(P, None, None, reads=[t_rt4, t_c], writes=[t_RTd], owner=t_rt4,
                       fn=lambda: E[P].indirect_dma_start(out=RTd[:, :], out_offset=bass.IndirectOffsetOnAxis(ap=dpp[:, tile:tile + 1], axis=0),
                                                          in_=iotas[:, tile:tile + 1], in_offset=None,
                                                          bounds_check=rb_rt, oob_is_err=False))

    def bc(ap2d, shape3, axis):
        pst, _ = ap2d.ap[0]
        est, n = ap2d.ap[1]
        if axis == 1:
            return bass.AP(tensor=ap2d.tensor, offset=ap2d.offset, ap=[[pst, 128], [0, shape3[1]], [est, n]])
        return bass.AP(tensor=ap2d.tensor, offset=ap2d.offset, ap=[[pst, 128], [est, n], [0, shape3[2]]])
    cmpk = sb("cmpk", [128, 32, 33]); ntl = sb("ntl", [128, 32]); padd = sb("padd", [128, 32]); pend = sb("pend", [128, 32])
    pstt = sb("pstt", [128, 32]); bigt = sb("bigt", [128, NT, 32]); psel = sb("psel", [128, NT]); destf = sb("destf", [128, NT])
    cmpt = sb("cmpt", [128, NTL, 32]); tef = sb("tef", [128, NTL]); widf = sb("widf", [128, NTL])
    t_fin = TR("fin")

    def fop(fn, reads=(), writes=()):
        sc.op(V, fn, list(reads) + [t_fin], list(writes) + [t_fin])
    fop(lambda: E[V].tensor_tensor(out=cmpk[:], in0=bc(carryR[:], [128, 32, 33], 2), in1=bc(thrs[:], [128, 32, 33], 1), op=ALU.is_gt),
        [t_carryR, t_c])
    fop(lambda: E[V].reduce_sum(out=ntl[:], in_=cmpk[:], axis=AX.X))
    fop(lambda: E[V].tensor_scalar(out=padd[:], in0=ntl[:], scalar1=float(TS), scalar2=None, op0=ALU.mult))
    fop(lambda: E[V].tensor_tensor_scan(out=pend[:], data0=ones512[:, 0:32], data1=padd[:], initial=0.0, op0=ALU.mult, op1=ALU.add),
        [t_ones512])
    fop(lambda: E[V].tensor_tensor(out=pstt[:], in0=pend[:], in1=padd[:], op=ALU.subtract))
    for (rk, ei, dd) in ((rank1, eid1, dest1), (rank2, eid2, dest2)):
        fop(lambda: E[V].tensor_tensor(out=bigt[:], in0=bc(ei[:], [128, NT, 32], 2), in1=bc(ebs[:], [128, NT, 32], 1), op=ALU.is_equal),
            [t_rk, t_c])
        fop(lambda: E[V].tensor_tensor(out=bigt[:], in0=bigt[:], in1=bc(pstt[:], [128, NT, 32], 1), op=ALU.mult))
        fop(lambda: E[V].reduce_sum(out=psel[:], in_=bigt[:], axis=AX.X))
        fop(lambda: E[V].tensor_tensor(out=destf[:], in0=psel[:], in1=rk[:], op=ALU.add), [t_rk])
        fop(lambda: E[V].tensor_copy(out=dd[:], in_=destf[:]), [], [t_route])
    fop(lambda: E[V].tensor_tensor(out=cmpt[:], in0=bc(pend[:], [128, NTL, 32], 1), in1=bc(tsts[:], [128, NTL, 32], 2), op=ALU.is_le),
        [t_c])
    fop(lambda: E[V].reduce_sum(out=tef[:], in_=cmpt[:], axis=AX.X))
    fop(lambda: E[V].tensor_scalar(out=widf[:], in0=tef[:], scalar1=128.0, scalar2=pcols[:, 0:1], op0=ALU.mult, op1=ALU.add), [t_c])
    eqf = sb("eqf", [128, NTL])
    fop(lambda: E[V].memset(eqf[:], 0.0))
    fop(lambda: E[V].tensor_tensor(out=eqf[:, 2:NTL], in0=tef[:, 2:NTL], in1=tef[:, 0:NTL - 2], op=ALU.is_equal))
    fop(lambda: E[V].scalar_tensor_tensor(out=widf[:], in0=eqf[:], scalar=float(NE * 128), in1=widf[:], op0=ALU.mult, op1=ALU.add))
    fop(lambda: E[V].tensor_copy(out=widx[:], in_=widf[:]), [], [t_widx])
    pstl = sb("pstl", [128, NTL]); basef = sb("basef", [128, NTL]); rif = sb("rif", [128, NTL])
    fop(lambda: E[V].tensor_tensor(out=cmpt[:], in0=bc(tef[:], [128, NTL, 32], 2), in1=bc(ebs[:], [128, NTL, 32], 1), op=ALU.is_equal),
        [t_c])
    fop(lambda: E[V].tensor_tensor(out=cmpt[:], in0=cmpt[:], in1=bc(pstt[:], [128, NTL, 32], 1), op=ALU.mult))
    fop(lambda: E[V].reduce_sum(out=pstl[:], in_=cmpt[:], axis=AX.X))
    fop(lambda: E[V].scalar_tensor_tensor(out=basef[:], in0=tef[:], scalar=float(S), in1=tsts[:], op0=ALU.mult, op1=ALU.add), [t_c])
    fop(lambda: E[V].tensor_tensor(out=basef[:], in0=basef[:], in1=pstl[:], op=ALU.subtract))
    for st in range(TS // 128):
        fop(lambda: E[V].tensor_scalar(out=rif[:], in0=basef[:], scalar1=pcols[:, 0:1], scalar2=float(128 * st), op0=ALU.add, op1=ALU.add),
            [t_c])
        fop(lambda: E[V].tensor_copy(out=ridx[:, :, st], in_=rif[:]), [], [t_widx])

    new_phase()
    tp, t_tp, pm, t_pm = mkpsum(2, 6)
    t_Ysd = TR("Ysd"); t_Ysd.multi = True
    NSUB = TS // 128
    Wg = [sb("Wg%d" % i, [128, 8, DE], BF16) for i in range(2)]; t_Wg = [TR("Wg%d" % i) for i in range(2)]
    Wu = [sb("Wu%d" % i, [128, 8, DE], BF16) for i in range(2)]; t_Wu = [TR("Wu%d" % i) for i in range(2)]
    Wd = [sb("Wd%d" % i, [128, 2, D], BF16) for i in range(2)]; t_Wd = [TR("Wd%d" % i) for i in range(2)]
    Wgs = [sb("Wgs%d" % i, [128, 8 * DE]) for i in range(2)]; t_Wgs = [TR("Wgs%d" % i) for i in range(2)]
    Wus = [sb("Wus%d" % i, [128, 8 * DE]) for i in range(2)]; t_Wus = [TR("Wus%d" % i) for i in range(2)]
    Wds = [sb("Wds%d" % i, [128, 2 * D]) for i in range(2)]; t_Wds = [TR("Wds%d" % i) for i in range(2)]
    for i_ in range(2):
        sc.op(V, lambda: E[V].memset(Wgs[i_][:], 0.0), [], [t_Wgs[i_]])
        sc.op(V, lambda: E[V].memset(Wus[i_][:], 0.0), [], [t_Wus[i_]])
        sc.op(V, lambda: E[V].memset(Wds[i_][:], 0.0), [], [t_Wds[i_]])
    idxs = [[sb("idx%d_%d" % (i, j), [128, 1], I32) for j in range(NSUB)] for i in range(2)]
    t_idx = [[TR("idx%d_%d" % (i, j)) for j in range(NSUB)] for i in range(2)]
    Xg = [[sb("Xg%d_%d" % (i, j), [128, D], BF16) for j in range(NSUB)] for i in range(2)]
    t_Xg = [[TR("Xg%d_%d" % (i, j)) for j in range(NSUB)] for i in range(2)]
    XgT = [sb("XgT%d" % i, [128, 8, TS], BF16) for i in range(2)]; t_XgT = [TR("XgT%d" % i) for i in range(2)]
    sgl = [sb("sgl%d" % i, [128, TS]) for i in range(2)]; t_sgl = [TR("sgl%d" % i) for i in range(2)]
    AT = [sb("AT%d" % i, [128, 2, TS], BF16) for i in range(2)]; t_AT = [TR("AT%d" % i) for i in range(2)]
    NYB = 4
    Yt = [sb("Yt%d" % i, [128, D], BF16) for i in range(NYB)]; t_Yt = [TR("Yt%d" % i) for i in range(NYB)]

    def L2(t):
        b = t % 2
        for st in range(NSUB):
            sc.dma(P, None, None, reads=[t_widx, t_RTd], writes=[t_idx[b][st]], owner=t_idx[b][st],
                   fn=lambda: E[P].indirect_dma_start(out=idxs[b][st][:, :], out_offset=None, in_=RTd[:, :],
                                                      in_offset=bass.IndirectOffsetOnAxis(ap=ridx[:, t, st:st + 1], axis=0),
                                                      bounds_check=rb_rt, oob_is_err=False))
        for (wt, twt, src) in ((Wgs[b], t_Wgs[b], w_gate), (Wus[b], t_Wus[b], w_up), (Wds[b], t_Wds[b], w_down)):
            sc.dma(P, None, None, reads=[t_widx], writes=[twt], owner=twt,
                   fn=lambda: E[P].indirect_dma_start(out=wt[:, :], out_offset=None, in_=src[:, :],
                                                      in_offset=bass.IndirectOffsetOnAxis(ap=widx[:, t:t + 1], axis=0),
                                                      bounds_check=rb_w, oob_is_err=False))
        for st in range(NSUB):
            sc.dma(P, None, None, reads=[t_idx[b][st], t_H2d], writes=[t_Xg[b][st]], owner=t_Xg[b][st],
                   fn=lambda: E[P].indirect_dma_start(out=Xg[b][st][:, :], out_offset=None, in_=H2d[:, :],
                                                      in_offset=bass.IndirectOffsetOnAxis(ap=idxs[b][st][:, 0:1], axis=0),
                                                      bounds_check=rb_h2, oob_is_err=False))

    def C2(t):
        b = t % 2
        sc.op(V, lambda: E[V].tensor_copy(out=Wg[b][:].rearrange("p k n -> p (k n)"), in_=Wgs[b][:]), [t_Wgs[b]], [t_Wg[b]])
        sc.op(A, lambda: E[A].copy(out=Wu[b][:].rearrange("p k n -> p (k n)"), in_=Wus[b][:]), [t_Wus[b]], [t_Wu[b]])
        sc.op(V, lambda: E[V].tensor_copy(out=Wd[b][:].rearrange("p k n -> p (k n)"), in_=Wds[b][:]), [t_Wds[b]], [t_Wd[b]])

    def T2(t):
        b = t % 2
        for st in range(NSUB):
            tb = tp[st % 2]; ttb = t_tp[st % 2]

            def f():
                ins = None
                for kc in range(8):
                    ins = E[PE].transpose(out=tb[:, kc * 128:(kc + 1) * 128], in_=Xg[b][st][:, kc * 128:(kc + 1) * 128],
                                          identity=identb[:])
                return ins
            sc.op(PE, f, [t_Xg[b][st], t_identb], [ttb])
            if st % 2 == 0:
                sc.op(A, lambda: E[A].copy(out=XgT[b][:, :, st * 128:(st + 1) * 128], in_=tb[:].rearrange("p (k t) -> p k t", k=8)),
                      [ttb], [t_XgT[b]])
            else:
                sc.op(V, lambda: E[V].tensor_copy(out=XgT[b][:, :, st * 128:(st + 1) * 128],
                                                  in_=tb[:].rearrange("p (k t) -> p k t", k=8)), [ttb], [t_XgT[b]])

    def M2a(t):
        b = t % 2
        N = TS
        for fc in range(2):
            bg = 2 * fc; bu = 2 * fc + 1

            def fg():
                ins = None
                for kc in range(8):
                    ins = E[PE].matmul(pm[bg][:, 0:N], lhsT=Wg[b][:, kc, fc * 128:(fc + 1) * 128], rhs=XgT[b][:, kc, 0:N],
                                       start=(kc == 0), stop=(kc == 7))
                return ins

            def fu():
                ins = None
                for kc in range(8):
                    ins = E[PE].matmul(pm[bu][:, 0:N], lhsT=Wu[b][:, kc, fc * 128:(fc + 1) * 128], rhs=XgT[b][:, kc, 0:N],
                                       start=(kc == 0), stop=(kc == 7))
                return ins
            sc.op(PE, fg, [t_Wg[b], t_XgT[b]], [t_pm[bg]])
            sc.op(PE, fu, [t_Wu[b], t_XgT[b]], [t_pm[bu]])
            sc.op(A, lambda: E[A].activation(out=sgl[fc][:, 0:N], in_=pm[bg][:, 0:N], func=AF.Silu), [t_pm[bg]], [t_sgl[fc]])
            sc.op(V, lambda: E[V].tensor_tensor(out=AT[b][:, fc, 0:N], in0=pm[bu][:, 0:N], in1=sgl[fc][:, 0:N], op=ALU.mult),
                  [t_pm[bu], t_sgl[fc]], [t_AT[b]])

    ny = [0]

    def M2b(t):
        b = t % 2
        for st in range(NSUB):
            yb = ny[0] % NYB; ny[0] += 1
            for n in range(2):
                bank = 4 + n

                def fy():
                    ins = None
                    for fc in range(2):
                        ins = E[PE].matmul(pm[bank][:], lhsT=AT[b][:, fc, st * 128:(st + 1) * 128],
                                           rhs=Wd[b][:, fc, n * 512:(n + 1) * 512], start=(fc == 0), stop=(fc == 1))
                    return ins
                sc.op(PE, fy, [t_AT[b], t_Wd[b]], [t_pm[bank]])
                if n == 0:
                    sc.op(A, lambda: E[A].copy(out=Yt[yb][:, 0:512], in_=pm[bank][:]), [t_pm[bank]], [t_Yt[yb]])
                else:
                    sc.op(V, lambda: E[V].tensor_copy(out=Yt[yb][:, 512:1024], in_=pm[bank][:]), [t_pm[bank]], [t_Yt[yb]])
            r0 = t * TS + st * 128
            sc.dma(SP, Ysd[r0:r0 + 128, :], Yt[yb][:], reads=[t_Yt[yb]], writes=[t_Ysd], owner=t_Yt[yb])

    L2(0); C2(0); T2(0)
    for t in range(NTL):
        if t + 1 < NTL:
            L2(t + 1)
        M2a(t)
        if t + 1 < NTL:
            C2(t + 1)
        M2b(t)
        if t + 1 < NTL:
            T2(t + 1)

    new_phase()
    t_out = TR("out"); t_out.multi = True
    fgs = sb("fgs", [128, D]); t_fgs = TR("fgs")
    sc.dma(SP, fgs[:], fing, writes=[t_fgs], owner=t_fgs)
    NB3 = 4
    xm3 = [sb("xm3%d" % i, [128, D]) for i in range(NB3)]; t_xm3 = [TR("xm3%d" % i) for i in range(NB3)]
    y1 = [sb("y1%d" % i, [128, D], BF16) for i in range(NB3)]; t_y1 = [TR("y1%d" % i) for i in range(NB3)]
    y2 = [sb("y2%d" % i, [128, D], BF16) for i in range(NB3)]; t_y2 = [TR("y2%d" % i) for i in range(NB3)]
    ot = [sb("ot%d" % i, [128, D]) for i in range(2)]; t_ot = [TR("ot%d" % i) for i in range(2)]
    junk3 = sb("junk3", [128, D], BF16); t_junk3 = TR("junk3")
    sm3 = [sb("sm3_%d" % i, [128, 4]) for i in range(2)]; t_sm3 = [TR("sm3_%d" % i) for i in range(2)]

    def L3(t):
        b = t % NB3
        sc.dma(SP, xm3[b][:], Xmd[t * 128:(t + 1) * 128, :], reads=[t_Xmd], writes=[t_xm3[b]], owner=t_xm3[b])
        for (yy, tyy, dd) in ((y1[b], t_y1[b], dest1), (y2[b], t_y2[b], dest2)):
            sc.dma(P, None, None, reads=[t_route, t_Ysd], writes=[tyy], owner=tyy,
                   fn=lambda: E[P].indirect_dma_start(out=yy[:, :], out_offset=None, in_=Ysd[:, :],
                                                      in_offset=bass.IndirectOffsetOnAxis(ap=dd[:, t:t + 1], axis=0),
                                                      bounds_check=rb_ys, oob_is_err=False))

    def C3(t):
        b = t % NB3
        ob = t % 2
        s3 = sm3[ob]; ts3 = t_sm3[ob]
        sc.op(V, lambda: E[V].scalar_tensor_tensor(out=xm3[b][:], in0=y1[b][:], scalar=wgt1[:, t:t + 1], in1=xm3[b][:],
                                                   op0=ALU.mult, op1=ALU.add), [t_y1[b], t_xm3[b], t_route], [t_xm3[b]])
        sc.op(V, lambda: E[V].scalar_tensor_tensor(out=xm3[b][:], in0=y2[b][:], scalar=wgt2[:, t:t + 1], in1=xm3[b][:],
                                                   op0=ALU.mult, op1=ALU.add), [t_y2[b], t_xm3[b], t_route], [t_xm3[b]])
        sc.op(A, lambda: E[A].activation(out=junk3[:], in_=xm3[b][:], func=AF.Square, accum_out=s3[:, 0:1]),
              [t_xm3[b]], [t_junk3, ts3])
        sc.op(A, lambda: E[A].activation(out=s3[:, 1:2], in_=s3[:, 0:1], func=AF.Sqrt, scale=1.0 / D, bias=epsb[:, 0:1]),
              [ts3, t_eps], [ts3])
        sc.op(V, lambda: E[V].reciprocal(out=s3[:, 2:3], in_=s3[:, 1:2]), [ts3], [ts3])
        sc.op(V, lambda: E[V].scalar_tensor_tensor(out=ot[ob][:], in0=xm3[b][:], scalar=s3[:, 2:3], in1=fgs[:],
                                                   op0=ALU.mult, op1=ALU.mult), [t_xm3[b], ts3, t_fgs], [t_ot[ob]])
        sc.dma(SP, out[t * 128:(t + 1) * 128, :], ot[ob][:], reads=[t_ot[ob]], writes=[t_out], owner=t_ot[ob])

    for t in range(min(2, NT)):
        L3(t)
    for t in range(NT):
        if t + 2 < NT:
            L3(t + 2)
        C3(t)
    sc.wait_all(P, [t_out])
    barrier()
    return nc


def _prep_inputs(inp, b, S):
    f = np.float32

    def T8(v):
        return np.ascontiguousarray(v.reshape(8, 128).T).astype(f)

    def T4(v):
        return np.ascontiguousarray(v.reshape(4, 128).T).astype(f)
    d = {}
    d["x"] = np.ascontiguousarray(inp["x"][b][:S])
    d["w_in"] = inp["w_in"][0]
    d["w_out"] = inp["w_out"][0]
    d["wr"] = np.ascontiguousarray(np.concatenate([inp["w_r1"][0]] + [inp["w_r2"][0][g] for g in range(4)], axis=1))
    d["w_gate"] = np.ascontiguousarray(inp["w_gate"][0].reshape(32, 8, 128, 256).transpose(0, 2, 1, 3).reshape(32 * 128, 2048))
    d["w_up"] = np.ascontiguousarray(inp["w_up"][0].reshape(32, 8, 128, 256).transpose(0, 2, 1, 3).reshape(32 * 128, 2048))
    d["w_down"] = np.ascontiguousarray(inp["w_down"][0].reshape(32, 2, 128, 1024).transpose(0, 2, 1, 3).reshape(32 * 128, 2048))
    d["g1T"] = T8(inp["norm1_g"][0])
    d["g2T"] = T8(inp["norm2_g"][0])
    d["gconvT"] = T4(inp["out_g_conv"][0])
    d["gattT"] = T4(inp["out_g_att"][0])
    d["bdwT"] = T4(inp["b_dw"][0])
    d["lngT"] = T4(inp["conv_ln_g"][0])
    d["lnbT"] = T4(inp["conv_ln_b"][0])
    wd = inp["w_dw"][0]
    d["wdwT"] = np.ascontiguousarray(wd.reshape(31, 4, 128).transpose(2, 1, 0).reshape(128, 124))
    nb = np.zeros((128, 1), f)
    for q in range(4):
        nb[q * 32:q * 32 + 8, 0] = -inp["b_f"][0]
    d["negbf"] = nb
    d["fing"] = np.ascontiguousarray(np.broadcast_to(inp["final_g"][None, :], (128, 1024))).astype(f)
    rb = np.concatenate([inp["b_r1"][0], inp["b_r2"][0].reshape(-1)])
    d["g2bc"] = np.ascontiguousarray(np.broadcast_to(inp["norm2_g"][0][None, :], (128, 1024))).astype(f)
    d["rbias"] = np.ascontiguousarray(np.broadcast_to(rb[None, :], (128, 36))).astype(f)
    c = np.zeros((128, 640), f)
    c[:, 0:128] = np.eye(128)
    c[:, 128:256] = 1.0
    c[:, 256:384] = np.triu(np.ones((128, 128)), 1)
    c[:, 384:512] = np.tril(np.ones((128, 128)), -1) * -30000.0
    c[:, 512:640] = 1.0
    d["consts"] = c
    d["ebase"] = np.ascontiguousarray(np.broadcast_to(np.arange(32)[None, :], (128, 32))).astype(f)
    d["thr256"] = np.ascontiguousarray(np.broadcast_to((np.arange(33) * TS)[None, :], (128, 33))).astype(f)
    NTL = NE * CAP // TS
    d["tst256"] = np.ascontiguousarray(np.broadcast_to((np.arange(NTL) * TS)[None, :], (128, NTL))).astype(f)
    d["pcol"] = np.arange(128).reshape(128, 1).astype(f)
    NT = S // 128
    d["iota_tok"] = (np.arange(NT)[None, :] * 128 + np.arange(128)[:, None]).astype(np.int32)
    return d


def kernel(**inputs):
    inp = {k: np.asarray(v) for k, v in inputs.items()}
    B, S, _ = inp["x"].shape
    nc = build(S)
    in_maps = [_prep_inputs(inp, b, S) for b in range(B)]
    res = run_bass_kernel_spmd(nc, in_maps, core_ids=list(range(B)))
    return np.stack([np.asarray(r["out"]) for r in res.results], axis=0).astype(np.float32)
```

```python
import numpy as np
import ml_dtypes
import concourse.bass as bass
import concourse.mybir as mybir
from concourse.bass_utils import run_bass_kernel_spmd

F32 = mybir.dt.float32
BF16 = mybir.dt.bfloat16
I32 = mybir.dt.int32
AF = mybir.ActivationFunctionType
ALU = mybir.AluOpType
AX = mybir.AxisListType

D = 1024
DIN = 2568
NH = 8
HD = 64
CW = 31
NE = 32
DE = 256
CAP = 768
TS = 256
EPS = 1e-6
MT = 512


class TR:
    __slots__ = ("name", "w", "r", "dsem", "dcnt", "multi")

    all = []

    def __init__(self, name):
        TR.all.append(self)
        self.name = name
        self.w = {}
        self.r = {}
        self.dsem = {}
        self.dcnt = {}
        self.multi = False


class Sched:
    def __init__(self, nc):
        self.nc = nc
        self.eng = {"pe": nc.tensor, "act": nc.scalar, "dve": nc.vector, "pool": nc.gpsimd, "sp": nc.sync}
        self.sem = {k: nc.alloc_semaphore("sem_" + k) for k in self.eng}
        self.cnt = {k: 0 for k in self.eng}
        self.waited = {k: {} for k in self.eng}
        self.nsem = 0
        self.pool = {"sw": [], "hw": []}

    def _deps(self, e, reads, writes):
        deps = {}
        for t in list(reads) + list(writes):
            for k, (s, v) in t.w.items():
                if deps.get(k, (None, 0))[1] < v:
                    deps[k] = (s, v)
        for t in writes:
            for k, (s, v) in t.r.items():
                if deps.get(k, (None, 0))[1] < v:
                    deps[k] = (s, v)
        wd = self.waited[e]
        own = id(self.sem[e])
        for k, (s, v) in deps.items():
            if e == "pe" and k == own:
                continue
            if wd.get(k, 0) < v:
                self.eng[e].wait_ge(s, v)
                wd[k] = v

    def op(self, e, fn, reads=(), writes=()):
        self._deps(e, reads, writes)
        ins = fn()
        s = self.sem[e]
        ins.then_inc(s, 1)
        self.cnt[e] += 1
        v = self.cnt[e]
        k = id(s)
        for t in writes:
            t.w = {k: (s, v)}
            t.r = {}
        for t in reads:
            t.r[k] = (s, v)

    def dma(self, q, out, in_, reads=(), writes=(), owner=None, n=1, fn=None):
        self._deps(q, reads, writes)
        kind = "sw" if q == "pool" else "hw"
        if kind not in owner.dsem:
            if self.pool[kind]:
                owner.dsem[kind], owner.dcnt[kind] = self.pool[kind].pop()
            else:
                owner.dsem[kind] = self.nc.alloc_semaphore("d%s_%d" % (kind, self.nsem))
                owner.dcnt[kind] = 0
                self.nsem += 1
        if fn is None:
            ins = self.eng[q].dma_start(out=out, in_=in_)
        else:
            ins = fn()
        sem = owner.dsem[kind]
        ins.then_inc(sem, 16)
        owner.dcnt[kind] += 16
        k = id(sem)
        ent = (sem, owner.dcnt[kind])
        for t in writes:
            if not t.multi:
                t.w = {}
            t.w[k] = ent
            t.r = {}
        for t in reads:
            t.r[k] = ent

    def wait_all(self, e, tracked):
        self._deps(e, tracked, tracked)


def build(S, debug=None):
    NM = S // MT
    NT = S // 128
    NSLOT = NE * CAP
    nc = bass.Bass("TRN2", target_bir_lowering=False)
    TR.all = []
    sc = Sched(nc)

    def din(name, shape, dt=F32):
        return nc.dram_tensor(name, list(shape), dt, kind="ExternalInput").ap()

    def dscr(name, shape, dt):
        return nc.dram_tensor(name, list(shape), dt, kind=("ExternalOutput" if debug else "Internal")).ap()

    x = din("x", [S, D])
    w_in = din("w_in", [D, DIN])
    w_out = din("w_out", [D, D])
    wr = din("wr", [D, 36])
    w_gate = din("w_gate", [NE * 128, 8 * DE])
    w_up = din("w_up", [NE * 128, 8 * DE])
    w_down = din("w_down", [NE * 128, 2 * D])
    g1T = din("g1T", [128, 8])
    g2T = din("g2T", [128, 8])
    gconvT = din("gconvT", [128, 4])
    gattT = din("gattT", [128, 4])
    bdwT = din("bdwT", [128, 4])
    lngT = din("lngT", [128, 4])
    lnbT = din("lnbT", [128, 4])
    wdwT = din("wdwT", [128, 4 * CW])
    negbf = din("negbf", [128, 1])
    fing = din("fing", [128, D])
    g2bc = din("g2bc", [128, D])
    rbias = din("rbias", [128, 36])
    consts = din("consts", [128, 5 * 128])
    ebase = din("ebase", [128, NE])
    thr256 = din("thr256", [128, 33])
    tst256 = din("tst256", [128, NSLOT // TS])
    pcol = din("pcol", [128, 1])
    iota_tok = din("iota_tok", [128, NT], I32)
    out = nc.dram_tensor("out", [S, D], F32, kind="ExternalOutput").ap()

    Qd = dscr("Qd", [4, NM, 128, MT], BF16)
    Kd = dscr("Kd", [4, NM, 128, MT], BF16)
    Vd = dscr("Vd", [4, NM, 128, 4, 130], BF16)
    Ad = dscr("Ad", [NM, 128, MT], BF16)
    Mcd = dscr("Mcd", [NM, 128, 4, MT], BF16)
    Yad = dscr("Yad", [NM, 64, 8, MT], BF16)
    Xmd = dscr("Xmd", [S, D], F32)
    H2d = dscr("H2d", [S + 128, D], BF16)
    RTd = dscr("RTd", [NE * S + 128, 1], I32)
    Ysd = dscr("Ysd", [NSLOT, D], BF16)


    from contextlib import ExitStack
    stk = [ExitStack()]

    def sb(name, shape, dt=F32):
        return stk[0].enter_context(nc.sbuf_tensor(name, list(shape), dt))

    dma_owners = []

    def barrier():
        ents = [(sc.sem[k], sc.cnt[k]) for k in sc.eng if sc.cnt[k] > 0]
        for t in TR.all:
            for kd in t.dsem:
                if t.dcnt[kd] > 0:
                    ents.append((t.dsem[kd], t.dcnt[kd]))
        for e in sc.eng:
            wd = sc.waited[e]
            for (sm, v) in ents:
                if sm is sc.sem[e]:
                    continue
                if wd.get(id(sm), 0) < v:
                    sc.eng[e].wait_ge(sm, v)
                    wd[id(sm)] = v

    def new_phase():
        barrier()
        for t in TR.all:
            for kd in list(t.dsem):
                sc.pool[kd].append((t.dsem[kd], t.dcnt[kd]))
            t.dsem = {}
            t.dcnt = {}
            t.w = {}
            t.r = {}
        stk[0].close()
        stk[0] = ExitStack()

    psn = [0]

    def ps(name, shape, dt=F32):
        psn[0] += 1
        return stk[0].enter_context(nc.psum_tensor("%s_%d" % (name, psn[0]), list(shape), dt))

    def mkpsum(ntp, npm):
        tp_ = [ps("tp%d" % i, [128, 1024], BF16) for i in range(ntp)]
        ttp_ = [TR("tp%d" % i) for i in range(ntp)]
        pm_ = [ps("pm%d" % i, [128, 512]) for i in range(npm)]
        tpm_ = [TR("pm%d" % i) for i in range(npm)]
        return tp_, ttp_, pm_, tpm_

    V, A, P, PE, SP = "dve", "act", "pool", "pe", "sp"
    E = sc.eng
    rb_rt = E[P].to_reg(NE * S + 127)
    rb_h2 = E[P].to_reg(S + 127)
    rb_ys = E[P].to_reg(NSLOT - 1)

    def E_pool_to_reg(v):
        return E[P].to_reg(v)

    cst = sb("cst", [128, 5 * 128]); t_cst = TR("cst")
    sc.dma(SP, cst[:], consts, writes=[t_cst], owner=t_cst)
    ident_f = cst[:, 0:128]
    ones_f = cst[:, 128:256]
    identb = sb("identb", [128, 128], BF16); t_identb = TR("identb")
    onesb = sb("onesb", [128, 128], BF16)
    lstrb = sb("lstrb", [128, 128], BF16)
    negmb = sb("negmb", [128, 128], BF16)
    sc.op(V, lambda: E[V].tensor_copy(out=identb[:], in_=cst[:, 0:128]), [t_cst], [t_identb])
    sc.op(V, lambda: E[V].tensor_copy(out=onesb[:], in_=cst[:, 128:256]), [t_cst], [t_identb])
    sc.op(V, lambda: E[V].tensor_copy(out=lstrb[:], in_=cst[:, 256:384]), [t_cst], [t_identb])
    sc.op(V, lambda: E[V].tensor_copy(out=negmb[:], in_=cst[:, 384:512]), [t_cst], [t_identb])
    ones512 = sb("ones512", [128, MT]); t_ones512 = TR("ones512")
    sc.op(P, lambda: E[P].memset(ones512[:], 1.0), [], [t_ones512])
    epsb = sb("epsb", [128, 1]); t_eps = TR("epsb")
    sc.op(P, lambda: E[P].memset(epsb[:], EPS), [], [t_eps])
    t_c = TR("consts_all")

    def load_small(name, ap, shape, dt=F32):
        t = sb(name, shape, dt)
        sc.dma(SP, t[:], ap, writes=[t_c], owner=t_c)
        return t

    t_c.multi = True
    g1s = load_small("g1s", g1T, [128, 8])
    g2s = load_small("g2s", g2T, [128, 8])
    gconvs = load_small("gconvs", gconvT, [128, 4])
    gatts = load_small("gatts", gattT, [128, 4])
    bdws = load_small("bdws", bdwT, [128, 4])
    lngs = load_small("lngs", lngT, [128, 4])
    lnbs = load_small("lnbs", lnbT, [128, 4])
    wdws = load_small("wdws", wdwT, [128, 4 * CW])
    negbs = load_small("negbs", negbf, [128, 1])
    rbs = load_small("rbs", rbias, [128, 36])
    ebs = load_small("ebs", ebase, [128, NE])
    thrs = load_small("thrs", thr256, [128, 33])
    tsts = load_small("tsts", tst256, [128, NSLOT // TS])
    pcols = load_small("pcols", pcol, [128, 1])
    iotas = load_small("iotas", iota_tok, [128, NT], I32)

    biasT = sb("biasT", [128, NT, 8]); t_biasT = TR("biasT")
    dest1 = sb("dest1", [128, NT], I32); dest2 = sb("dest2", [128, NT], I32)
    wgt1 = sb("wgt1", [128, NT]); wgt2 = sb("wgt2", [128, NT])
    t_route = TR("route")
    NTL = NSLOT // TS
    widx = sb("widx", [128, NTL], I32); t_widx = TR("widx")
    ridx = sb("ridx", [128, NTL, TS // 128], I32)
    rb_w = E_pool_to_reg(NE * 128 - 1)

    selB = sb("selB", [128, 8, 128], BF16); t_sel = TR("selB")
    selc = sb("selc", [128, 8]); t_selc = TR("selc")
    sc.op(V, lambda: E[V].tensor_tensor(out=selc[:], in0=cst[:, 0:8], in1=cst[:, 32:40], op=ALU.add), [t_cst], [t_selc])
    for h in range(8):
        sc.op(V, lambda: E[V].tensor_scalar(out=selB[:, h, :], in0=cst[:, 128:256], scalar1=selc[:, h:h + 1], scalar2=None,
                                            op0=ALU.mult), [t_selc, t_cst], [t_sel])

    e64 = sb("e64", [65, 64]); t_e64 = TR("e64")
    sc.op(V, lambda: E[V].memset(e64[:], 0.0), [], [t_e64])
    sc.op(V, lambda: E[V].memset(e64[64:65, :], 1.0), [t_e64], [t_e64])
    persist = stk[0]
    stk[0] = ExitStack()
    WinB = sb("WinB", [128, 8, DIN], BF16); t_win = TR("WinB")
    WfB = sb("WfB", [128, 8, 128], BF16); t_wf = TR("WfB")
    stg = [sb("stg%d" % i, [128, DIN]) for i in range(1)] * 2
    t_stg = [TR("stg%d" % i) for i in range(1)] * 2
    sc.op(P, lambda: E[P].memset(WfB[:], 0.0), [], [t_wf])
    w_in_v = w_in.rearrange("(kc p) n -> p kc n", p=128)
    for kc in range(8):
        b = kc % 2
        sc.dma(SP, stg[b][:], w_in_v[:, kc, :], writes=[t_stg[b]], owner=t_stg[b])
        eng = A if kc % 2 == 0 else V
        if eng == A:
            sc.op(A, lambda: E[A].activation(out=WinB[:, kc, :], in_=stg[b][:], func=AF.Copy, scale=g1s[:, kc:kc + 1]),
                  [t_stg[b], t_c], [t_win])
        else:
            sc.op(V, lambda: E[V].tensor_scalar(out=WinB[:, kc, :], in0=stg[b][:], scalar1=g1s[:, kc:kc + 1], scalar2=None,
                                                op0=ALU.mult), [t_stg[b], t_c], [t_win])
    for q in range(4):
        sc.op(P, lambda: E[P].tensor_copy(out=WfB[:, :, q * 32:q * 32 + 8], in_=WinB[:, :, 2560:2568]), [t_win], [t_wf])

    diagW = sb("diagW", [128, CW, 4, 128], BF16); t_diag = TR("diagW")
    for k in range(CW):
        for c in range(4):
            eng = V if (k + c) % 2 == 0 else P
            sc.op(eng, lambda: E[eng].tensor_scalar(out=diagW[:, k, c, :], in0=cst[:, 0:128],
                                                    scalar1=wdws[:, c * CW + k:c * CW + k + 1], scalar2=None, op0=ALU.mult),
                  [t_cst, t_c], [t_diag])

    tp, t_tp, pm, t_pm = mkpsum(2, 6)

    xs = [sb("xs%d" % i, [128, 4, D]) for i in range(2)]; t_xs = [TR("xs%d" % i) for i in range(2)]
    junk = sb("junk", [128, D], BF16); t_junk = TR("junk")
    ssq = sb("ssq", [128, 4]); t_ssq = TR("ssq")
    rstd = sb("rstd", [128, 4]); t_rstd = TR("rstd")
    hb = sb("hb", [128, 4, D], BF16); t_hb = TR("hb")
    hT = sb("hT", [128, 8, MT], BF16); t_hT = TR("hT")
    aT = [sb("aT%d" % i, [128, 4, 30 + MT], BF16) for i in range(2)]; t_aT = [TR("aT%d" % i) for i in range(2)]
    sg = sb("sg", [128, MT]); t_sg = TR("sg")
    QT = sb("QT", [128, 4, MT], BF16); t_QT = TR("QT")
    KT = sb("KT", [128, 4, MT], BF16); t_KT = TR("KT")
    Vt = sb("Vt", [128, 4, 8, 65], BF16); t_Vt = TR("Vt")
    ef = sb("ef", [128, MT]); t_ef = TR("ef")
    spf = sb("spf", [128, MT]); t_spf = TR("spf")
    cum = sb("cum", [128, MT]); t_cum = TR("cum")
    carry = sb("carry", [128, 1]); t_carry = TR("carry")
    hiall = sb("hiall", [128, MT], BF16); t_hiall = TR("hiall")
    arow = sb("arow", [128, MT], BF16); t_arow = TR("arow")
    uu = sb("uu", [128, 4, MT]); t_uu = TR("uu")
    usq = sb("usq", [128, 4, MT], BF16); t_usq = TR("usq")
    m1 = sb("m1", [128, MT]); t_m1 = TR("m1")
    rs1 = sb("rs1", [128, MT]); t_rs1 = TR("rs1")
    mixc = sb("mixc", [128, 4, MT], BF16); t_mixc = TR("mixc")
    t_Qd = TR("Qd"); t_Kd = TR("Kd"); t_Vd = TR("Vd"); t_Ad = TR("Ad"); t_Mcd = TR("Mcd")
    for t in (t_Qd, t_Kd, t_Vd, t_Ad, t_Mcd):
        t.multi = True

    sc.op(P, lambda: E[P].memset(Vt[:], 1.0), [], [t_Vt])
    sc.op(P, lambda: E[P].memset(carry[:], 0.0), [], [t_carry])
    sc.op(P, lambda: E[P].memset(aT[0][:], 0.0), [], [t_aT[0]])
    sc.op(P, lambda: E[P].memset(aT[1][:], 0.0), [], [t_aT[1]])

    x_v = x.rearrange("(m s p) d -> m p s d", p=128, s=4)

    def inproj(bank, tbank, col0, M=128, lhs_src=None):
        def f():
            ins = None
            for kc in range(8):
                lhsT = WinB[:, kc, col0:col0 + M] if lhs_src is None else lhs_src[:, kc, :]
                ins = E[PE].matmul(pm[bank][0:M, :], lhsT=lhsT, rhs=hT[:, kc, :], start=(kc == 0), stop=(kc == 7))
            return ins
        sc.op(PE, f, [t_win, t_wf, t_hT], [tbank])

    def ph_A0(m):
        xb = xs[m % 2]; txb = t_xs[m % 2]
        sc.dma(SP, xb[:], x_v[m], writes=[txb], owner=txb)
        for s in range(4):
            sc.op(A, lambda: E[A].activation(out=junk[:], in_=xb[:, s, :], func=AF.Square, accum_out=ssq[:, s:s + 1]),
                  [txb], [t_junk, t_ssq])
        sc.op(A, lambda: E[A].activation(out=rstd[:], in_=ssq[:], func=AF.Sqrt, scale=1.0 / D, bias=epsb[:, 0:1]),
              [t_ssq, t_eps], [t_rstd])
        sc.op(V, lambda: E[V].reciprocal(out=rstd[:], in_=rstd[:]), [t_rstd], [t_rstd])
        for s in range(4):
            sc.op(A, lambda: E[A].activation(out=hb[:, s, :], in_=xb[:, s, :], func=AF.Copy, scale=rstd[:, s:s + 1]),
                  [txb, t_rstd], [t_hb])

    def ph_A1(m):
        xb = xs[m % 2]; txb = t_xs[m % 2]
        ab = aT[m % 2]; tab = t_aT[m % 2]
        abp = aT[(m + 1) % 2]; tabp = t_aT[(m + 1) % 2]
        for s in range(4):
            tb = tp[s % 2]; ttb = t_tp[s % 2]

            def f():
                ins = None
                for kc in range(8):
                    ins = E[PE].transpose(out=tb[:, kc * 128:(kc + 1) * 128], in_=hb[:, s, kc * 128:(kc + 1) * 128],
                                          identity=identb[:])
                return ins
            sc.op(PE, f, [t_hb, t_identb], [ttb])
            eng = A if s % 2 == 0 else V
            if eng == A:
                sc.op(A, lambda: E[A].copy(out=hT[:, :, s * 128:(s + 1) * 128], in_=tb[:].rearrange("p (k t) -> p k t", k=8)),
                      [ttb], [t_hT])
            else:
                sc.op(V, lambda: E[V].tensor_copy(out=hT[:, :, s * 128:(s + 1) * 128],
                                                  in_=tb[:].rearrange("p (k t) -> p k t", k=8)), [ttb], [t_hT])
        if m > 0:
            sc.op(P, lambda: E[P].tensor_copy(out=ab[:, :, 0:30], in_=abp[:, :, MT:MT + 30]), [tabp], [tab])
        for c in range(4):
            inproj(0, t_pm[0], 512 + c * 128)
            sc.op(A, lambda: E[A].activation(out=sg[:], in_=pm[0][:], func=AF.Sigmoid), [t_pm[0]], [t_sg])
            inproj(1, t_pm[1], c * 128)
            sc.op(V, lambda: E[V].tensor_tensor(out=ab[:, c, 30:30 + MT], in0=pm[1][:], in1=sg[:], op=ALU.mult),
                  [t_pm[1], t_sg], [tab])
        for p in range(4):
            inproj(0, t_pm[0], 1024 + p * 128)
            sc.op(A, lambda: E[A].copy(out=QT[:, p, :], in_=pm[0][:]), [t_pm[0]], [t_QT])
            inproj(1, t_pm[1], 1536 + p * 128)
            sc.op(V, lambda: E[V].tensor_copy(out=KT[:, p, :], in_=pm[1][:]), [t_pm[1]], [t_KT])
        sc.dma(P, Qd[:, m].rearrange("a p t -> p a t"), QT[:], reads=[t_QT], writes=[t_Qd], owner=t_QT)
        sc.dma(P, Kd[:, m].rearrange("a p t -> p a t"), KT[:], reads=[t_KT], writes=[t_Kd], owner=t_KT)
        for s in range(4):
            bank = (s % 2)

            def f():
                ins = None
                for kc in range(8):
                    ins = E[PE].matmul(pm[bank][:], lhsT=hT[:, kc, s * 128:(s + 1) * 128], rhs=WinB[:, kc, 2048:2560],
                                       start=(kc == 0), stop=(kc == 7))
                return ins
            sc.op(PE, f, [t_win, t_hT], [t_pm[bank]])
            eng = A if s % 2 == 0 else V
            src = pm[bank][:].rearrange("p (h d) -> p h d", h=8)
            if eng == A:
                sc.op(A, lambda: E[A].copy(out=Vt[:, s, :, 0:64], in_=src), [t_pm[bank]], [t_Vt])
            else:
                sc.op(V, lambda: E[V].tensor_copy(out=Vt[:, s, :, 0:64], in_=src), [t_pm[bank]], [t_Vt])
        for a in range(4):
            sc.dma(P, Vd[a, m], Vt[:, :, 2 * a:2 * a + 2, :].rearrange("p s h c -> p s (h c)"),
                   reads=[t_Vt], writes=[t_Vd], owner=t_Vt)
        inproj(0, t_pm[0], 0, lhs_src=WfB)
        sc.op(A, lambda: E[A].activation(out=ef[:], in_=pm[0][:], func=AF.Exp, scale=-1.0, bias=negbs[:, 0:1]),
              [t_pm[0], t_c], [t_ef])
        sc.op(A, lambda: E[A].activation(out=spf[:], in_=ef[:], func=AF.Ln, bias=1.0), [t_ef], [t_spf])
        sc.op(V, lambda: E[V].tensor_tensor_scan(out=cum[:], data0=ones512[:], data1=spf[:], initial=carry[:, 0:1],
                                                 op0=ALU.mult, op1=ALU.subtract), [t_spf, t_carry, t_ones512], [t_cum])
        sc.op(V, lambda: E[V].tensor_copy(out=carry[:], in_=cum[:, MT - 1:MT]), [t_cum], [t_carry])
        sc.op(V, lambda: E[V].tensor_scalar(out=hiall[:], in0=cum[:], scalar1=8.0, scalar2=None, op0=ALU.mult), [t_cum], [t_hiall])
        sc.op(P, lambda: E[P].tensor_copy(out=arow[:], in_=hiall[:]), [t_hiall], [t_arow])
        for q in (1, 3):
            sc.op(V, lambda: E[V].scalar_tensor_tensor(out=arow[q * 32:q * 32 + 32, :], in0=cum[q * 32:q * 32 + 32, :], scalar=8.0,
                                                       in1=hiall[q * 32:q * 32 + 32, :], op0=ALU.mult, op1=ALU.subtract),
                  [t_cum, t_hiall, t_arow], [t_arow])
        sc.dma(P, Ad[m], arow[:], reads=[t_arow], writes=[t_Ad], owner=t_arow)
        for s in range(4):
            sc.op(PE, lambda: E[PE].transpose(out=pm[1][:, 0:8], in_=cum[0:8, s * 128:(s + 1) * 128], identity=cst[0:8, 0:8]),
                  [t_cum, t_cst], [t_pm[1]])
            sc.op(V, lambda: E[V].tensor_scalar(out=biasT[:, m * 4 + s, :], in0=pm[1][:, 0:8], scalar1=-1.0, scalar2=None,
                                                op0=ALU.mult), [t_pm[1]], [t_biasT])

    def stat(bank, src, tsrc):
        ones_ = onesb[:] if src is usq else cst[:, 128:256]

        def f():
            ins = None
            for c in range(4):
                ins = E[PE].matmul(pm[bank][:], lhsT=ones_, rhs=src[:, c, :], start=(c == 0), stop=(c == 3))
            return ins
        sc.op(PE, f, [t_cst, t_identb, tsrc], [t_pm[bank]])

    def ph_B1(m):
        ab = aT[m % 2]; tab = t_aT[m % 2]
        for c in range(4):
            bank = 2 + (c % 2)

            def f():
                ins = None
                for k in range(CW):
                    ins = E[PE].matmul(pm[bank][:], lhsT=diagW[:, k, c, :], rhs=ab[:, c, k:k + MT], start=(k == 0),
                                       stop=(k == CW - 1))
                return ins
            sc.op(PE, f, [t_diag, tab], [t_pm[bank]])
            sc.op(A, lambda: E[A].activation(out=uu[:, c, :], in_=pm[bank][:], func=AF.Identity, bias=bdws[:, c:c + 1]),
                  [t_pm[bank], t_c], [t_uu])
            sc.op(A, lambda: E[A].activation(out=usq[:, c, :], in_=pm[bank][:], func=AF.Square, bias=bdws[:, c:c + 1]),
                  [t_pm[bank], t_c], [t_usq])

        stat(4, uu, t_uu)
        stat(5, usq, t_usq)

    def ph_B2(m):
        sc.op(V, lambda: E[V].tensor_scalar(out=m1[:], in0=pm[4][:], scalar1=1.0 / 512, scalar2=None, op0=ALU.mult), [t_pm[4]], [t_m1])
        sc.op(V, lambda: E[V].tensor_tensor(out=rs1[:], in0=m1[:], in1=m1[:], op=ALU.mult), [t_m1], [t_rs1])
        sc.op(V, lambda: E[V].scalar_tensor_tensor(out=rs1[:], in0=pm[5][:], scalar=1.0 / 512, in1=rs1[:], op0=ALU.mult,
                                                   op1=ALU.subtract), [t_pm[5], t_rs1], [t_rs1])
        sc.op(A, lambda: E[A].activation(out=rs1[:], in_=rs1[:], func=AF.Ln, bias=epsb[:, 0:1]), [t_rs1, t_eps], [t_rs1])
        sc.op(A, lambda: E[A].activation(out=rs1[:], in_=rs1[:], func=AF.Exp, scale=-0.5), [t_rs1], [t_rs1])
        for c in range(4):
            eng = V
            sc.op(eng, lambda: E[eng].tensor_tensor(out=uu[:, c, :], in0=uu[:, c, :], in1=m1[:], op=ALU.subtract), [t_uu, t_m1], [t_uu])
            sc.op(eng, lambda: E[eng].tensor_tensor(out=uu[:, c, :], in0=uu[:, c, :], in1=rs1[:], op=ALU.mult), [t_uu, t_rs1], [t_uu])
            sc.op(A, lambda: E[A].activation(out=uu[:, c, :], in_=uu[:, c, :], func=AF.Silu, scale=lngs[:, c:c + 1],
                                             bias=lnbs[:, c:c + 1]), [t_uu, t_c], [t_uu])
            sc.op(A, lambda: E[A].activation(out=usq[:, c, :], in_=uu[:, c, :], func=AF.Square), [t_uu], [t_usq])
        stat(4, usq, t_usq)
        sc.op(A, lambda: E[A].activation(out=rs1[:], in_=pm[4][:], func=AF.Ln, scale=1.0 / 512, bias=epsb[:, 0:1]),
              [t_pm[4], t_eps], [t_rs1])
        sc.op(A, lambda: E[A].activation(out=rs1[:], in_=rs1[:], func=AF.Exp, scale=-0.5), [t_rs1], [t_rs1])
        for c in range(4):
            eng = V
            sc.op(eng, lambda: E[eng].tensor_tensor(out=mixc[:, c, :], in0=uu[:, c, :], in1=rs1[:], op=ALU.mult),
                  [t_uu, t_rs1], [t_mixc])
        sc.dma(P, Mcd[m], mixc[:], reads=[t_mixc], writes=[t_Mcd], owner=t_mixc)


    ph_A0(0)
    ph_A1(0)
    for m in range(NM):
        if m + 1 < NM:
            ph_A0(m + 1)
        ph_B1(m)
        if m + 1 < NM:
            ph_A1(m + 1)
        ph_B2(m)

    if debug == "p1a":
        sc.wait_all(P, [t_Qd, t_Kd, t_Vd, t_Ad, t_Mcd, t_biasT])
        return nc

    new_phase()
    tp, t_tp, pm, t_pm = mkpsum(0, 8)
    NSB = 3
    Qs = [[sb("Qs%d_%d" % (i, j), [128, MT], BF16) for j in range(2)] for i in range(2)]
    t_Qs = [[TR("Qs%d_%d" % (i, j)) for j in range(2)] for i in range(2)]
    NKB = 5
    Kc = [[sb("Kc%d_%d" % (i, j), [128, MT], BF16) for j in range(2)] for i in range(NKB)]
    t_Kc = [[TR("Kc%d_%d" % (i, j)) for j in range(2)] for i in range(NKB)]
    Vc = [sb("Vc%d" % i, [128, 4, 2, 128], BF16) for i in range(NKB)]; t_Vc = [TR("Vc%d" % i) for i in range(NKB)]
    NPB = 4
    Pt = [sb("Pt%d" % i, [128, MT], BF16) for i in range(NPB)]; t_Pt = [TR("Pt%d" % i) for i in range(NPB)]
    Osb = [sb("Osb%d" % i, [65, MT]) for i in range(2)]; t_Osb = [TR("Osb%d" % i) for i in range(2)]
    Ya = [sb("Ya%d" % i, [64, 2, MT], BF16) for i in range(2)]; t_Ya = [TR("Ya%d" % i) for i in range(2)]
    for i_ in range(2):
        for j_ in range(2):
            sc.op(P, lambda: E[P].memset(Qs[i_][j_][:], 0.0), [], [t_Qs[i_][j_]])
            t_Qs[i_][j_].multi = True
    for i_ in range(NKB):
        for j_ in range(2):
            sc.op(P, lambda: E[P].memset(Kc[i_][j_][:], 0.0), [], [t_Kc[i_][j_]])
            sc.op(P, lambda: E[P].memset(Kc[i_][j_][64:66, :], 1.0), [t_Kc[i_][j_]], [t_Kc[i_][j_]])
        sc.op(P, lambda: E[P].memset(Vc[i_][:], 0.0), [], [t_Vc[i_]])
        t_Vc[i_].multi = True
    t_Yad = TR("Yad"); t_Yad.multi = True

    steps = []
    grp = 0
    for a in range(4):
        for i in range(NM):
            for c in range(i + 1):
                for hh in range(2):
                    for jj in range(4):
                        steps.append(dict(a=a, i=i, c=c, hh=hh, jj=jj, grp=grp,
                                          first=(c == 0 and jj == 0), last=(c == i and jj == 3)))
            grp += 1
    nld = 0
    pendB = []
    cur_q = {}
    cur_c = {}

    def emit_S(n):
        nonlocal nld
        st = steps[n]
        a, i, c, hh, jj = st["a"], st["i"], st["c"], st["hh"], st["jj"]
        g = st["grp"]
        if g not in cur_q:
            qb = g % 2
            for h2 in range(2):
                h_ = 2 * a + h2
                tq = t_Qs[qb][h2]
                sc.dma(SP, Qs[qb][h2][0:64, :], Qd[a, i, h2 * 64:(h2 + 1) * 64, :], reads=[t_Qd], writes=[tq], owner=tq)
                sc.dma(SP, Qs[qb][h2][64:65, :], Ad[i, h_:h_ + 1, :], reads=[t_Ad], writes=[tq], owner=tq)
                sc.dma(SP, Qs[qb][h2][65:66, :], Ad[i, 32 + h_:33 + h_, :], reads=[t_Ad], writes=[tq], owner=tq)
            cur_q[g] = qb
        if (g, c) not in cur_c:
            kb = nld % NKB
            nld += 1
            for h2 in range(2):
                sc.dma(SP, Kc[kb][h2][0:64, :], Kd[a, c, h2 * 64:(h2 + 1) * 64, :], reads=[t_Kd], writes=[t_Kc[kb][h2]],
                       owner=t_Kc[kb][h2])
                sc.dma(SP, Vc[kb][:, :, h2, 0:65], Vd[a, c, :, :, h2 * 65:(h2 + 1) * 65], reads=[t_Vd], writes=[t_Vc[kb]],
                       owner=t_Vc[kb])
            cur_c[(g, c)] = kb
        qb = cur_q[g]; kb = cur_c[(g, c)]
        st["qb"] = qb; st["kb"] = kb
        h = 2 * a + hh
        diag = (c == i)
        q0 = 128 * jj if diag else 0
        st["q0"] = q0; st["h"] = h
        bank = n % NSB
        st["bank"] = bank

        def f():
            ins = E[PE].matmul(pm[bank][:, q0:], lhsT=Kc[kb][hh][:, jj * 128:(jj + 1) * 128], rhs=Qs[qb][hh][:, q0:],
                               start=True, stop=not diag)
            if diag:
                ins = E[PE].matmul(pm[bank][:, q0:q0 + 128], lhsT=identb[:], rhs=negmb[:], start=False, stop=True)
            return ins
        sc.op(PE, f, [t_Kc[kb][hh], t_Qs[qb][hh], t_identb], [t_pm[bank]])

    def emit_exp(n):
        st = steps[n]
        bank = st["bank"]; q0 = st["q0"]; h = st["h"]
        j = 4 * st["c"] + st["jj"]
        pb = n % NPB
        sc.op(A, lambda: E[A].activation(out=Pt[pb][:, q0:], in_=pm[bank][:, q0:], func=AF.Exp, scale=0.125,
                                         bias=biasT[:, j, h:h + 1]), [t_pm[bank], t_biasT], [t_Pt[pb]])

    def emit_PV(n):
        st = steps[n]
        hh = st["hh"]; q0 = st["q0"]; kb = st["kb"]; jj = st["jj"]
        pb = n % NPB
        ob = 3 + 2 * (st["grp"] % 2) + hh
        sc.op(PE, lambda: E[PE].matmul(pm[ob][:, q0:], lhsT=Vc[kb][:, jj, hh, :], rhs=Pt[pb][:, q0:],
                                       start=st["first"], stop=st["last"]), [t_Vc[kb], t_Pt[pb]], [t_pm[ob]])
        if st["last"] and hh == 1:
            a, i = st["a"], st["i"]
            yb = st["grp"] % 2
            gsel = st["grp"] % 2
            for h2 in range(2):
                obk = 3 + 2 * gsel + h2
                sc.op(V, lambda: E[V].tensor_copy(out=Osb[h2][:], in_=pm[obk][0:65, :]), [t_pm[obk]], [t_Osb[h2]])
                sc.op(V, lambda: E[V].reciprocal(out=Osb[h2][64:65, :], in_=Osb[h2][64:65, :]), [t_Osb[h2]], [t_Osb[h2]])

            def partB(a=a, i=i, yb=yb):
                for h2 in range(2):
                    sc.op(PE, lambda: E[PE].matmul(pm[7][0:64, :], lhsT=e64[:], rhs=Osb[h2][:], start=True, stop=True),
                          [t_e64, t_Osb[h2]], [t_pm[7]])
                    sc.op(V, lambda: E[V].tensor_tensor(out=Ya[yb][:, h2, :], in0=Osb[h2][0:64, :], in1=pm[7][0:64, :], op=ALU.mult),
                          [t_Osb[h2], t_pm[7]], [t_Ya[yb]])
                sc.dma(P, Yad[i, :, 2 * a:2 * a + 2, :], Ya[yb][:], reads=[t_Ya[yb]], writes=[t_Yad], owner=t_Ya[yb])
            pendB.append([6, partB])

    NS = len(steps)
    LAG = 2
    for n in range(NS + LAG):
        if n < NS:
            emit_S(n)
            emit_exp(n)
        if n >= LAG:
            emit_PV(n - LAG)
        for pb_ in list(pendB):
            pb_[0] -= 1
            if pb_[0] <= 0:
                pb_[1]()
                pendB.remove(pb_)
    for pb_ in list(pendB):
        pb_[1]()

    if debug == "p1b":
        sc.wait_all(P, [t_Qd, t_Kd, t_Vd, t_Ad, t_Mcd, t_biasT, t_Yad])
        return nc

    new_phase()
    tp, t_tp, pm, t_pm = mkpsum(2, 6)
    t_Xmd = TR("Xmd"); t_Xmd.multi = True
    t_H2d = TR("H2d"); t_H2d.multi = True
    t_RTd = TR("RTd"); t_RTd.multi = True
    WoC = sb("WoC", [128, 4, D], BF16); t_WoC = TR("WoC")
    WoA = sb("WoA", [128, 4, D], BF16); t_WoA = TR("WoA")
    WrB = sb("WrB", [128, 8, 36], BF16); t_WrB = TR("WrB")
    wst = sb("wst", [128, 8, D]); t_wst = TR("wst")
    wrs = sb("wrs", [128, 8, 36]); t_wrs = TR("wrs")
    g2b = sb("g2b", [128, D]); t_g2b = TR("g2b")
    sc.dma(SP, g2b[:], g2bc, writes=[t_g2b], owner=t_g2b)
    sc.dma(SP, wst[:, 0:4, :], w_out[0:512, :].rearrange("(c p) n -> p c n", p=128), writes=[t_wst], owner=t_wst)
    for c in range(4):
        sc.op(V, lambda: E[V].tensor_scalar(out=WoC[:, c, :], in0=wst[:, c, :], scalar1=gconvs[:, c:c + 1], scalar2=None,
                                            op0=ALU.mult), [t_wst, t_c], [t_WoC])
    sc.dma(SP, wst[:, 4:8, :], w_out[512:1024, :].rearrange("(a p) n -> p a n", p=128), writes=[t_wst], owner=t_wst)
    for a in range(4):
        sc.op(V, lambda: E[V].tensor_scalar(out=WoA[:, a, :], in0=wst[:, 4 + a, :], scalar1=gatts[:, a:a + 1], scalar2=None,
                                            op0=ALU.mult), [t_wst, t_c], [t_WoA])
    sc.dma(SP, wrs[:], wr.rearrange("(kc p) n -> p kc n", p=128), writes=[t_wrs], owner=t_wrs)
    sc.op(V, lambda: E[V].tensor_copy(out=WrB[:], in_=wrs[:]), [t_wrs], [t_WrB])
    NRT = (NE * S + 128) // 128
    rti = sb("rti", [128, NRT], I32); t_rti = TR("rti")
    sc.op(P, lambda: E[P].memset(rti[:], S), [], [t_rti])
    sc.dma(P, RTd.rearrange("(p n) o -> p (n o)", p=128), rti[:], reads=[t_rti], writes=[t_RTd], owner=t_rti)
    dp1 = sb("dp1", [128, NT], I32); dp2 = sb("dp2", [128, NT], I32)
    zrow = sb("zrow", [128, D], BF16); t_zrow = TR("zrow")
    sc.op(P, lambda: E[P].memset(zrow[:], 0.0), [], [t_zrow])
    sc.dma(P, H2d[S:S + 128, :], zrow[:], reads=[t_zrow], writes=[t_H2d], owner=t_zrow)

    mcs = [sb("mcs%d" % i, [128, 4, MT], BF16) for i in range(2)]; t_mcs = [TR("mcs%d" % i) for i in range(2)]
    yas = [sb("yas%d" % i, [128, 4, MT], BF16) for i in range(2)]; t_yas = [TR("yas%d" % i) for i in range(2)]
    for i_ in range(2):
        t_yas[i_].multi = True
    xs2 = [sb("xs2%d" % i, [128, 4, D]) for i in range(2)]; t_xs2 = [TR("xs2%d" % i) for i in range(2)]
    sqa = sb("sqa", [128, 4, MT], BF16); t_sqa = TR("sqa")
    r4 = sb("r4", [128, MT]); t_r4 = TR("r4")
    mixa = sb("mixa", [128, 4, MT], BF16); t_mixa = TR("mixa")
    xm = [sb("xm%d" % i, [128, D]) for i in range(4)]; t_xm = [TR("xm%d" % i) for i in range(4)]
    h2 = [sb("h2%d" % i, [128, D], BF16) for i in range(4)]; t_h2 = [TR("h2%d" % i) for i in range(4)]
    h2T = [sb("h2T%d" % i, [128, 8, 128], BF16) for i in range(4)]; t_h2T = [TR("h2T%d" % i) for i in range(4)]
    junk2 = sb("junk2", [128, D], BF16); t_junk2 = TR("junk2")
    R = []
    for i_ in range(4):
        R.append(dict(
            sm=sb("sm%d" % i_, [128, 32]), lg=sb("lg%d" % i_, [128, 36]), l2=sb("l2_%d" % i_, [128, 8]), l2m=sb("l2m%d" % i_, [128, 8]),
            oh1=sb("oh1_%d" % i_, [128, 8]), oh2=sb("oh2_%d" % i_, [128, 8]), ohg=sb("ohg%d" % i_, [128, 4]), e1=sb("e1_%d" % i_, [128, 4]),
            M1=sb("M1_%d" % i_, [128, 32]), M2=sb("M2_%d" % i_, [128, 32]), Mb=sb("Mb%d" % i_, [128, 32], BF16),
            Rf=sb("Rf%d" % i_, [128, 32]), tmpr=sb("tmpr%d" % i_, [128, 32]), tmpr2=sb("tmpr2_%d" % i_, [128, 32]),
            t=TR("rt%d" % i_), tlg=TR("lgp%d" % i_), tR=TR("Rp%d" % i_), tC=TR("Cp%d" % i_)))
    rank1 = sb("rank1", [128, NT]); rank2 = sb("rank2", [128, NT]); eid1 = sb("eid1", [128, NT]); eid2 = sb("eid2", [128, NT])
    t_rk = TR("rankeid")
    carryR = sb("carryR", [128, 32]); t_carryR = TR("carryR")
    sc.op(V, lambda: E[V].memset(carryR[:], 0.0), [], [t_carryR])
    lg4 = sb("lg4", [128, 4, 36]); mx4 = sb("mx4", [128, 4]); ohg4 = sb("ohg4", [128, 4, 4]); d14 = sb("d14", [128, 4, 4])
    e14 = sb("e14", [128, 4, 4]); se4 = sb("se4", [128, 4]); p1s4 = sb("p1s4", [128, 4]); prod4 = sb("prod4", [128, 4, 4, 8])
    l24 = sb("l24", [128, 4, 8]); m14 = sb("m14", [128, 4]); oh14 = sb("oh14", [128, 4, 8]); l2m4 = sb("l2m4", [128, 4, 8])
    m24 = sb("m24", [128, 4]); oh24 = sb("oh24", [128, 4, 8]); d214 = sb("d214", [128, 4]); ed4 = sb("ed4", [128, 4])
    den4 = sb("den4", [128, 4]); w14 = sb("w14", [128, 4]); w24 = sb("w24", [128, 4])
    M14 = sb("M14", [128, 4, 32]); M24 = sb("M24", [128, 4, 32]); Mb4 = sb("Mb4", [128, 4, 32], BF16)
    Rf4 = sb("Rf4", [128, 4, 32]); tmp4 = sb("tmp4", [128, 4, 32]); dpf4 = sb("dpf4", [128, 4])
    t_rt4 = TR("rt4")

    def vr(fn, reads=(), writes=()):
        sc.op(V, fn, list(reads) + [t_rt4], list(writes) + [t_rt4])

    def bc(ap2d, shape3, axis):
        pst, _ = ap2d.ap[0]
        est, n = ap2d.ap[1]
        if axis == 1:
            return bass.AP(tensor=ap2d.tensor, offset=ap2d.offset, ap=[[pst, 128], [0, shape3[1]], [est, n]])
        return bass.AP(tensor=ap2d.tensor, offset=ap2d.offset, ap=[[pst, 128], [est, n], [0, shape3[2]]])

    def load1c(m):
        b = m % 2
        sc.dma(SP, mcs[b][:], Mcd[m], reads=[t_Mcd], writes=[t_mcs[b]], owner=t_mcs[b])
        for hh in range(2):
            sc.dma(SP, yas[b][hh * 64:(hh + 1) * 64, :, :], Yad[m].rearrange("d (a two) t -> d a two t", two=2)[:, :, hh, :],
                   reads=[t_Yad], writes=[t_yas[b]], owner=t_yas[b])
        sc.dma(SP, xs2[b][:], x_v[m], writes=[t_xs2[b]], owner=t_xs2[b])

    load1c(0)
    for m in range(NM):
        b = m % 2
        if m + 1 < NM:
            load1c(m + 1)
        sc.op(A, lambda: E[A].activation(out=sqa[:], in_=yas[b][:], func=AF.Square), [t_yas[b]], [t_sqa])

        def f():
            ins = None
            for a in range(4):
                ins = E[PE].matmul(pm[5][:], lhsT=onesb[:], rhs=sqa[:, a, :], start=(a == 0), stop=(a == 3))
            return ins
        sc.op(PE, f, [t_identb, t_sqa], [t_pm[5]])
        sc.op(A, lambda: E[A].activation(out=r4[:], in_=pm[5][:], func=AF.Ln, scale=1.0 / 512, bias=epsb[:, 0:1]),
              [t_pm[5], t_eps], [t_r4])
        sc.op(A, lambda: E[A].activation(out=r4[:], in_=r4[:], func=AF.Exp, scale=-0.5), [t_r4], [t_r4])
        for a in range(4):
            sc.op(V, lambda: E[V].tensor_tensor(out=mixa[:, a, :], in0=yas[b][:, a, :], in1=r4[:], op=ALU.mult),
                  [t_yas[b], t_r4], [t_mixa])
        for s in range(4):
            tile = m * 4 + s
            rs_ = R[s]; sm = rs_["sm"]
            xb2 = xm[s]; txm = t_xm[s]
            hb2 = h2[s]; th2 = t_h2[s]
            for n in range(2):
                bank = n

                def f():
                    ins = None
                    for c in range(4):
                        ins = E[PE].matmul(pm[bank][:], lhsT=mcs[b][:, c, s * 128:(s + 1) * 128], rhs=WoC[:, c, n * 512:(n + 1) * 512],
                                           start=(c == 0), stop=False)
                    for a in range(4):
                        ins = E[PE].matmul(pm[bank][:], lhsT=mixa[:, a, s * 128:(s + 1) * 128], rhs=WoA[:, a, n * 512:(n + 1) * 512],
                                           start=False, stop=(a == 3))
                    return ins
                sc.op(PE, f, [t_mcs[b], t_mixa, t_WoC, t_WoA], [t_pm[bank]])
                sc.op(V, lambda: E[V].tensor_tensor(out=xb2[:, n * 512:(n + 1) * 512], in0=pm[bank][:],
                                                    in1=xs2[b][:, s, n * 512:(n + 1) * 512], op=ALU.add),
                      [t_pm[bank], t_xs2[b]], [txm])
            sc.dma(SP, Xmd[tile * 128:(tile + 1) * 128, :], xb2[:], reads=[txm], writes=[t_Xmd], owner=txm)
            sc.op(A, lambda: E[A].activation(out=junk2[:], in_=xb2[:], func=AF.Square, accum_out=sm[:, 0:1]),
                  [txm], [t_junk2, rs_["t"]])
            sc.op(A, lambda: E[A].activation(out=sm[:, 1:2], in_=sm[:, 0:1], func=AF.Sqrt, scale=1.0 / D, bias=epsb[:, 0:1]),
                  [rs_["t"], t_eps], [rs_["t"]])
            sc.op(V, lambda: E[V].reciprocal(out=sm[:, 2:3], in_=sm[:, 1:2]), [rs_["t"]], [rs_["t"]])
            sc.op(V, lambda: E[V].scalar_tensor_tensor(out=hb2[:], in0=xb2[:], scalar=sm[:, 2:3], in1=g2b[:], op0=ALU.mult,
                                                       op1=ALU.mult), [txm, rs_["t"], t_g2b], [th2])
            sc.dma(SP, H2d[tile * 128:(tile + 1) * 128, :], hb2[:], reads=[th2], writes=[t_H2d], owner=th2)
        for s in range(4):
            tile = m * 4 + s
            rs_ = R[s]
            hb2 = h2[s]; th2 = t_h2[s]
            tb = tp[tile % 2]; ttb = t_tp[tile % 2]

            def f():
                ins = None
                for kc in range(8):
                    ins = E[PE].transpose(out=tb[:, kc * 128:(kc + 1) * 128], in_=hb2[:, kc * 128:(kc + 1) * 128], identity=identb[:])
                return ins
            sc.op(PE, f, [th2, t_identb], [ttb])
            sc.op(A, lambda: E[A].copy(out=h2T[s][:], in_=tb[:].rearrange("p (k t) -> p k t", k=8)), [ttb], [t_h2T[s]])

            def f():
                ins = None
                for kc in range(8):
                    ins = E[PE].matmul(pm[2][:, s * 64:s * 64 + 36], lhsT=h2T[s][:, kc, :], rhs=WrB[:, kc, :], start=(kc == 0),
                                       stop=(kc == 7))
                return ins
            sc.op(PE, f, [t_h2T[s], t_WrB], [rs_["tlg"]])

        c0 = 4 * m
        lgp = pm[2][:, 0:256].rearrange("p (s c) -> p s c", c=64)[:, :, 0:36]
        vr(lambda: E[V].tensor_tensor(out=lg4[:], in0=lgp, in1=bc(rbs[:], [128, 4, 36], 1), op=ALU.add),
           [R[0]["tlg"], R[1]["tlg"], R[2]["tlg"], R[3]["tlg"], t_c])
        vr(lambda: E[V].reduce_max(out=mx4[:], in_=lg4[:, :, 0:4], axis=AX.X))
        vr(lambda: E[V].tensor_tensor(out=ohg4[:], in0=lg4[:, :, 0:4], in1=bc(mx4[:], [128, 4, 4], 2), op=ALU.is_equal))
        vr(lambda: E[V].tensor_tensor(out=d14[:], in0=lg4[:, :, 0:4], in1=bc(mx4[:], [128, 4, 4], 2), op=ALU.subtract))
        sc.op(A, lambda: E[A].activation(out=e14[:], in_=d14[:], func=AF.Exp), [t_rt4], [t_rt4])
        vr(lambda: E[V].reduce_sum(out=se4[:], in_=e14[:], axis=AX.X))
        vr(lambda: E[V].reciprocal(out=p1s4[:], in_=se4[:]))
        vr(lambda: E[V].tensor_tensor(out=prod4[:], in0=lg4[:, :, 4:36].rearrange("p s (g e) -> p s g e", e=8),
                                      in1=bass.AP(tensor=ohg4[:].tensor, offset=ohg4[:].offset,
                                                  ap=[[16, 128], [4, 4], [1, 4], [0, 8]]), op=ALU.mult))
        vr(lambda: E[V].reduce_sum(out=l24[:], in_=prod4[:].rearrange("p s g e -> p s e g"), axis=AX.X))
        vr(lambda: E[V].reduce_max(out=m14[:], in_=l24[:], axis=AX.X))
        vr(lambda: E[V].tensor_tensor(out=oh14[:], in0=l24[:], in1=bc(m14[:], [128, 4, 8], 2), op=ALU.is_equal))
        vr(lambda: E[V].scalar_tensor_tensor(out=l2m4[:], in0=oh14[:], scalar=-1e30, in1=l24[:], op0=ALU.mult, op1=ALU.add))
        vr(lambda: E[V].reduce_max(out=m24[:], in_=l2m4[:], axis=AX.X))
        vr(lambda: E[V].tensor_tensor(out=oh24[:], in0=l2m4[:], in1=bc(m24[:], [128, 4, 8], 2), op=ALU.is_equal))
        vr(lambda: E[V].tensor_tensor(out=d214[:], in0=m24[:], in1=m14[:], op=ALU.subtract))
        sc.op(A, lambda: E[A].activation(out=ed4[:], in_=d214[:], func=AF.Exp), [t_rt4], [t_rt4])
        vr(lambda: E[V].tensor_scalar(out=den4[:], in0=ed4[:], scalar1=1.0, scalar2=None, op0=ALU.add))
        vr(lambda: E[V].reciprocal(out=w14[:], in_=den4[:]))
        vr(lambda: E[V].tensor_tensor(out=wgt1[:, c0:c0 + 4], in0=w14[:], in1=p1s4[:], op=ALU.mult), [], [t_route])
        vr(lambda: E[V].tensor_tensor(out=w24[:], in0=ed4[:], in1=w14[:], op=ALU.mult))
        vr(lambda: E[V].tensor_tensor(out=wgt2[:, c0:c0 + 4], in0=w24[:], in1=p1s4[:], op=ALU.mult), [], [t_route])
        ohg_b = bass.AP(tensor=ohg4[:].tensor, offset=ohg4[:].offset, ap=[[16, 128], [4, 4], [1, 4], [0, 8]])
        oh1_b = bass.AP(tensor=oh14[:].tensor, offset=oh14[:].offset, ap=[[32, 128], [8, 4], [0, 4], [1, 8]])
        oh2_b = bass.AP(tensor=oh24[:].tensor, offset=oh24[:].offset, ap=[[32, 128], [8, 4], [0, 4], [1, 8]])
        vr(lambda: E[V].tensor_tensor(out=M14[:].rearrange("p s (g e) -> p s g e", e=8), in0=ohg_b, in1=oh1_b, op=ALU.mult))
        vr(lambda: E[V].tensor_tensor(out=M24[:].rearrange("p s (g e) -> p s g e", e=8), in0=ohg_b, in1=oh2_b, op=ALU.mult))
        vr(lambda: E[V].tensor_tensor(out=Mb4[:], in0=M14[:], in1=M24[:], op=ALU.add))

        def fR():
            ins = None
            for s_ in range(4):
                E[PE].matmul(pm[3][:, s_ * 32:(s_ + 1) * 32], lhsT=lstrb[:], rhs=Mb4[:, s_, :], start=True, stop=True)
                ins = E[PE].matmul(pm[4][:, s_ * 32:(s_ + 1) * 32], lhsT=onesb[:], rhs=Mb4[:, s_, :], start=True, stop=True)
            return ins
        sc.op(PE, fR, [t_identb, t_rt4], [t_pm[3], t_pm[4]])
        for s_ in range(4):
            vr(lambda: E[V].tensor_tensor(out=Rf4[:, s_, :], in0=pm[3][:, s_ * 32:(s_ + 1) * 32], in1=carryR[:], op=ALU.add),
               [t_pm[3], t_carryR])
            sc.op(V, lambda: E[V].tensor_tensor(out=carryR[:], in0=carryR[:], in1=pm[4][:, s_ * 32:(s_ + 1) * 32], op=ALU.add),
                  [t_pm[4], t_carryR], [t_carryR])
        for (Mx, rk, ei) in ((M14, rank1, eid1), (M24, rank2, eid2)):
            vr(lambda: E[V].tensor_tensor(out=tmp4[:], in0=Rf4[:], in1=Mx[:], op=ALU.mult))
            vr(lambda: E[V].reduce_sum(out=rk[:, c0:c0 + 4], in_=tmp4[:], axis=AX.X), [], [t_rk])
            vr(lambda: E[V].tensor_tensor(out=tmp4[:], in0=bc(ebs[:], [128, 4, 32], 1), in1=Mx[:], op=ALU.mult), [t_c])
            vr(lambda: E[V].reduce_sum(out=ei[:, c0:c0 + 4], in_=tmp4[:], axis=AX.X), [], [t_rk])
        for (ei, rk, dpp) in ((eid1, rank1, dp1), (eid2, rank2, dp2)):
            vr(lambda: E[V].scalar_tensor_tensor(out=dpf4[:], in0=ei[:, c0:c0 + 4], scalar=float(S), in1=rk[:, c0:c0 + 4],
                                                 op0=ALU.mult, op1=ALU.add), [t_rk])
            vr(lambda: E[V].tensor_copy(out=dpp[:, c0:c0 + 4], in_=dpf4[:]))
            for s_ in range(4):
                tile = c0 + s_
                sc.dma(P, None, None, reads=[t_rt4, t_c], writes=[t_RTd], owner=t_rt4,
                       fn=lambda: E[P].indirect_dma_start(out=RTd[:, :], out_offset=bass.IndirectOffsetOnAxis(ap=dpp[:, tile:tile + 1], axis=0),
                                                          in_=iotas[:, tile:tile + 1], in_offset=None,
                                                          bounds_check=rb_rt, oob_is_err=False))

    def bc(ap2d, shape3, axis):
        pst, _ = ap2d.ap[0]
        est, n = ap2d.ap[1]
        if axis == 1:
            return bass.AP(tensor=ap2d.tensor, offset=ap2d.offset, ap=[[pst, 128], [0, shape3[1]], [est, n]])
        return bass.AP(tensor=ap2d.tensor, offset=ap2d.offset, ap=[[pst, 128], [est, n], [0, shape3[2]]])
    cmpk = sb("cmpk", [128, 32, 33]); ntl = sb("ntl", [128, 32]); padd = sb("padd", [128, 32]); pend = sb("pend", [128, 32])
    pstt = sb("pstt", [128, 32]); bigt = sb("bigt", [128, NT, 32]); psel = sb("psel", [128, NT]); destf = sb("destf", [128, NT])
    cmpt = sb("cmpt", [128, NTL, 32]); tef = sb("tef", [128, NTL]); widf = sb("widf", [128, NTL])
    t_fin = TR("fin")

    def fop(fn, reads=(), writes=()):
        sc.op(V, fn, list(reads) + [t_fin], list(writes) + [t_fin])
    fop(lambda: E[V].tensor_tensor(out=cmpk[:], in0=bc(carryR[:], [128, 32, 33], 2), in1=bc(thrs[:], [128, 32, 33], 1), op=ALU.is_gt),
        [t_carryR, t_c])
    fop(lambda: E[V].reduce_sum(out=ntl[:], in_=cmpk[:], axis=AX.X))
    fop(lambda: E[V].tensor_scalar(out=padd[:], in0=ntl[:], scalar1=float(TS), scalar2=None, op0=ALU.mult))
    fop(lambda: E[V].tensor_tensor_scan(out=pend[:], data0=ones512[:, 0:32], data1=padd[:], initial=0.0, op0=ALU.mult, op1=ALU.add),
        [t_ones512])
    fop(lambda: E[V].tensor_tensor(out=pstt[:], in0=pend[:], in1=padd[:], op=ALU.subtract))
    for (rk, ei, dd) in ((rank1, eid1, dest1), (rank2, eid2, dest2)):
        fop(lambda: E[V].tensor_tensor(out=bigt[:], in0=bc(ei[:], [128, NT, 32], 2), in1=bc(ebs[:], [128, NT, 32], 1), op=ALU.is_equal),
            [t_rk, t_c])
        fop(lambda: E[V].tensor_tensor(out=bigt[:], in0=bigt[:], in1=bc(pstt[:], [128, NT, 32], 1), op=ALU.mult))
        fop(lambda: E[V].reduce_sum(out=psel[:], in_=bigt[:], axis=AX.X))
        fop(lambda: E[V].tensor_tensor(out=destf[:], in0=psel[:], in1=rk[:], op=ALU.add), [t_rk])
        fop(lambda: E[V].tensor_copy(out=dd[:], in_=destf[:]), [], [t_route])
    fop(lambda: E[V].tensor_tensor(out=cmpt[:], in0=bc(pend[:], [128, NTL, 32], 1), in1=bc(tsts[:], [128, NTL, 32], 2), op=ALU.is_le),
        [t_c])
    fop(lambda: E[V].reduce_sum(out=tef[:], in_=cmpt[:], axis=AX.X))
    fop(lambda: E[V].tensor_scalar(out=widf[:], in0=tef[:], scalar1=128.0, scalar2=pcols[:, 0:1], op0=ALU.mult, op1=ALU.add), [t_c])
    eqf = sb("eqf", [128, NTL])
    fop(lambda: E[V].memset(eqf[:], 0.0))
    fop(lambda: E[V].tensor_tensor(out=eqf[:, 2:NTL], in0=tef[:, 2:NTL], in1=tef[:, 0:NTL - 2], op=ALU.is_equal))
    fop(lambda: E[V].scalar_tensor_tensor(out=widf[:], in0=eqf[:], scalar=float(NE * 128), in1=widf[:], op0=ALU.mult, op1=ALU.add))
    fop(lambda: E[V].tensor_copy(out=widx[:], in_=widf[:]), [], [t_widx])
    pstl = sb("pstl", [128, NTL]); basef = sb("basef", [128, NTL]); rif = sb("rif", [128, NTL])
    fop(lambda: E[V].tensor_tensor(out=cmpt[:], in0=bc(tef[:], [128, NTL, 32], 2), in1=bc(ebs[:], [128, NTL, 32], 1), op=ALU.is_equal),
        [t_c])
    fop(lambda: E[V].tensor_tensor(out=cmpt[:], in0=cmpt[:], in1=bc(pstt[:], [128, NTL, 32], 1), op=ALU.mult))
    fop(lambda: E[V].reduce_sum(out=pstl[:], in_=cmpt[:], axis=AX.X))
    fop(lambda: E[V].scalar_tensor_tensor(out=basef[:], in0=tef[:], scalar=float(S), in1=tsts[:], op0=ALU.mult, op1=ALU.add), [t_c])
    fop(lambda: E[V].tensor_tensor(out=basef[:], in0=basef[:], in1=pstl[:], op=ALU.subtract))
    for st in range(TS // 128):
        fop(lambda: E[V].tensor_scalar(out=rif[:], in0=basef[:], scalar1=pcols[:, 0:1], scalar2=float(128 * st), op0=ALU.add, op1=ALU.add),
            [t_c])
        fop(lambda: E[V].tensor_copy(out=ridx[:, :, st], in_=rif[:]), [], [t_widx])

    new_phase()
    tp, t_tp, pm, t_pm = mkpsum(2, 6)
    t_Ysd = TR("Ysd"); t_Ysd.multi = True
    NSUB = TS // 128
    Wg = [sb("Wg%d" % i, [128, 8, DE], BF16) for i in range(2)]; t_Wg = [TR("Wg%d" % i) for i in range(2)]
    Wu = [sb("Wu%d" % i, [128, 8, DE], BF16) for i in range(2)]; t_Wu = [TR("Wu%d" % i) for i in range(2)]
    Wd = [sb("Wd%d" % i, [128, 2, D], BF16) for i in range(2)]; t_Wd = [TR("Wd%d" % i) for i in range(2)]
    Wgs = [sb("Wgs%d" % i, [128, 8 * DE]) for i in range(2)]; t_Wgs = [TR("Wgs%d" % i) for i in range(2)]
    Wus = [sb("Wus%d" % i, [128, 8 * DE]) for i in range(2)]; t_Wus = [TR("Wus%d" % i) for i in range(2)]
    Wds = [sb("Wds%d" % i, [128, 2 * D]) for i in range(2)]; t_Wds = [TR("Wds%d" % i) for i in range(2)]
    for i_ in range(2):
        sc.op(V, lambda: E[V].memset(Wgs[i_][:], 0.0), [], [t_Wgs[i_]])
        sc.op(V, lambda: E[V].memset(Wus[i_][:], 0.0), [], [t_Wus[i_]])
        sc.op(V, lambda: E[V].memset(Wds[i_][:], 0.0), [], [t_Wds[i_]])
    idxs = [[sb("idx%d_%d" % (i, j), [128, 1], I32) for j in range(NSUB)] for i in range(2)]
    t_idx = [[TR("idx%d_%d" % (i, j)) for j in range(NSUB)] for i in range(2)]
    Xg = [[sb("Xg%d_%d" % (i, j), [128, D], BF16) for j in range(NSUB)] for i in range(2)]
    t_Xg = [[TR("Xg%d_%d" % (i, j)) for j in range(NSUB)] for i in range(2)]
    XgT = [sb("XgT%d" % i, [128, 8, TS], BF16) for i in range(2)]; t_XgT = [TR("XgT%d" % i) for i in range(2)]
    sgl = [sb("sgl%d" % i, [128, TS]) for i in range(2)]; t_sgl = [TR("sgl%d" % i) for i in range(2)]
    AT = [sb("AT%d" % i, [128, 2, TS], BF16) for i in range(2)]; t_AT = [TR("AT%d" % i) for i in range(2)]
    NYB = 4
    Yt = [sb("Yt%d" % i, [128, D], BF16) for i in range(NYB)]; t_Yt = [TR("Yt%d" % i) for i in range(NYB)]

    def L2(t):
        b = t % 2
        for st in range(NSUB):
            sc.dma(P, None, None, reads=[t_widx, t_RTd], writes=[t_idx[b][st]], owner=t_idx[b][st],
                   fn=lambda: E[P].indirect_dma_start(out=idxs[b][st][:, :], out_offset=None, in_=RTd[:, :],
                                                      in_offset=bass.IndirectOffsetOnAxis(ap=ridx[:, t, st:st + 1], axis=0),
                                                      bounds_check=rb_rt, oob_is_err=False))
        for (wt, twt, src) in ((Wgs[b], t_Wgs[b], w_gate), (Wus[b], t_Wus[b], w_up), (Wds[b], t_Wds[b], w_down)):
            sc.dma(P, None, None, reads=[t_widx], writes=[twt], owner=twt,
                   fn=lambda: E[P].indirect_dma_start(out=wt[:, :], out_offset=None, in_=src[:, :],
                                                      in_offset=bass.IndirectOffsetOnAxis(ap=widx[:, t:t + 1], axis=0),
                                                      bounds_check=rb_w, oob_is_err=False))
        for st in range(NSUB):
            sc.dma(P, None, None, reads=[t_idx[b][st], t_H2d], writes=[t_Xg[b][st]], owner=t_Xg[b][st],
                   fn=lambda: E[P].indirect_dma_start(out=Xg[b][st][:, :], out_offset=None, in_=H2d[:, :],
                                                      in_offset=bass.IndirectOffsetOnAxis(ap=idxs[b][st][:, 0:1], axis=0),
                                                      bounds_check=rb_h2, oob_is_err=False))

    def C2(t):
        b = t % 2
        sc.op(V, lambda: E[V].tensor_copy(out=Wg[b][:].rearrange("p k n -> p (k n)"), in_=Wgs[b][:]), [t_Wgs[b]], [t_Wg[b]])
        sc.op(A, lambda: E[A].copy(out=Wu[b][:].rearrange("p k n -> p (k n)"), in_=Wus[b][:]), [t_Wus[b]], [t_Wu[b]])
        sc.op(V, lambda: E[V].tensor_copy(out=Wd[b][:].rearrange("p k n -> p (k n)"), in_=Wds[b][:]), [t_Wds[b]], [t_Wd[b]])

    def T2(t):
        b = t % 2
        for st in range(NSUB):
            tb = tp[st % 2]; ttb = t_tp[st % 2]

            def f():
                ins = None
                for kc in range(8):
                    ins = E[PE].transpose(out=tb[:, kc * 128:(kc + 1) * 128], in_=Xg[b][st][:, kc * 128:(kc + 1) * 128],
                                          identity=identb[:])
                return ins
            sc.op(PE, f, [t_Xg[b][st], t_identb], [ttb])
            if st % 2 == 0:
                sc.op(A, lambda: E[A].copy(out=XgT[b][:, :, st * 128:(st + 1) * 128], in_=tb[:].rearrange("p (k t) -> p k t", k=8)),
                      [ttb], [t_XgT[b]])
            else:
                sc.op(V, lambda: E[V].tensor_copy(out=XgT[b][:, :, st * 128:(st + 1) * 128],
                                                  in_=tb[:].rearrange("p (k t) -> p k t", k=8)), [ttb], [t_XgT[b]])

    def M2a(t):
        b = t % 2
        N = TS
        for fc in range(2):
            bg = 2 * fc; bu = 2 * fc + 1

            def fg():
                ins = None
                for kc in range(8):
                    ins = E[PE].matmul(pm[bg][:, 0:N], lhsT=Wg[b][:, kc, fc * 128:(fc + 1) * 128], rhs=XgT[b][:, kc, 0:N],
                                       start=(kc == 0), stop=(kc == 7))
                return ins

            def fu():
                ins = None
                for kc in range(8):
                    ins = E[PE].matmul(pm[bu][:, 0:N], lhsT=Wu[b][:, kc, fc * 128:(fc + 1) * 128], rhs=XgT[b][:, kc, 0:N],
                                       start=(kc == 0), stop=(kc == 7))
                return ins
            sc.op(PE, fg, [t_Wg[b], t_XgT[b]], [t_pm[bg]])
            sc.op(PE, fu, [t_Wu[b], t_XgT[b]], [t_pm[bu]])
            sc.op(A, lambda: E[A].activation(out=sgl[fc][:, 0:N], in_=pm[bg][:, 0:N], func=AF.Silu), [t_pm[bg]], [t_sgl[fc]])
            sc.op(V, lambda: E[V].tensor_tensor(out=AT[b][:, fc, 0:N], in0=pm[bu][:, 0:N], in1=sgl[fc][:, 0:N], op=ALU.mult),
                  [t_pm[bu], t_sgl[fc]], [t_AT[b]])

    ny = [0]

    def M2b(t):
        b = t % 2
        for st in range(NSUB):
            yb = ny[0] % NYB; ny[0] += 1
            for n in range(2):
                bank = 4 + n

                def fy():
                    ins = None
                    for fc in range(2):
                        ins = E[PE].matmul(pm[bank][:], lhsT=AT[b][:, fc, st * 128:(st + 1) * 128],
                                           rhs=Wd[b][:, fc, n * 512:(n + 1) * 512], start=(fc == 0), stop=(fc == 1))
                    return ins
                sc.op(PE, fy, [t_AT[b], t_Wd[b]], [t_pm[bank]])
                if n == 0:
                    sc.op(A, lambda: E[A].copy(out=Yt[yb][:, 0:512], in_=pm[bank][:]), [t_pm[bank]], [t_Yt[yb]])
                else:
                    sc.op(V, lambda: E[V].tensor_copy(out=Yt[yb][:, 512:1024], in_=pm[bank][:]), [t_pm[bank]], [t_Yt[yb]])
            r0 = t * TS + st * 128
            sc.dma(SP, Ysd[r0:r0 + 128, :], Yt[yb][:], reads=[t_Yt[yb]], writes=[t_Ysd], owner=t_Yt[yb])

    L2(0); C2(0); T2(0)
    for t in range(NTL):
        if t + 1 < NTL:
            L2(t + 1)
        M2a(t)
        if t + 1 < NTL:
            C2(t + 1)
        M2b(t)
        if t + 1 < NTL:
            T2(t + 1)

    new_phase()
    t_out = TR("out"); t_out.multi = True
    fgs = sb("fgs", [128, D]); t_fgs = TR("fgs")
    sc.dma(SP, fgs[:], fing, writes=[t_fgs], owner=t_fgs)
    NB3 = 4
    xm3 = [sb("xm3%d" % i, [128, D]) for i in range(NB3)]; t_xm3 = [TR("xm3%d" % i) for i in range(NB3)]
    y1 = [sb("y1%d" % i, [128, D], BF16) for i in range(NB3)]; t_y1 = [TR("y1%d" % i) for i in range(NB3)]
    y2 = [sb("y2%d" % i, [128, D], BF16) for i in range(NB3)]; t_y2 = [TR("y2%d" % i) for i in range(NB3)]
    ot = [sb("ot%d" % i, [128, D]) for i in range(2)]; t_ot = [TR("ot%d" % i) for i in range(2)]
    junk3 = sb("junk3", [128, D], BF16); t_junk3 = TR("junk3")
    sm3 = [sb("sm3_%d" % i, [128, 4]) for i in range(2)]; t_sm3 = [TR("sm3_%d" % i) for i in range(2)]

    def L3(t):
        b = t % NB3
        sc.dma(SP, xm3[b][:], Xmd[t * 128:(t + 1) * 128, :], reads=[t_Xmd], writes=[t_xm3[b]], owner=t_xm3[b])
        for (yy, tyy, dd) in ((y1[b], t_y1[b], dest1), (y2[b], t_y2[b], dest2)):
            sc.dma(P, None, None, reads=[t_route, t_Ysd], writes=[tyy], owner=tyy,
                   fn=lambda: E[P].indirect_dma_start(out=yy[:, :], out_offset=None, in_=Ysd[:, :],
                                                      in_offset=bass.IndirectOffsetOnAxis(ap=dd[:, t:t + 1], axis=0),
                                                      bounds_check=rb_ys, oob_is_err=False))

    def C3(t):
        b = t % NB3
        ob = t % 2
        s3 = sm3[ob]; ts3 = t_sm3[ob]
        sc.op(V, lambda: E[V].scalar_tensor_tensor(out=xm3[b][:], in0=y1[b][:], scalar=wgt1[:, t:t + 1], in1=xm3[b][:],
                                                   op0=ALU.mult, op1=ALU.add), [t_y1[b], t_xm3[b], t_route], [t_xm3[b]])
        sc.op(V, lambda: E[V].scalar_tensor_tensor(out=xm3[b][:], in0=y2[b][:], scalar=wgt2[:, t:t + 1], in1=xm3[b][:],
                                                   op0=ALU.mult, op1=ALU.add), [t_y2[b], t_xm3[b], t_route], [t_xm3[b]])
        sc.op(A, lambda: E[A].activation(out=junk3[:], in_=xm3[b][:], func=AF.Square, accum_out=s3[:, 0:1]),
              [t_xm3[b]], [t_junk3, ts3])
        sc.op(A, lambda: E[A].activation(out=s3[:, 1:2], in_=s3[:, 0:1], func=AF.Sqrt, scale=1.0 / D, bias=epsb[:, 0:1]),
              [ts3, t_eps], [ts3])
        sc.op(V, lambda: E[V].reciprocal(out=s3[:, 2:3], in_=s3[:, 1:2]), [ts3], [ts3])
        sc.op(V, lambda: E[V].scalar_tensor_tensor(out=ot[ob][:], in0=xm3[b][:], scalar=s3[:, 2:3], in1=fgs[:],
                                                   op0=ALU.mult, op1=ALU.mult), [t_xm3[b], ts3, t_fgs], [t_ot[ob]])
        sc.dma(SP, out[t * 128:(t + 1) * 128, :], ot[ob][:], reads=[t_ot[ob]], writes=[t_out], owner=t_ot[ob])

    for t in range(min(2, NT)):
        L3(t)
    for t in range(NT):
        if t + 2 < NT:
            L3(t + 2)
        C3(t)
    sc.wait_all(P, [t_out])
    barrier()
    return nc


def _prep_inputs(inp, b, S):
    f = np.float32

    def T8(v):
        return np.ascontiguousarray(v.reshape(8, 128).T).astype(f)

    def T4(v):
        return np.ascontiguousarray(v.reshape(4, 128).T).astype(f)
    d = {}
    d["x"] = np.ascontiguousarray(inp["x"][b][:S])
    d["w_in"] = inp["w_in"][0]
    d["w_out"] = inp["w_out"][0]
    d["wr"] = np.ascontiguousarray(np.concatenate([inp["w_r1"][0]] + [inp["w_r2"][0][g] for g in range(4)], axis=1))
    d["w_gate"] = np.ascontiguousarray(inp["w_gate"][0].reshape(32, 8, 128, 256).transpose(0, 2, 1, 3).reshape(32 * 128, 2048))
    d["w_up"] = np.ascontiguousarray(inp["w_up"][0].reshape(32, 8, 128, 256).transpose(0, 2, 1, 3).reshape(32 * 128, 2048))
    d["w_down"] = np.ascontiguousarray(inp["w_down"][0].reshape(32, 2, 128, 1024).transpose(0, 2, 1, 3).reshape(32 * 128, 2048))
    d["g1T"] = T8(inp["norm1_g"][0])
    d["g2T"] = T8(inp["norm2_g"][0])
    d["gconvT"] = T4(inp["out_g_conv"][0])
    d["gattT"] = T4(inp["out_g_att"][0])
    d["bdwT"] = T4(inp["b_dw"][0])
    d["lngT"] = T4(inp["conv_ln_g"][0])
    d["lnbT"] = T4(inp["conv_ln_b"][0])
    wd = inp["w_dw"][0]
    d["wdwT"] = np.ascontiguousarray(wd.reshape(31, 4, 128).transpose(2, 1, 0).reshape(128, 124))
    nb = np.zeros((128, 1), f)
    for q in range(4):
        nb[q * 32:q * 32 + 8, 0] = -inp["b_f"][0]
    d["negbf"] = nb
    d["fing"] = np.ascontiguousarray(np.broadcast_to(inp["final_g"][None, :], (128, 1024))).astype(f)
    rb = np.concatenate([inp["b_r1"][0], inp["b_r2"][0].reshape(-1)])
    d["g2bc"] = np.ascontiguousarray(np.broadcast_to(inp["norm2_g"][0][None, :], (128, 1024))).astype(f)
    d["rbias"] = np.ascontiguousarray(np.broadcast_to(rb[None, :], (128, 36))).astype(f)
    c = np.zeros((128, 640), f)
    c[:, 0:128] = np.eye(128)
    c[:, 128:256] = 1.0
    c[:, 256:384] = np.triu(np.ones((128, 128)), 1)
    c[:, 384:512] = np.tril(np.ones((128, 128)), -1) * -30000.0
    c[:, 512:640] = 1.0
    d["consts"] = c
    d["ebase"] = np.ascontiguousarray(np.broadcast_to(np.arange(32)[None, :], (128, 32))).astype(f)
    d["thr256"] = np.ascontiguousarray(np.broadcast_to((np.arange(33) * TS)[None, :], (128, 33))).astype(f)
    NTL = NE * CAP // TS
    d["tst256"] = np.ascontiguousarray(np.broadcast_to((np.arange(NTL) * TS)[None, :], (128, NTL))).astype(f)
    d["pcol"] = np.arange(128).reshape(128, 1).astype(f)
    NT = S // 128
    d["iota_tok"] = (np.arange(NT)[None, :] * 128 + np.arange(128)[:, None]).astype(np.int32)
    return d


def kernel(**inputs):
    inp = {k: np.asarray(v) for k, v in inputs.items()}
    B, S, _ = inp["x"].shape
    nc = build(S)
    in_maps = [_prep_inputs(inp, b, S) for b in range(B)]
    res = run_bass_kernel_spmd(nc, in_maps, core_ids=list(range(B)))
    return np.stack([np.asarray(r["out"]) for r in res.results], axis=0).astype(np.float32)
```
